# Optimizing a Trainium2 kernel written in Bass

```python
import math
import jax, jax.numpy as jnp
from jax import lax
import numpy as np

D_MODEL = 2048
BATCH = 1
SEQ = 16384
DEPTH = 1

CHUNK = 64
N_META = 16
HEAD_DIM = 128
SB_HEADS = 8
FOX_HEADS = 8
SB_WIDTH = SB_HEADS * HEAD_DIM
FOX_WIDTH = FOX_HEADS * HEAD_DIM
MIX_WIDTH = SB_WIDTH + FOX_WIDTH
C_IN = 3 * SB_WIDTH + 4 * FOX_WIDTH + FOX_HEADS
Q_BLOCK = 128
PEER_HEADS = 8
PEER_TOPK = 16
N_KEYS = 128
N_EXPERTS = N_KEYS * N_KEYS
D_KEY = 256
PEER_BLOCK = 128
EPS = 1e-6

kernel_name = "hymba_sbfox_peer_block"


def rms_norm(x, gain):
    xf = x.astype(jnp.float32)
    y = xf * lax.rsqrt(jnp.mean(xf * xf, axis=-1, keepdims=True) + EPS)
    return y.astype(x.dtype) * gain


def to_query_blocks(a):
    b, l = a.shape[:2]
    return jnp.moveaxis(a.reshape((b, l // Q_BLOCK, Q_BLOCK) + a.shape[2:]), 1, 0)


def from_query_blocks(a):
    a = jnp.moveaxis(a, 0, 1)
    b, nb, qb = a.shape[:3]
    return a.reshape(b, nb * qb, -1)


def stick_breaking_attention(q, k, v):
    L = k.shape[1]
    k_pos = jnp.arange(L)
    scale = 1.0 / math.sqrt(HEAD_DIM)
    starts = jnp.arange(L // Q_BLOCK, dtype=jnp.int32) * Q_BLOCK

    def block(args):
        qb, start = args
        q_pos = start + jnp.arange(Q_BLOCK)
        z = jnp.einsum('bqhd,bkhd->bhqk', qb, k).astype(jnp.float32) * scale
        visible = k_pos[None, :] < q_pos[:, None]
        log_1mb = jnp.where(visible, jax.nn.log_sigmoid(-z), 0.0)
        after = lax.cumsum(log_1mb, axis=3, reverse=True) - log_1mb
        w = jnp.where(visible, jnp.exp(jax.nn.log_sigmoid(z) + after), 0.0)
        return jnp.einsum('bhqk,bkhd->bqhd', w.astype(v.dtype), v)

    out = lax.map(block, (to_query_blocks(q), starts))
    return from_query_blocks(out)


def forgetting_attention(q, k, v, log_f):
    L = k.shape[1]
    k_pos = jnp.arange(L)
    scale = 1.0 / math.sqrt(HEAD_DIM)
    cum_f = lax.cumsum(log_f, axis=1)
    cum_f_k = jnp.transpose(cum_f, (0, 2, 1))
    starts = jnp.arange(L // Q_BLOCK, dtype=jnp.int32) * Q_BLOCK

    def block(args):
        qb, fq, start = args
        q_pos = start + jnp.arange(Q_BLOCK)
        z = jnp.einsum('bqhd,bkhd->bhqk', qb, k).astype(jnp.float32) * scale
        logits = z + jnp.transpose(fq, (0, 2, 1))[..., None] - cum_f_k[:, :, None, :]
        visible = k_pos[None, :] <= q_pos[:, None]
        p = jax.nn.softmax(jnp.where(visible, logits, -jnp.inf), axis=-1)
        return jnp.einsum('bhqk,bkhd->bqhd', p.astype(v.dtype), v)

    out = lax.map(block, (to_query_blocks(q), to_query_blocks(cum_f), starts))
    return from_query_blocks(out)


def mixer_sublayer(h, w_in, b_forget, fox_q_gain, fox_k_gain, sb_out_gain, fox_out_gain, w_out):
    B, L, _ = h.shape
    proj = h @ w_in
    s3 = 3 * SB_WIDTH
    sb_q, sb_k, sb_v, fq, fk, fv, fgate, flogit = jnp.split(
        proj,
        [SB_WIDTH, 2 * SB_WIDTH, s3, s3 + FOX_WIDTH, s3 + 2 * FOX_WIDTH,
         s3 + 3 * FOX_WIDTH, s3 + 4 * FOX_WIDTH],
        axis=-1)

    def heads(a, n):
        return a.reshape(B, L, n, HEAD_DIM)

    sb = stick_breaking_attention(heads(sb_q, SB_HEADS), heads(sb_k, SB_HEADS), heads(sb_v, SB_HEADS))
    log_f = jax.nn.log_sigmoid((flogit + b_forget).astype(jnp.float32))
    fox = forgetting_attention(rms_norm(heads(fq, FOX_HEADS), fox_q_gain),
                               rms_norm(heads(fk, FOX_HEADS), fox_k_gain),
                               heads(fv, FOX_HEADS), log_f)
    merged = jnp.concatenate(
        [rms_norm(sb, sb_out_gain),
         rms_norm(fox, fox_out_gain) * jax.nn.sigmoid(fgate)], axis=-1)
    return merged @ w_out


def peer_sublayer(h, w_query, sub_keys, u, v):
    B, L, D = h.shape
    blocks = h.reshape(-1, PEER_BLOCK, D)

    def block(hb):
        q = (hb @ w_query).reshape(PEER_BLOCK, PEER_HEADS, 2, D_KEY // 2)
        s = jnp.einsum('thpc,hpnc->thpn', q, sub_keys).astype(jnp.float32)
        top_s, top_i = lax.top_k(s, PEER_TOPK)
        cand_s = (top_s[:, :, 0, :, None] + top_s[:, :, 1, None, :]).reshape(PEER_BLOCK, PEER_HEADS, -1)
        cand_i = (top_i[:, :, 0, :, None] * N_KEYS + top_i[:, :, 1, None, :]).reshape(PEER_BLOCK, PEER_HEADS, -1)
        best_s, best_j = lax.top_k(cand_s, PEER_TOPK)
        idx = jnp.take_along_axis(cand_i, best_j, axis=-1)
        g = jax.nn.softmax(best_s, axis=-1)
        u_sel = jnp.take(u, idx, axis=0)
        a = jax.nn.gelu(jnp.einsum('thkd,td->thk', u_sel, hb), approximate=False)
        v_sel = jnp.take(v, idx, axis=0)
        return jnp.einsum('thk,thkd->td', (g * a).astype(hb.dtype), v_sel)

    return lax.map(block, blocks).reshape(B, L, D)


def setup_inputs(seed: int = 0) -> dict:
    key = jax.random.key(seed)
    ks = jax.random.split(key, 15)
    f32 = jnp.float32
    nrm = lambda k, shape, s: jax.random.normal(k, shape, f32) * s
    gain = lambda k, shape: 1.0 + 0.02 * jax.random.normal(k, shape, f32)
    return {
        "x": nrm(ks[0], (BATCH, SEQ, D_MODEL), 1.0),
        "meta_tokens": nrm(ks[1], (N_META, D_MODEL), 1.0),
        "norm_mix": gain(ks[2], (DEPTH, D_MODEL)),
        "w_in": nrm(ks[3], (DEPTH, D_MODEL, C_IN), D_MODEL ** -0.5),
        "b_forget": jax.random.uniform(ks[4], (DEPTH, FOX_HEADS), f32, 1.0, 4.0),
        "fox_q_gain": gain(ks[5], (DEPTH, HEAD_DIM)),
        "fox_k_gain": gain(ks[6], (DEPTH, HEAD_DIM)),
        "sb_out_gain": gain(ks[7], (DEPTH, SB_WIDTH)),
        "fox_out_gain": gain(ks[8], (DEPTH, FOX_WIDTH)),
        "w_out": nrm(ks[9], (DEPTH, MIX_WIDTH, D_MODEL), MIX_WIDTH ** -0.5),
        "norm_ffn": gain(ks[10], (DEPTH, D_MODEL)),
        "peer_w_query": nrm(ks[11], (DEPTH, D_MODEL, PEER_HEADS * D_KEY), D_MODEL ** -0.5),
        "peer_sub_keys": nrm(ks[12], (DEPTH, PEER_HEADS, 2, N_KEYS, D_KEY // 2), (D_KEY // 2) ** -0.5),
        "peer_u": nrm(ks[13], (DEPTH, N_EXPERTS, D_MODEL), D_MODEL ** -0.5),
        "peer_v": nrm(ks[14], (DEPTH, N_EXPERTS, D_MODEL), PEER_HEADS ** -0.5),
    }


def reference(x, meta_tokens, norm_mix, w_in, b_forget, fox_q_gain, fox_k_gain,
              sb_out_gain, fox_out_gain, w_out, norm_ffn, peer_w_query, peer_sub_keys,
              peer_u, peer_v):
    B, S, D = x.shape
    l_pad = ((S + N_META + Q_BLOCK - 1) // Q_BLOCK) * Q_BLOCK
    meta = jnp.broadcast_to(meta_tokens[None].astype(x.dtype), (B, N_META, D))
    pad = jnp.zeros((B, l_pad - S - N_META, D), x.dtype)
    h = jnp.concatenate([meta, x, pad], axis=1)
    for l in range(DEPTH):
        h = h + mixer_sublayer(rms_norm(h, norm_mix[l]), w_in[l], b_forget[l],
                               fox_q_gain[l], fox_k_gain[l], sb_out_gain[l],
                               fox_out_gain[l], w_out[l])
        h = h + peer_sublayer(rms_norm(h, norm_ffn[l]), peer_w_query[l],
                              peer_sub_keys[l], peer_u[l], peer_v[l])
    return h[:, N_META:N_META + S]
```

```python
import math
from contextlib import ExitStack

import numpy as np
import concourse.bass as bass
import concourse.mybir as mybir
from concourse.bass_utils import run_bass_kernel_spmd

F32 = mybir.dt.float32
BF16 = mybir.dt.bfloat16
I32 = mybir.dt.int32
U32 = mybir.dt.uint32
AF = mybir.ActivationFunctionType
ALU = mybir.AluOpType
AX = mybir.AxisListType

D = 2048
KC = 16
NCORE = 8
N_META = 16
EPS = 1e-6
SCALE = 1.0 / math.sqrt(128.0)
NEG = -30000.0
N_EXP = 16384


class Tk:
    def __init__(self, t, name=""):
        self.t = t
        self.name = name
        self.lw = None
        self.rd = []

    def __getitem__(self, k):
        return self.t[k]


class TkView:
    def __init__(self, base, fn):
        self.base = base
        self.fn = fn
        self.name = base.name

    def __getitem__(self, k):
        return self.fn(self.base.t[:])[k]

    @property
    def lw(self):
        return self.base.lw

    @lw.setter
    def lw(self, v):
        self.base.lw = v

    @property
    def rd(self):
        return self.base.rd

    @rd.setter
    def rd(self, v):
        self.base.rd = v


class Op:
    __slots__ = ("eng", "fn", "deps", "dma", "stream", "sidx", "awaited", "mile", "idx")

    def __init__(self, eng, fn, dma=False, stream=None):
        self.eng = eng
        self.fn = fn
        self.deps = []
        self.dma = dma
        self.stream = stream
        self.sidx = 0
        self.awaited = False
        self.mile = 0
        self.idx = 0


class Sched:
    ENGS = ("tensor", "vector", "scalar", "gpsimd", "sync")

    def __init__(self, nc, es):
        self.nc = nc
        self.es = es
        self.es0 = es
        self.ops = {e: [] for e in self.ENGS}
        self.streams = {}
        self.nops = 0

    def tile(self, name, shape, dt):
        return Tk(self.es.enter_context(self.nc.sbuf_tensor(name, list(shape), dt)), name)

    def psum(self, name, shape=(128, 512), dt=F32):
        return Tk(self.es.enter_context(self.nc.psum_tensor(name, list(shape), dt)), name)

    def dram(self, name, shape, dt, kind="Internal"):
        t = self.nc.dram_tensor(name, list(shape), dt, kind=kind)
        return Tk(t.ap(), name)

    def _add(self, op, reads, writes):
        deps = []
        for t in reads:
            if t.lw is not None:
                deps.append(t.lw)
        for t in writes:
            if t.lw is not None and (t.lw.dma or op.dma or t.lw.eng != op.eng):
                deps.append(t.lw)
            deps.extend(r for r in t.rd if r.dma or op.dma or r.eng != op.eng)
        seen = set()
        for d in deps:
            if d is op or id(d) in seen:
                continue
            seen.add(id(d))
            if (not d.dma) and d.eng == op.eng and op.eng == "tensor" and not op.dma:
                continue
            op.deps.append(d)
            d.awaited = True
        for t in reads:
            t.rd = [r for r in t.rd if r.dma or r.eng != op.eng or op.dma] + [op]
        for t in writes:
            t.lw = op
            t.rd = []
        op.idx = self.nops
        self.nops += 1
        self.ops[op.eng].append(op)
        return op

    def op(self, eng, fn, reads=(), writes=()):
        return self._add(Op(eng, fn), reads, writes)

    def dma(self, eng, out, in_, reads=(), writes=(), stream=None, fn=None):
        if stream is None:
            stream = "dma_" + (writes[0].name if writes else "x")
        if fn is None:
            fn = lambda e, out=out, in_=in_: e.dma_start(out=out, in_=in_)
        op = Op(eng, fn, dma=True, stream=stream)
        st = self.streams.setdefault(stream, [])
        if st:
            op.deps.append(st[-1])
            st[-1].awaited = True
        st.append(op)
        op.sidx = len(st)
        return self._add(op, reads, writes)

    def flush(self, final_streams=()):
        nc = self.nc
        if not hasattr(self, "prog"):
            self.prog = {e: self.es0.enter_context(nc.semaphore("prog_" + e)) for e in self.ENGS}
            self.ssem = {}
            self.mcount = {e: 0 for e in self.ENGS}
            self.waited = {e: {} for e in self.ENGS}
            self.first_flush = True
        prog, ssem = self.prog, self.ssem
        for s in self.streams:
            if s not in ssem:
                ssem[s] = self.es0.enter_context(nc.semaphore("s_" + s))
        for e in self.ENGS:
            comp = [o for o in self.ops[e] if not o.dma]
            if comp:
                comp[-1].awaited = True
            m = self.mcount[e]
            pending = []
            for o in comp:
                pending.append(o)
                if o.awaited:
                    m += 1
                    for p in pending:
                        p.mile = m
                    pending = []
            self.mcount[e] = m
        barrier = {}
        if not self.first_flush:
            for e in self.ENGS:
                if self.bar_m[e] > 0:
                    barrier[("p", e)] = self.bar_m[e]
            for s, n in self.bar_s.items():
                if n > 0:
                    barrier[("s", s)] = 16 * n
        streams = self.streams
        block = self.es.enter_context(nc.Block())

        def run(ename, eng):
            waited = self.waited[ename]

            def do_wait(key, val):
                if waited.get(key, 0) >= val:
                    return
                waited[key] = val
                sem = ssem[key[1]] if key[0] == "s" else prog[key[1]]
                eng.wait_ge(sem, val)

            for key, val in barrier.items():
                do_wait(key, val)
            for o in self.ops[ename]:
                need = {}
                for d in o.deps:
                    if d.dma:
                        key = ("s", d.stream)
                        val = 16 * d.sidx
                    else:
                        key = ("p", d.eng)
                        val = d.mile
                    if val > need.get(key, 0):
                        need[key] = val
                for key, val in need.items():
                    do_wait(key, val)
                ins = o.fn(eng)
                if o.dma:
                    ins.then_inc(ssem[o.stream], 16)
                elif o.awaited:
                    ins.then_inc(prog[ename], 1)
            if ename == "sync":
                for s in final_streams:
                    eng.wait_ge(ssem[s], 16 * len(streams[s]))

        @block.tensor
        def _(e):
            run("tensor", e)

        @block.vector
        def _(e):
            run("vector", e)

        @block.scalar
        def _(e):
            run("scalar", e)

        @block.gpsimd
        def _(e):
            run("gpsimd", e)

        @block.sync
        def _(e):
            run("sync", e)

        self.bar_m = dict(self.mcount)
        self.bar_s = {s: len(v) for s, v in self.streams.items()}
        self.first_flush = False
        self.ops = {e: [] for e in self.ENGS}


def build_nc(NJ, debug=False):
    NXB = NCORE * NJ
    NKB = NXB + 1
    T_ALL = N_META + NXB * 128
    TO = NJ * 128
    NMT = NJ // 4
    assert NJ % 4 == 0

    nc = bass.Bass("TRN2", target_bir_lowering=False)
    es = ExitStack()
    es.enter_context(nc.allow_low_precision("bf16 matmul operands by design; fp32 accumulation"))
    S = Sched(nc, es)

    def ext(name, shape, dt=F32, kind="ExternalInput"):
        return Tk(nc.dram_tensor(name, list(shape), dt, kind=kind).ap(), name)

    xT_all = ext("xT_all", [D, T_ALL])
    xT_own = ext("xT_own", [D, TO])
    x_own = ext("x_own", [TO, D])
    w_in = ext("w_in", [D, 7176])
    w_out = ext("w_out", [D, D])
    w_q = ext("w_q", [D, D])
    skT = ext("skT", [128, 16, 128])
    u_t = ext("peer_u", [N_EXP, D])
    v_t = ext("peer_v", [N_EXP, D])
    vecs = ext("vecs", [128, 64])
    g2row = ext("g2row", [128, D])
    consts = ext("consts", [128, 4, 128])
    maskadd = ext("maskadd", [128, 2, 8, 128])
    onehot_c = ext("onehot_c", [1, 8])
    out_own = ext("out_own", [TO, D], kind="ExternalOutput")

    kT_s = S.dram("kT_s", [16, 128, T_ALL], BF16)
    v_s = S.dram("v_s", [16, 128, NKB, 128], BF16)
    y0_s = S.dram("y0_s", [8, T_ALL], F32)
    qT_s = S.dram("qT_s", [16, 128, TO], BF16)
    gT_s = S.dram("gT_s", [8, 128, TO], BF16)
    oT_s = S.dram("oT_s", [16, 128, TO], F32)
    uv_b = S.dram("uv_b", [N_EXP, 2 * D], BF16)
    wo_fm = S.dram("wo_fm", [16, 128, KC, 128], BF16)
    wo_tm = S.dram("wo_tm", [8, 128, KC, 256], BF16)
    wq_fm = S.dram("wq_fm", [16, 128, KC, 128], BF16)

    ps = [S.psum("ps%d" % i) for i in range(8)]

    V, A, P, G, Q = "vector", "scalar", "tensor", "gpsimd", "sync"

    def act(out, in_, func, reads, writes, bias=None, scale=None, accum=None, eng=A):
        kw = {}
        if bias is not None:
            kw["bias"] = bias
        if scale is not None:
            kw["scale"] = scale
        if accum is not None:
            kw["accum_out"] = accum
        return S.op(eng, lambda e: e.activation(out=out, in_=in_, func=func, **kw), reads, writes)

    def tt(out, in0, in1, op, reads, writes, eng=V):
        return S.op(eng, lambda e: e.tensor_tensor(out=out, in0=in0, in1=in1, op=op), reads, writes)

    def ts(out, in0, s1, op0, reads, writes, s2=None, op1=None, eng=V):
        if op1 is None:
            return S.op(eng, lambda e: e.tensor_scalar(out=out, in0=in0, scalar1=s1, scalar2=None, op0=op0), reads, writes)
        return S.op(eng, lambda e: e.tensor_scalar(out=out, in0=in0, scalar1=s1, scalar2=s2, op0=op0, op1=op1), reads, writes)

    def stt(out, in0, scalar, in1, op0, op1, reads, writes, accum=None):
        if accum is None:
            return S.op(V, lambda e: e.scalar_tensor_tensor(out=out, in0=in0, scalar=scalar, in1=in1, op0=op0, op1=op1), reads, writes)
        return S.op(V, lambda e: e.scalar_tensor_tensor(out=out, in0=in0, scalar=scalar, in1=in1, op0=op0, op1=op1, accum_out=accum), reads, writes)

    def cp(out, in_, reads, writes, eng=V):
        return S.op(eng, lambda e: e.tensor_copy(out=out, in_=in_), reads, writes)

    def mm(out, lhsT, rhs, start, stop, reads, writes):
        return S.op(P, lambda e: e.matmul(out, lhsT, rhs, start=start, stop=stop), reads, writes)

    def memset(ap, val, writes, eng=V):
        return S.op(eng, lambda e: e.memset(ap, val), (), writes)

    def rstd_from(out_t, out_ap, ps_t, ps_ap, inv_n, tmp_t, tmp_ap):
        act(tmp_ap, ps_ap, AF.Ln, [ps_t], [tmp_t], bias=EPS, scale=inv_n)
        act(out_ap, tmp_ap, AF.Exp, [tmp_t], [out_t], scale=-0.5)

    c32 = S.tile("c32", [128, 4, 128], F32)
    S.dma(Q, c32[:], consts[:], [consts], [c32])
    negtri = S.tile("negtri", [128, 128], BF16)
    onesb = S.tile("onesb", [128, 128], BF16)
    identb = S.tile("identb", [128, 128], BF16)
    zerob = S.tile("zerob", [128, 512], BF16)
    cp(negtri[:], c32[:, 0, :], [c32], [negtri])
    cp(onesb[:], c32[:, 1, :], [c32], [onesb])
    cp(identb[:], c32[:, 2, :], [c32], [identb])
    memset(zerob[:], 0.0, [zerob])
    vec = S.tile("vec", [128, 64], F32)
    S.dma(Q, vec[:], vecs[:], [vecs], [vec])
    vec2 = S.tile("vec2", [128, 2], F32)
    ts(vec2[:, 0:1], vec[:, 32:33], SCALE, ALU.mult, [vec], [vec2])
    ts(vec2[:, 1:2], vec[:, 50:51], -1.0, ALU.mult, [vec], [vec2])
    maskb = S.tile("maskb", [128, 2, 8, 128], BF16)
    ohc = S.tile("ohc", [128, 8], F32)
    S.dma(Q, ohc[64:65, :], onehot_c[:], [onehot_c], [ohc])
    ncf_cols = S.tile("ncf_cols", [128, 8, NKB], F32)
    cmid = S.tile("cmid", [128, 8, NJ], F32)
    cfull = S.tile("cfull", [128, 8, NJ], F32)

    NHT = 4 * (N_EXP // 128)
    conv_state = [0, 0]

    def conv_load(cst32):
        n_ = conv_state[0]
        if n_ >= NHT:
            return
        conv_state[0] += 1
        src = u_t if n_ < NHT // 2 else v_t
        rt = (n_ % (NHT // 2)) // 2
        hf = n_ % 2
        a = cst32[n_ % len(cst32)]
        S.dma(Q, a[:], src[rt * 128:(rt + 1) * 128, hf * 1024:(hf + 1) * 1024], [src], [a], stream=a.name)

    g2_tile = [None]

    def conv_finish(cst32, cst16, on_act=False):
        n_ = conv_state[1]
        if n_ >= conv_state[0]:
            return
        conv_state[1] += 1
        coff = 0 if n_ < NHT // 2 else D
        rt = (n_ % (NHT // 2)) // 2
        hf = n_ % 2
        a, b = cst32[n_ % len(cst32)], cst16[n_ % len(cst16)]
        if on_act:
            act(b[:], a[:], AF.Copy, [a], [b])
        else:
            cp(b[:], a[:], [a], [b])
        S.dma(Q, uv_b[rt * 128:(rt + 1) * 128, coff + hf * 1024:coff + (hf + 1) * 1024], b[:], [b], [uv_b], stream=b.name)

    def conv_steps(k, cst32, cst16, lookahead=3, on_act=False):
        for _ in range(k):
            while conv_state[0] < min(NHT, conv_state[1] + lookahead):
                conv_load(cst32)
            conv_finish(cst32, cst16, on_act)

    def conv_drain(cst32, cst16, on_act=False):
        while conv_state[1] < conv_state[0]:
            conv_finish(cst32, cst16, on_act)

    def kcol(slot):
        if slot == 0:
            return 0, N_META
        return N_META + 128 * (slot - 1), 128

    with ExitStack() as es0:
        S.es = es0
        m32 = S.tile("m32", [128, 2, 8, 128], F32)
        S.dma(Q, m32[:], maskadd[:], [maskadd], [m32])
        cp(maskb[:], m32[:], [m32], [maskb])
        S.flush()

    with ExitStack() as es1:
        S.es = es1
        GK = 256
        wk = S.tile("wk", [128, KC, 2048], BF16)
        wf = S.tile("wf", [128, KC, 8], BF16)
        wld = [S.tile("wldk%d" % i, [128, 1024], F32) for i in range(2)]
        n = 0
        for (dst, coff, scol) in ((wk, 0, 1024), (wk, 1024, 4096)):
            for kc in range(KC):
                a = wld[n % 2]
                S.dma(Q, a[:], w_in[kc * 128:(kc + 1) * 128, scol:scol + 1024], [w_in], [a])
                ts(dst[:, kc, coff:coff + 1024], a[:], vec[:, kc:kc + 1], ALU.mult, [a, vec], [dst],
                   eng=(V if n % 2 else G))
                n += 1
        wfl = S.tile("wfl", [128, KC, 8], F32)
        S.dma(Q, wfl[:], None, [w_in], [wfl],
              fn=lambda e: e.dma_start(out=wfl[:], in_=w_in[:, 7168:7176].rearrange("(kc p) c -> p kc c", p=128)))
        for kc in range(KC):
            ts(wf[:, kc, :], wfl[:, kc, :], vec[:, kc:kc + 1], ALU.mult, [wfl, vec], [wf])
        xs = [S.tile("xsk%d" % i, [128, KC, GK], F32) for i in range(2)]
        xbk2 = [S.tile("xbk%d" % i, [128, KC, GK], BF16) for i in range(2)]
        sqx2 = [S.tile("sqx%d" % i, [128, KC, GK], BF16) for i in range(2)]
        lnt = S.tile("lnt", [128, GK], F32)
        lnt2 = [S.tile("lnt2_%d" % i, [128, GK], F32) for i in range(2)]
        rsk = S.tile("rsk", [128, GK], F32)
        kst2 = [S.tile("kstk%d" % i, [128, 16, GK], BF16) for i in range(2)]
        kf = [S.tile("kf%d" % i, [128, GK], F32) for i in range(2)]
        sqk = [S.tile("sqk%d" % i, [128, GK], BF16) for i in range(2)]
        rk = [S.tile("rk%d" % i, [128, GK], F32) for i in range(2)]
        yst = [S.tile("yst%d" % i, [8, GK], F32) for i in range(2)]
        xTv = xT_all[:].rearrange("(kc p) t -> p kc t", p=128)
        groups = [(0, N_META)] + [(N_META + GK * i, GK) for i in range(NXB * 128 // GK)]
        def k_prologue(gi):
            t0, Gn = groups[gi]
            x_ = xs[gi % 2]
            S.dma(Q, x_[:, :, 0:Gn], xTv[:, :, t0:t0 + Gn], [xT_all], [x_], stream="xsk%d" % (gi % 2))
            cp(xbk2[gi % 2][:, :, 0:Gn], x_[:, :, 0:Gn], [x_], [xbk2[gi % 2]], eng=G)
            act(sqx2[gi % 2][:, :, 0:Gn], x_[:, :, 0:Gn], AF.Square, [x_], [sqx2[gi % 2]])

        k_prologue(0)
        for gi, (t0, Gn) in enumerate(groups):
            xbk, sqx, kst = xbk2[gi % 2], sqx2[gi % 2], kst2[gi % 2]
            for kc in range(KC):
                mm(ps[0][:, 0:Gn], onesb[:], sqx[:, kc, 0:Gn], kc == 0, kc == KC - 1, [onesb, sqx], [ps[0]])
            rstd_from(rsk, rsk[:, 0:Gn], ps[0], ps[0][:, 0:Gn], 1.0 / D, lnt, lnt[:, 0:Gn])
            for kc in range(KC):
                mm(ps[1][0:8, 0:Gn], wf[:, kc, :], xbk[:, kc, 0:Gn], kc == 0, kc == KC - 1, [wf, xbk], [ps[1]])
            ys = yst[gi % 2]
            tt(ys[:, 0:Gn], ps[1][0:8, 0:Gn], rsk[0:8, 0:Gn], ALU.mult, [ps[1], rsk], [ys])
            S.dma(A, y0_s[:, t0:t0 + Gn], ys[:, 0:Gn], [ys], [y0_s], stream="yst%d" % (gi % 2))
            if gi + 1 < len(groups):
                k_prologue(gi + 1)

            def fox_norm(h):
                kf_, sqk_, rk_ = kf[h % 2], sqk[h % 2], rk[h % 2]
                pn = ps[6 + h % 2]
                mm(pn[:, 0:Gn], onesb[:], sqk_[:, 0:Gn], True, True, [onesb, sqk_], [pn])
                rstd_from(rk_, rk_[:, 0:Gn], pn, pn[:, 0:Gn], 1.0 / 128, lnt2[h % 2], lnt2[h % 2][:, 0:Gn])
                stt(kst[:, h, 0:Gn], kf_[:, 0:Gn], vec[:, 33:34], rk_[:, 0:Gn], ALU.mult, ALU.mult, [kf_, vec, rk_], [kst])

            order = [8, 9, 0, 10, 1, 11, 2, 12, 3, 13, 4, 14, 5, 15, 6, 7]
            pend = []
            for oi, h in enumerate(order):
                pk = ps[2 + oi % 4]
                for kc in range(KC):
                    mm(pk[:, 0:Gn], wk[:, kc, h * 128:(h + 1) * 128], xbk[:, kc, 0:Gn], kc == 0, kc == KC - 1, [wk, xbk], [pk])
                if h < 8:
                    tt(kst[:, h, 0:Gn], pk[:, 0:Gn], rsk[:, 0:Gn], ALU.mult, [pk, rsk], [kst])
                else:
                    kf_, sqk_ = kf[h % 2], sqk[h % 2]
                    tt(kf_[:, 0:Gn], pk[:, 0:Gn], rsk[:, 0:Gn], ALU.mult, [pk, rsk], [kf_])
                    act(sqk_[:, 0:Gn], kf_[:, 0:Gn], AF.Square, [kf_], [sqk_])
                    pend.append((oi, h))
                while pend and pend[0][0] <= oi - 1:
                    fox_norm(pend.pop(0)[1])
            while pend:
                fox_norm(pend.pop(0)[1])
            S.dma(A, kT_s[:, :, t0:t0 + Gn].rearrange("h d t -> d h t"), kst[:, :, 0:Gn], [kst], [kT_s], stream="kstk%d" % (gi % 2))
        S.flush()

    with ExitStack() as es1v:
        S.es = es1v
        wv = S.tile("wv", [128, KC, 2048], BF16)
        stg32 = [S.tile("stg32_%d" % i, [128, 1024], F32) for i in range(2)]
        wld = stg32
        n = 0
        for (dst, coff, scol) in ((wv, 0, 2048), (wv, 1024, 5120)):
            for kc in range(KC):
                a = wld[n % 2]
                S.dma(Q, a[:], w_in[kc * 128:(kc + 1) * 128, scol:scol + 1024], [w_in], [a], stream="stg32_%d" % (n % 2))
                ts(dst[:, kc, coff:coff + 1024], a[:], vec[:, kc:kc + 1], ALU.mult, [a, vec], [dst],
                   eng=(V if n % 2 else G))
                n += 1
        xs = [S.tile("xs%d" % i, [128, KC, 128], F32) for i in range(3)]
        xb = [S.tile("xb%d" % i, [128, KC, 128], BF16) for i in range(2)]
        sq = [S.tile("sq%d" % i, [128, KC, 128], BF16) for i in range(2)]
        lntv = [S.tile("lntv%d" % i, [128, 1], F32) for i in range(2)]
        rcol = [S.tile("rcol%d" % i, [128, 1], F32) for i in range(2)]
        vst = [S.tile("vst%d" % i, [128, 2048], BF16) for i in range(2)]
        cstB32 = [S.tile("cstB32_%d" % i, [128, 1024], F32) for i in range(4)]
        cstB16 = [S.tile("cstB16_%d" % i, [128, 1024], BF16) for i in range(2)]

        wjobs = [(src, dst, gcol, kc, hf) for (src, dst, gcol) in ((w_out, wo_fm, None), (w_q, wq_fm, 16))
                 for kc in range(KC) for hf in range(2)]
        wstate = [0, 0]

        def w_load():
            n_ = wstate[0]
            if n_ >= len(wjobs):
                return
            wstate[0] += 1
            src, dst, gcol, kc, hf = wjobs[n_]
            a = cstB32[n_ % 4]
            S.dma(Q, a[:], src[kc * 128:(kc + 1) * 128, hf * 1024:(hf + 1) * 1024], [src], [a], stream=a.name)

        def w_finish():
            n_ = wstate[1]
            if n_ >= wstate[0]:
                return
            wstate[1] += 1
            src, dst, gcol, kc, hf = wjobs[n_]
            a, b = cstB32[n_ % 4], cstB16[n_ % 2]
            if gcol is None:
                cp(b[:], a[:], [a], [b])
            else:
                ts(b[:], a[:], vec[:, gcol + kc:gcol + kc + 1], ALU.mult, [a, vec], [b])
            S.dma(Q, dst[hf * 8:(hf + 1) * 8, :, kc, :].rearrange("n p c -> p n c"),
                  b[:].rearrange("p (n c) -> p n c", c=128), [b], [dst], stream=b.name)
            if gcol is None:
                S.dma(Q, wo_tm[hf * 4:(hf + 1) * 4, :, kc, :].rearrange("g p c -> p g c"),
                      b[:].rearrange("p (g c) -> p g c", c=256), [b], [wo_tm], stream=b.name + "t")

        def w_step():
            while wstate[0] < min(len(wjobs), wstate[1] + 3):
                w_load()
            w_finish()

        def v_prologue(slot):
            t0, Gn = kcol(slot)
            x_ = xs[slot % 3]
            S.dma(Q, x_[:, :, 0:Gn], xTv[:, :, t0:t0 + Gn], [xT_all], [x_], stream="xs%d" % (slot % 3))
            cp(xb[slot % 2][:, :, 0:Gn], x_[:, :, 0:Gn], [x_], [xb[slot % 2]], eng=G)
            act(sq[slot % 2][:, :, 0:Gn], x_[:, :, 0:Gn], AF.Square, [x_], [sq[slot % 2]])

        v_prologue(0)
        for slot in range(NKB):
            t0, Gn = kcol(slot)
            b2 = slot % 2
            xb_, sq_, rc_, vs_ = xb[b2], sq[b2], rcol[b2], vst[b2]
            for kc in range(KC):
                mm(ps[b2][0:Gn, 0:1], sq_[:, kc, 0:Gn], onesb[:, 0:1], kc == 0, kc == KC - 1, [onesb, sq_], [ps[b2]])
            rstd_from(rc_, rc_[0:Gn, :], ps[b2], ps[b2][0:Gn, 0:1], 1.0 / D, lntv[b2], lntv[b2][0:Gn, 0:1])
            if slot + 1 < NKB:
                v_prologue(slot + 1)
            if slot % 2 == 0:
                w_step()
            for cg in range(4):
                pv = ps[2 + (slot * 4 + cg) % 6]
                for kc in range(KC):
                    mm(pv[0:Gn, :], xb_[:, kc, 0:Gn], wv[:, kc, cg * 512:(cg + 1) * 512], kc == 0, kc == KC - 1, [xb_, wv], [pv])
                act(vs_[0:Gn, cg * 512:(cg + 1) * 512], pv[0:Gn, :], AF.Copy, [pv, rc_], [vs_], scale=rc_[0:Gn, 0:1])
            S.dma(A, v_s[:, 0:Gn, slot, :].rearrange("h t d -> t h d"), vs_[0:Gn, :].rearrange("t (h d) -> t h d", h=16),
                  [vs_], [v_s], stream="vst%d" % b2)
        while wstate[1] < len(wjobs):
            w_step()
        S.flush()

    with ExitStack() as es2:
        S.es = es2
        wqg = S.tile("wqg", [128, KC, 3072], BF16)
        wld = [S.tile("wld2_%d" % i, [128, 1024], F32) for i in range(2)]
        n = 0
        for (coff, scol) in ((0, 0), (1024, 3072), (2048, 6144)):
            for kc in range(KC):
                a = wld[n % 2]
                S.dma(Q, a[:], w_in[kc * 128:(kc + 1) * 128, scol:scol + 1024], [w_in], [a])
                ts(wqg[:, kc, coff:coff + 1024], a[:], vec[:, kc:kc + 1], ALU.mult, [a, vec], [wqg],
                   eng=(V if n % 2 else G))
                n += 1
        x2 = S.tile("x2", [128, KC, 512], F32)
        xb2 = S.tile("xb2", [128, KC, 512], BF16)
        sq2 = S.tile("sq2", [128, KC, 512], BF16)
        ln2 = S.tile("ln2", [128, 512], F32)
        rs2 = S.tile("rs2", [128, 512], F32)
        qf = [S.tile("qf%d" % i, [128, 512], F32) for i in range(2)]
        qsq = [S.tile("qsq%d" % i, [128, 512], BF16) for i in range(2)]
        rq = [S.tile("rq%d" % i, [128, 512], F32) for i in range(2)]
        qst = [S.tile("qst%d" % i, [128, 512], BF16) for i in range(3)]
        xTo = xT_own[:].rearrange("(kc p) t -> p kc t", p=128)
        for gi in range(TO // 512):
            c0 = gi * 512
            S.dma(Q, x2[:], xTo[:, :, c0:c0 + 512], [xT_own], [x2])
            cp(xb2[:], x2[:], [x2], [xb2], eng=G)
            act(sq2[:], x2[:], AF.Square, [x2], [sq2])
            for kc in range(KC):
                mm(ps[0][:], onesb[:], sq2[:, kc, :], kc == 0, kc == KC - 1, [onesb, sq2], [ps[0]])
            rstd_from(rs2, rs2[:], ps[0], ps[0][:], 1.0 / D, ln2, ln2[:])
            for cc in range(24):
                pq = ps[2 + cc % 4]
                for kc in range(KC):
                    mm(pq[:], wqg[:, kc, cc * 128:(cc + 1) * 128], xb2[:, kc, :], kc == 0, kc == KC - 1, [wqg, xb2], [pq])
                o_ = qst[cc % 3]
                if cc < 8:
                    stt(o_[:], pq[:], SCALE, rs2[:], ALU.mult, ALU.mult, [pq, rs2], [o_])
                    S.dma(A, qT_s[cc, :, c0:c0 + 512], o_[:], [o_], [qT_s], stream="qst%d" % (cc % 3))
                elif cc < 16:
                    f_, s_, r_ = qf[cc % 2], qsq[cc % 2], rq[cc % 2]
                    tt(f_[:], pq[:], rs2[:], ALU.mult, [pq, rs2], [f_])
                    act(s_[:], f_[:], AF.Square, [f_], [s_])
                    mm(ps[1][:], onesb[:], s_[:], True, True, [onesb, s_], [ps[1]])
                    rstd_from(r_, r_[:], ps[1], ps[1][:], 1.0 / 128, ln2, ln2[:])
                    stt(o_[:], f_[:], vec2[:, 0:1], r_[:], ALU.mult, ALU.mult, [f_, vec2, r_], [o_])
                    S.dma(A, qT_s[cc, :, c0:c0 + 512], o_[:], [o_], [qT_s], stream="qst%d" % (cc % 3))
                else:
                    f_ = qf[cc % 2]
                    tt(f_[:], pq[:], rs2[:], ALU.mult, [pq, rs2], [f_])
                    act(f_[:], f_[:], AF.Exp, [f_], [f_], scale=-1.0)
                    ts(f_[:], f_[:], 1.0, ALU.add, [f_], [f_])
                    S.op(V, lambda e, o=o_, f=f_: e.reciprocal(out=o[:], in_=f[:]), [f_], [o_])
                    S.dma(A, gT_s[cc - 16, :, c0:c0 + 512], o_[:], [o_], [gT_s], stream="qst%d" % (cc % 3))
        S.flush()

    with ExitStack() as es2b:
        S.es = es2b
        yl = S.tile("yl", [8, T_ALL], F32)
        ncf = S.tile("ncf", [8, T_ALL], F32)
        one8 = S.tile("one8", [8, 1], F32)
        id32 = S.tile("id32", [8, 8], F32)
        memset(one8[:], 1.0, [one8])
        cp(id32[:], c32[0:8, 2, 0:8], [c32], [id32])
        S.dma(Q, yl[:], y0_s[:], [y0_s], [yl])
        act(yl[:], yl[:], AF.Exp, [yl, vec2], [yl], bias=vec2[0:8, 1:2], scale=-1.0)
        act(yl[:], yl[:], AF.Ln, [yl], [yl], bias=1.0)
        CH = 2048
        pos = 0
        while pos < T_ALL:
            n_ = min(CH, T_ALL - pos)
            init = 0.0 if pos == 0 else ncf[:, pos - 1:pos]
            S.op(V, lambda e, pos=pos, n_=n_, init=init: e.tensor_tensor_scan(
                out=ncf[:, pos:pos + n_], data0=one8[:, 0:1].to_broadcast([8, n_]), data1=yl[:, pos:pos + n_],
                initial=init, op0=ALU.mult, op1=ALU.add), [yl, one8, ncf], [ncf])
            pos += n_
        for s0 in range(0, NKB, 64):
            ns = min(64, NKB - s0)
            pt = ps[(s0 // 64) % 2]
            for si in range(ns):
                t0, Gn = kcol(s0 + si)
                S.op(P, lambda e, pt=pt, si=si, t0=t0, Gn=Gn: e.transpose(pt[0:Gn, si * 8:si * 8 + 8], ncf[0:8, t0:t0 + Gn], id32[:]),
                     [ncf, id32], [pt])
            if s0 == 0:
                cp(ncf_cols[0:16, :, 0:1].rearrange("p h s -> p s h"), pt[0:16, 0:8].rearrange("p (s h) -> p s h", h=8),
                   [pt], [ncf_cols])
                cp(ncf_cols[:, :, 1:ns].rearrange("p h s -> p s h"), pt[:, 8:ns * 8].rearrange("p (s h) -> p s h", h=8),
                   [pt], [ncf_cols])
            else:
                cp(ncf_cols[:, :, s0:s0 + ns].rearrange("p h s -> p s h"), pt[:, 0:ns * 8].rearrange("p (s h) -> p s h", h=8),
                   [pt], [ncf_cols])
        tmpc = S.tile("tmpc", [128, 8, NJ, 8], F32)
        tt(tmpc[64:65], ncf_cols[64:65, :, 1:1 + NXB].rearrange("p h (j c) -> p h j c", c=8),
           ohc[64:65, :].unsqueeze(1).unsqueeze(1).to_broadcast([1, 8, NJ, 8]), ALU.mult, [ncf_cols, ohc], [tmpc])
        S.op(V, lambda e: e.tensor_reduce(out=cmid[64:65], in_=tmpc[64:65], axis=AX.X, op=ALU.add), [tmpc], [cmid])
        mm(ps[2][:, 0:8 * NJ], c32[64:65, 1, :], cmid[64:65].rearrange("p h j -> p (h j)"), True, True, [c32, cmid], [ps[2]])
        cp(cfull[:].rearrange("p h j -> p (h j)"), ps[2][:, 0:8 * NJ], [ps[2]], [cfull])
        S.flush()

    with ExitStack() as es3:
        S.es = es3
        KT = [S.tile("KT%d" % i, [128, T_ALL], BF16) for i in range(2)]
        VV = [S.tile("VV%d" % i, [128, NKB, 128], BF16) for i in range(2)]
        QT = [S.tile("QT%d" % i, [128, TO], BF16) for i in range(2)]
        crow = [S.tile("crow%d" % i, [128, NJ, 128], BF16) for i in range(2)]
        Et = [S.tile("Et%d" % i, [128, 512], F32) for i in range(2)]
        Lt = [S.tile("Lt%d" % i, [128, 512], BF16) for i in range(2)]
        Tt = [S.tile("Tt%d" % i, [128, 512], F32) for i in range(2)]
        Wt = [S.tile("Wt%d" % i, [128, 512], BF16) for i in range(3)]
        carry = [S.tile("carry%d" % i, [128, 512], F32) for i in range(2)]
        osb = [S.tile("osb%d" % i, [128, 512], F32) for i in range(2)]
        rden = S.tile("rden", [128, 512], F32)
        dacc = [S.tile("dacc%d" % i, [128, 512], F32) for i in range(4)]
        dhi = S.tile("dhi", [128, 512], BF16)
        dlo = S.tile("dlo", [128, 512], BF16)
        dtmp = S.tile("dtmp", [128, 512], F32)
        B1 = [ps[0], ps[1]]
        B2 = [ps[2], ps[3]]
        B3 = [ps[4], ps[5]]
        PO, PD = ps[6], ps[7]

        cstC32 = [S.tile("cstC32_%d" % i, [128, 1024], F32) for i in range(2)]
        cstC16 = [S.tile("cstC16_%d" % i, [128, 1024], BF16) for i in range(2)]
        fox_ctr = [0]

        def steps_for(m):
            st = []
            for g in range(4 * m + 3, -1, -1):
                r = max(0, g - 4 * m)
                for i in range(7, -1, -1):
                    st.append(dict(slot=1 + 8 * g + i, c0=128 * r, mask=(i if g >= 4 * m else None)))
            st.append(dict(slot=0, c0=0, mask=None))
            return st

        nmt = 0
        for h in range(16):
            hb = h % 2
            KT_, VV_, QT_ = KT[hb], VV[hb], QT[hb]
            is_sb = h < 8
            S.dma(Q, KT_[:], kT_s[h], [kT_s], [KT_])
            S.dma(Q, VV_[:], v_s[h], [v_s], [VV_])
            S.dma(Q, QT_[:], qT_s[h], [qT_s], [QT_])
            cr_ = crow[hb]
            if not is_sb:
                hf = h - 8
                ts(cr_[:], cfull[:, hf, :].unsqueeze(2).to_broadcast([128, NJ, 128]), -1.0 / 128, ALU.mult, [cfull], [cr_])
            for m in range(NMT):
                steps = steps_for(m)
                ns = len(steps)
                car = carry[nmt % 2]
                ob = osb[nmt % 2]
                nmt += 1
                q0 = 512 * m
                if is_sb:
                    memset(car[:], 0.0, [car], eng=G)
                mm(PO[:], zerob[:, 0:128], zerob[:], True, False, [zerob], [PO])
                da = dacc[nmt % 2]
                da2 = dacc[2 + nmt % 2]
                if not is_sb:
                    memset(da[:], 0.0, [da], eng=G)
                    memset(da2[:], 0.0, [da2], eng=G)

                def pe1(s):
                    d = steps[s]
                    t0, kp = kcol(d["slot"])
                    c0 = d["c0"]
                    b1 = B1[s % 2]
                    msk = d["mask"]
                    kl = KT_[:, t0:t0 + kp]
                    qr = QT_[:, q0 + c0:q0 + 512]
                    if is_sb:
                        b2_ = B2[s % 2]
                        for bb in (b1, b2_):
                            last = (msk is None) and (bb is b1)
                            mm(bb[0:kp, c0:512], kl, qr, True, last, [KT_, QT_], [bb])
                            if msk is not None:
                                mm(bb[0:kp, c0:c0 + 128], identb[:], maskb[:, 0, msk, :], False, bb is b1, [identb, maskb], [bb])
                    else:
                        mm(b1[0:kp, c0:512], kl, qr, True, False, [KT_, QT_], [b1])
                        mm(b1[0:kp, c0:512], onesb[:, 0:kp],
                           cr_[:, 4 * m:4 * m + 4, :].rearrange("p j t -> p (j t)")[:, c0:512],
                           False, msk is None, [onesb, cr_], [b1])
                        if msk is not None:
                            mm(b1[0:kp, c0:c0 + 128], identb[:], maskb[:, 1, msk, :], False, True, [identb, maskb], [b1])

                def act1(s):
                    d = steps[s]
                    t0, kp = kcol(d["slot"])
                    c0 = d["c0"]
                    b1 = B1[s % 2]
                    if is_sb:
                        l_ = Lt[s % 2]
                        act(b1[0:kp, c0:512], b1[0:kp, c0:512], AF.Exp, [b1], [b1])
                        act(l_[0:kp, c0:512], b1[0:kp, c0:512], AF.Ln, [b1], [l_], bias=1.0)
                    else:
                        w_ = Wt[s % 3]
                        act(w_[0:kp, c0:512], b1[0:kp, c0:512], AF.Exp, [b1, ncf_cols], [w_],
                            bias=ncf_cols[0:kp, h - 8, d["slot"]:d["slot"] + 1])

                def pe2(s):
                    d = steps[s]
                    t0, kp = kcol(d["slot"])
                    c0 = d["c0"]
                    l_ = Lt[s % 2]
                    mm(B2[s % 2][0:kp, c0:512], negtri[0:kp, 0:kp], l_[0:kp, c0:512], False, True, [negtri, l_], [B2[s % 2]])
                    mm(B3[s % 2][:, c0:512], onesb[0:kp, :], l_[0:kp, c0:512], True, True, [onesb, l_], [B3[s % 2]])

                def dve1(s):
                    d = steps[s]
                    t0, kp = kcol(d["slot"])
                    c0 = d["c0"]
                    tt(B2[s % 2][0:kp, c0:512], B2[s % 2][0:kp, c0:512], car[0:kp, c0:512], ALU.subtract, [B2[s % 2], car], [B2[s % 2]])
                    tt(car[:, c0:512], B3[s % 2][:, c0:512], car[:, c0:512], ALU.add, [B3[s % 2], car], [car])

                def act3(s):
                    d = steps[s]
                    t0, kp = kcol(d["slot"])
                    c0 = d["c0"]
                    act(Wt[s % 3][0:kp, c0:512], B2[s % 2][0:kp, c0:512], AF.Exp, [B2[s % 2]], [Wt[s % 3]])

                def pe3(s):
                    d = steps[s]
                    t0, kp = kcol(d["slot"])
                    c0 = d["c0"]
                    w_ = Wt[s % 3]
                    last = s == ns - 1
                    mm(PO[:, c0:512], VV_[0:kp, d["slot"], :], w_[0:kp, c0:512], False, last, [VV_, w_], [PO])
                    if not is_sb:
                        dd = da if s % 2 == 0 else da2
                        tt(dd[0:kp, c0:512], dd[0:kp, c0:512], w_[0:kp, c0:512], ALU.add, [dd, w_], [dd])

                if is_sb:
                    for t in range(ns + 2):
                        fox_ctr[0] += 1
                        if fox_ctr[0] % 5 == 0:
                            conv_steps(1, cstC32, cstC16, lookahead=2, on_act=False)
                        if t < ns:
                            pe1(t)
                            act1(t)
                        if 0 <= t - 1 < ns:
                            pe2(t - 1)
                            dve1(t - 1)
                            act3(t - 1)
                        if 0 <= t - 2 < ns:
                            pe3(t - 2)
                    act(ob[:], PO[:], AF.Copy, [PO], [ob])
                else:
                    for t in range(ns + 1):
                        fox_ctr[0] += 1
                        if fox_ctr[0] % 5 == 0:
                            conv_steps(1, cstC32, cstC16, lookahead=2, on_act=(fox_ctr[0] % 10 == 0))
                        if t < ns:
                            pe1(t)
                            act1(t)
                        if 0 <= t - 1 < ns:
                            pe3(t - 1)
                    tt(da[:], da[:], da2[:], ALU.add, [da, da2], [da])
                    cp(dhi[:], da[:], [da], [dhi])
                    tt(dtmp[:], da[:], dhi[:], ALU.subtract, [da, dhi], [dtmp])
                    cp(dlo[:], dtmp[:], [dtmp], [dlo])
                    mm(PD[:], onesb[:], dhi[:], True, False, [onesb, dhi], [PD])
                    mm(PD[:], onesb[:], dlo[:], False, True, [onesb, dlo], [PD])
                    S.op(V, lambda e: e.reciprocal(out=rden[:], in_=PD[:]), [PD], [rden])
                    tt(ob[:], PO[:], rden[:], ALU.mult, [PO, rden], [ob])
                S.dma(G, oT_s[h, :, q0:q0 + 512], ob[:], [ob], [oT_s], stream="ost%d" % ((nmt - 1) % 2))
        conv_steps(NHT, cstC32, cstC16, lookahead=2, on_act=True)
        conv_drain(cstC32, cstC16, on_act=True)
        S.flush()

    S.es = es
    skb = S.tile("skb", [128, 16, 128], BF16)
    with ExitStack() as es35:
        S.es = es35
        skl = S.tile("skl", [128, 16, 128], F32)
        S.dma(Q, skl[:], skT[:], [skT], [skl])
        cp(skb[:], skl[:], [skl], [skb])
        S.flush()
    with ExitStack() as es4:
        S.es = es4
        TC = 256
        NTB = TC // 128
        iota16 = S.tile("iota16", [128, 16], F32)
        cp(iota16[:], c32[:, 3, 0:16], [c32], [iota16])
        ol = [S.tile("ol%d" % i, [128, TC], F32) for i in range(3)]
        osq = [S.tile("osq%d" % i, [128, TC], BF16) for i in range(2)]
        gl = [S.tile("gl%d" % i, [128, TC], BF16) for i in range(2)]
        lnr = S.tile("lnr", [128, TC], F32)
        rsn = [S.tile("rsn%d" % i, [128, TC], F32) for i in range(2)]
        MT = S.tile("MT", [128, KC, TC], BF16)
        mtmp = S.tile("mtmp", [128, TC], F32)
        wos = [S.tile("wos%d" % i, [128, KC, 128], BF16) for i in range(2)]
        wot = [S.tile("wot%d" % i, [128, KC, 256], BF16) for i in range(1)] * 2
        xtl = [S.tile("xtl%d" % i, [128, TC], F32) for i in range(2)]
        hnT = S.tile("hnT", [128, KC, TC], BF16)
        H1 = S.tile("H1", [128, NTB, D], F32)
        xol = [S.tile("xol%d" % i, [128, 256], F32) for i in range(2)]
        qTt = S.tile("qTt", [128, 16, TC], BF16)
        junk = S.tile("junk", [128, D], BF16)
        ss2 = S.tile("ss2", [128, 4], F32)
        r2c = S.tile("r2c", [128, 4], F32)
        Ssc = S.tile("Ssc", [128, 16, 128], F32)
        scr = S.tile("scr", [128, 2048], F32)
        Ss2 = TkView(scr, lambda ap: ap.rearrange("p (a n) -> p a n", a=16))
        TOPV = S.tile("TOPV", [128, 16, 16], F32)
        TOPI = S.tile("TOPI", [128, 16, 16], U32)
        TOPF = S.tile("TOPF", [128, 16, 16], F32)
        CS = TkView(Ssc, lambda ap: ap.rearrange("p a n -> p (a n)").rearrange("p (h c) -> p h c", h=8))
        CS2 = TkView(scr, lambda ap: ap.rearrange("p (a n) -> p a n", a=8))
        BV = S.tile("BV", [128, 8, 16], F32)
        BJ = S.tile("BJ", [128, 8, 16], U32)
        K1 = S.tile("K1", [128, 8, 16], U32)
        K2 = S.tile("K2", [128, 8, 16], U32)
        K1f = S.tile("K1f", [128, 8, 16], F32)
        K2f = S.tile("K2f", [128, 8, 16], F32)
        OH = TkView(scr, lambda ap: ap.rearrange("p (a b c) -> p a b c", a=8, b=16))
        I0f = S.tile("I0f", [128, 8, 16], F32)
        I1f = S.tile("I1f", [128, 8, 16], F32)
        IDXf = S.tile("IDXf", [128, 128], F32)
        IDX = [S.tile("IDX%d" % i, [128, 128], I32) for i in range(2)]
        nbv = S.tile("nbv", [128, 8], F32)
        Eg = S.tile("Eg", [128, 8, 16], F32)
        Zg = S.tile("Zg", [128, 8], F32)
        Ag = S.tile("Ag", [128, 128], F32)
        Wg = S.tile("Wg", [128, 128], F32)
        NR = 7
        UR = [S.tile("UR%d" % i, [128, 2 * D], BF16) for i in range(NR)]
        Gs = S.tile("Gs", [128, 128], F32)
        dg = [S.tile("dg%d" % i, [128, 128], BF16) for i in range(4)]
        H1b = [H1, S.tile("H1b", [128, NTB, D], F32)]
        Egb = [Eg, S.tile("Egb", [128, 8, 16], F32)]
        junk2 = S.tile("junk2", [128, D], BF16)
        g2b = S.tile("g2b", [128, D], BF16)
        S.dma(Q, H1[:, 0, :], g2row[:], [g2row], [H1])
        cp(g2b[:], H1[:, 0, :], [H1], [g2b])
        HG = [S.tile("HG%d" % i, [128, D], BF16) for i in range(2)]
        nrow_c = [0]

        def chunk_work(tc):
            c0 = tc * TC
            H1_ = H1b[tc % 2]
            for grp in range(2):
                pss = ps[4 + grp]
                for hh in range(8):
                    h = grp * 8 + hh
                    o_ = ol[h % 3]
                    s_ = osq[h % 2]
                    S.dma(Q, o_[:], oT_s[h, :, c0:c0 + TC], [oT_s], [o_], stream="ol%d" % (h % 3))
                    act(s_[:], o_[:], AF.Square, [o_], [s_])
                    mm(pss[:, 0:TC], onesb[:], s_[:], hh == 0, hh == 7, [onesb, s_], [pss])
                    yield
                rstd_from(rsn[grp], rsn[grp][:], pss, pss[:, 0:TC], 1.0 / 1024, lnr, lnr[:])
                yield
            for h in range(16):
                o_ = ol[h % 3]
                S.dma(Q, o_[:], oT_s[h, :, c0:c0 + TC], [oT_s], [o_], stream="ol%d" % (h % 3))
                if h < 8:
                    stt(MT[:, h, :], o_[:], vec[:, 34 + h:35 + h], rsn[0][:], ALU.mult, ALU.mult, [o_, vec, rsn[0]], [MT])
                else:
                    g_ = gl[h % 2]
                    S.dma(Q, g_[:], gT_s[h - 8, :, c0:c0 + TC], [gT_s], [g_], stream="gl%d" % (h % 2))
                    stt(mtmp[:], o_[:], vec[:, 34 + h:35 + h], rsn[1][:], ALU.mult, ALU.mult, [o_, vec, rsn[1]], [mtmp])
                    tt(MT[:, h, :], mtmp[:], g_[:], ALU.mult, [mtmp, g_], [MT])
                yield
            for n in range(KC):
                w_ = wos[n % 2]
                S.dma(Q, w_[:], wo_fm[n], [wo_fm], [w_], stream="wos%d" % (n % 2))
                x_ = xtl[n % 2]
                S.dma(Q, x_[:], xT_own[n * 128:(n + 1) * 128, c0:c0 + TC], [xT_own], [x_], stream="xtl%d" % (n % 2))
                pp = ps[6 + n % 2]
                for kc in range(KC):
                    mm(pp[:, 0:TC], w_[:, kc, :], MT[:, kc, :], kc == 0, kc == KC - 1, [w_, MT], [pp])
                tt(hnT[:, n, :], pp[:, 0:TC], x_[:], ALU.add, [pp, x_], [hnT])
                yield
            for ng in range(8):
                w_ = wot[ng % 2]
                S.dma(Q, w_[:], wo_tm[ng], [wo_tm], [w_], stream="wot0")
                for tb in range(NTB):
                    x_ = xol[(ng * NTB + tb) % 2]
                    S.dma(Q, x_[:, 0:256], x_own[c0 + tb * 128:c0 + (tb + 1) * 128, ng * 256:(ng + 1) * 256], [x_own], [x_],
                          stream="xol%d" % ((ng * NTB + tb) % 2))
                    pp = ps[4 + (ng * NTB + tb) % 2]
                    for kc in range(KC):
                        mm(pp[:, 0:256], MT[:, kc, tb * 128:(tb + 1) * 128], w_[:, kc, :], kc == 0, kc == KC - 1, [MT, w_], [pp])
                    tt(H1_[:, tb, ng * 256:(ng + 1) * 256], pp[:, 0:256], x_[:, 0:256], ALU.add, [pp, x_], [H1_])
                yield
            for hp in range(16):
                w_ = wos[hp % 2]
                S.dma(Q, w_[:], wq_fm[hp], [wq_fm], [w_], stream="wos%d" % (hp % 2))
                pp = ps[6 + hp % 2]
                for kc in range(KC):
                    mm(pp[:, 0:TC], w_[:, kc, :], hnT[:, kc, :], kc == 0, kc == KC - 1, [w_, hnT], [pp])
                act(qTt[:, hp, :], pp[:, 0:TC], AF.Copy, [pp], [qTt])
                yield

        def prep_block(tc, tb):
            bi = tc * NTB + tb
            H1_, Eg_, idx_ = H1b[tc % 2], Egb[bi % 2], IDX[bi % 2]
            act(junk2[:], H1_[:, tb, :], AF.Square, [H1_], [junk2, ss2], accum=ss2[:, tb:tb + 1])
            rstd_from(r2c, r2c[:, tb:tb + 1], ss2, ss2[:, tb:tb + 1], 1.0 / D, lnr, lnr[:, 0:1])
            tt(HG[bi % 2][:], H1_[:, tb, :], g2b[:], ALU.mult, [H1_, g2b], [HG[bi % 2]])
            yield
            for b4 in range(4):
                pp = ps[4 + b4]
                for q4 in range(4):
                    hp = b4 * 4 + q4
                    mm(pp[:, q4 * 128:(q4 + 1) * 128], qTt[:, hp, tb * 128:(tb + 1) * 128], skb[:, hp, :], True, True,
                       [qTt, skb], [pp])
                act(Ssc[:, b4 * 4:b4 * 4 + 4, :], pp[:].rearrange("p (a n) -> p a n", a=4), AF.Copy,
                    [pp, r2c], [Ssc], scale=r2c[:, tb:tb + 1])
                yield
            for hp in range(16):
                S.op(V, lambda e, hp=hp: e.max(out=TOPV[:, hp, 0:8], in_=Ssc[:, hp, :]), [Ssc], [TOPV])
                S.op(V, lambda e, hp=hp: e.max_index(out=TOPI[:, hp, 0:8], in_max=TOPV[:, hp, 0:8], in_values=Ssc[:, hp, :]),
                     [Ssc, TOPV], [TOPI])
                S.op(V, lambda e, hp=hp: e.match_replace(out=Ss2[:, hp, :], in_to_replace=TOPV[:, hp, 0:8],
                                                         in_values=Ssc[:, hp, :], imm_value=-1e30), [Ssc, TOPV], [Ss2])
                S.op(V, lambda e, hp=hp: e.max(out=TOPV[:, hp, 8:16], in_=Ss2[:, hp, :]), [Ss2], [TOPV])
                S.op(V, lambda e, hp=hp: e.max_index(out=TOPI[:, hp, 8:16], in_max=TOPV[:, hp, 8:16], in_values=Ss2[:, hp, :]),
                     [Ss2, TOPV], [TOPI])
                yield
            cp(TOPF[:], TOPI[:], [TOPI], [TOPF])
            tv = TOPV[:].rearrange("p (h two) k -> p h two k", two=2)
            tf = TOPF[:].rearrange("p (h two) k -> p h two k", two=2)
            tt(CS[:].rearrange("p h (a b) -> p h a b", a=16),
               tv[:, :, 0, :].unsqueeze(3).to_broadcast([128, 8, 16, 16]),
               tv[:, :, 1, :].unsqueeze(2).to_broadcast([128, 8, 16, 16]), ALU.add, [TOPV], [CS])
            yield
            for hh in range(8):
                S.op(V, lambda e, hh=hh: e.max(out=BV[:, hh, 0:8], in_=CS[:, hh, :]), [CS], [BV])
                S.op(V, lambda e, hh=hh: e.max_index(out=BJ[:, hh, 0:8], in_max=BV[:, hh, 0:8], in_values=CS[:, hh, :]),
                     [CS, BV], [BJ])
                S.op(V, lambda e, hh=hh: e.match_replace(out=CS2[:, hh, :], in_to_replace=BV[:, hh, 0:8],
                                                         in_values=CS[:, hh, :], imm_value=-1e30), [CS, BV], [CS2])
                S.op(V, lambda e, hh=hh: e.max(out=BV[:, hh, 8:16], in_=CS2[:, hh, :]), [CS2], [BV])
                S.op(V, lambda e, hh=hh: e.max_index(out=BJ[:, hh, 8:16], in_max=BV[:, hh, 8:16], in_values=CS2[:, hh, :]),
                     [CS2, BV], [BJ])
                yield
            ts(K1[:], BJ[:], 4, ALU.logical_shift_right, [BJ], [K1])
            ts(K2[:], BJ[:], 15, ALU.bitwise_and, [BJ], [K2])
            cp(K1f[:], K1[:], [K1], [K1f])
            cp(K2f[:], K2[:], [K2], [K2f])
            yield
            iob = iota16[:].unsqueeze(1).unsqueeze(1).to_broadcast([128, 8, 16, 16])
            for (kf_, two, of_) in ((K1f, 0, I0f), (K2f, 1, I1f)):
                tt(OH[:], kf_[:].unsqueeze(3).to_broadcast([128, 8, 16, 16]), iob, ALU.is_equal, [kf_, iota16], [OH])
                yield
                tt(OH[:], OH[:], tf[:, :, two, :].unsqueeze(2).to_broadcast([128, 8, 16, 16]), ALU.mult, [OH, TOPF], [OH], eng=G)
                yield
                S.op(V, lambda e, of_=of_: e.tensor_reduce(out=of_[:], in_=OH[:], axis=AX.X, op=ALU.add), [OH], [of_])
                yield
            stt(IDXf[:], I0f[:].rearrange("p h k -> p (h k)"), 128.0, I1f[:].rearrange("p h k -> p (h k)"),
                ALU.mult, ALU.add, [I0f, I1f], [IDXf])
            cp(idx_[:], IDXf[:], [IDXf], [idx_])
            ts(nbv[:], BV[:, :, 0], -1.0, ALU.mult, [BV], [nbv])
            for hh in range(8):
                act(Eg_[:, hh, :], BV[:, hh, :], AF.Exp, [BV, nbv], [Eg_, Zg], bias=nbv[:, hh:hh + 1], accum=Zg[:, hh:hh + 1])
            S.op(V, lambda e: e.reciprocal(out=Zg[:], in_=Zg[:]), [Zg], [Zg])
            tt(Eg_[:], Eg_[:], Zg[:].unsqueeze(2).to_broadcast([128, 8, 16]), ALU.mult, [Eg_, Zg], [Eg_])
            yield

        def gather_block(tc, tb, filler):
            bi = tc * NTB + tb
            r0 = tc * TC + tb * 128
            H1_, Eg_, idx_ = H1b[tc % 2], Egb[bi % 2], IDX[bi % 2]
            egf = Eg_[:].rearrange("p h k -> p (h k)")
            rows = {}

            def second_half(sl):
                u_ = rows.pop(sl)
                act(Wg[:, sl:sl + 1], Gs[:, sl:sl + 1], AF.Copy, [Gs, Eg_], [Wg], scale=egf[:, sl:sl + 1])
                d_ = dg[sl % 4]
                act(d_[:], identb[:], AF.Copy, [identb, Wg], [d_], scale=Wg[:, sl:sl + 1])
                for n4 in range(4):
                    mm(ps[n4][:], d_[:], u_[:, D + n4 * 512:D + (n4 + 1) * 512], sl == 0, sl == 127, [d_, u_], [ps[n4]])

            for sl in range(128):
                u_ = UR[nrow_c[0] % NR]
                nrow_c[0] += 1
                rows[sl] = u_
                S.dma(G, None, None, [idx_, uv_b], [u_], stream=u_.name,
                      fn=lambda e, u_=u_, idx_=idx_, sl=sl: e.indirect_dma_start(
                          out=u_[:], out_offset=None, in_=uv_b[:],
                          in_offset=bass.IndirectOffsetOnAxis(ap=idx_[:, sl:sl + 1], axis=0)))
                stt(junk[:], u_[:, 0:D], 1.0, HG[bi % 2][:], ALU.mult, ALU.mult, [u_, HG[bi % 2]], [junk, Ag], accum=Ag[:, sl:sl + 1])
                act(Gs[:, sl:sl + 1], Ag[:, sl:sl + 1], AF.Gelu, [Ag, r2c], [Gs], scale=r2c[:, tb:tb + 1])
                if sl >= 1:
                    second_half(sl - 1)
                if sl >= 2:
                    next(filler, None)
            second_half(127)
            for _ in filler:
                pass
            for n4 in range(4):
                tt(H1_[:, tb, n4 * 512:(n4 + 1) * 512], ps[n4][:], H1_[:, tb, n4 * 512:(n4 + 1) * 512], ALU.add,
                   [ps[n4], H1_], [H1_])
            S.dma(Q, out_own[r0:r0 + 128, :], H1_[:, tb, :], [H1_], [out_own], stream="outst")

        blocks = [(tc, tb) for tc in range(TO // TC) for tb in range(NTB)]

        def filler_for(nxt):
            if nxt is None:
                return
            if nxt[1] == 0:
                yield from chunk_work(nxt[0])
            yield from prep_block(*nxt)

        for _ in filler_for(blocks[0]):
            pass
        for bi, (tc, tb) in enumerate(blocks):
            nxt = blocks[bi + 1] if bi + 1 < len(blocks) else None
            gather_block(tc, tb, filler_for(nxt))
        S.flush(final_streams=["outst"])

    S.es = es
    es.close()
    return nc


def make_in_maps(inputs, NJ):
    f = lambda a: np.ascontiguousarray(np.asarray(a, dtype=np.float32))
    x = f(inputs["x"])[0]
    S_ = x.shape[0]
    assert S_ == NCORE * NJ * 128
    meta = f(inputs["meta_tokens"])
    xT_all = np.ascontiguousarray(np.concatenate([meta, x], axis=0).T)
    w_in = f(inputs["w_in"])[0]
    w_out = f(inputs["w_out"])[0]
    w_q = f(inputs["peer_w_query"])[0]
    sk = f(inputs["peer_sub_keys"])[0]
    skT = np.ascontiguousarray(sk.reshape(16, 128, 128).transpose(2, 0, 1))
    u = f(inputs["peer_u"])[0]
    v = f(inputs["peer_v"])[0]
    vecs = np.zeros((128, 64), np.float32)
    vecs[:, 0:16] = f(inputs["norm_mix"])[0].reshape(16, 128).T
    vecs[:, 16:32] = f(inputs["norm_ffn"])[0].reshape(16, 128).T
    vecs[:, 32] = f(inputs["fox_q_gain"])[0]
    vecs[:, 33] = f(inputs["fox_k_gain"])[0]
    vecs[:, 34:42] = f(inputs["sb_out_gain"])[0].reshape(8, 128).T
    vecs[:, 42:50] = f(inputs["fox_out_gain"])[0].reshape(8, 128).T
    vecs[0:8, 50] = f(inputs["b_forget"])[0]
    g2row = np.ascontiguousarray(np.broadcast_to(f(inputs["norm_ffn"])[0][None, :], (128, D)))
    consts = np.zeros((128, 4, 128), np.float32)
    kk = np.arange(128)
    consts[:, 0, :] = np.where(kk[:, None] >= kk[None, :], -1.0, 0.0)
    consts[:, 1, :] = 1.0
    consts[:, 2, :] = np.eye(128, dtype=np.float32)
    consts[:, 3, :] = kk[None, :].astype(np.float32)
    maps = []
    for c in range(NCORE):
        blocks = [c + NCORE * j for j in range(NJ)]
        rows = np.concatenate([np.arange(b * 128, (b + 1) * 128) for b in blocks])
        x_own = np.ascontiguousarray(x[rows])
        xT_own = np.ascontiguousarray(x_own.T)
        mk = np.zeros((128, 2, 8, 128), np.float32)
        for i in range(8):
            if i > c:
                mk[:, :, i, :] = NEG
            elif i == c:
                mk[:, 0, i, :] = np.where(kk[:, None] < kk[None, :], 0.0, NEG)
                mk[:, 1, i, :] = np.where(kk[:, None] <= kk[None, :], 0.0, NEG)
        oh = np.zeros((1, 8), np.float32)
        oh[0, c] = 1.0
        maps.append({
            "xT_all": xT_all, "xT_own": xT_own, "x_own": x_own, "w_in": w_in, "w_out": w_out, "w_q": w_q,
            "skT": skT, "peer_u": u, "peer_v": v, "vecs": vecs, "g2row": g2row, "consts": consts,
            "maskadd": mk, "onehot_c": oh,
        })
    return maps


def assemble(results, NJ):
    S_ = NCORE * NJ * 128
    out = np.zeros((1, S_, D), np.float32)
    for c in range(NCORE):
        o = np.asarray(results[c]["out_own"], dtype=np.float32)
        for j in range(NJ):
            b = c + NCORE * j
            out[0, b * 128:(b + 1) * 128] = o[j * 128:(j + 1) * 128]
    return out


_NC_CACHE = {}


def kernel(**inputs):
    NJ = 16
    if NJ not in _NC_CACHE:
        _NC_CACHE[NJ] = build_nc(NJ)
    nc = _NC_CACHE[NJ]
    maps = make_in_maps(inputs, NJ)
    res = run_bass_kernel_spmd(nc, maps, core_ids=list(range(NCORE)))
    return assemble(res.results, NJ)
```

```python
import math
from contextlib import ExitStack

import numpy as np
import concourse.bass as bass
import concourse.mybir as mybir
from concourse.bass_utils import run_bass_kernel_spmd

F32 = mybir.dt.float32
BF16 = mybir.dt.bfloat16
I32 = mybir.dt.int32
U32 = mybir.dt.uint32
AF = mybir.ActivationFunctionType
ALU = mybir.AluOpType
AX = mybir.AxisListType

D = 2048
KC = 16
NCORE = 8
N_META = 16
EPS = 1e-6
SCALE = 1.0 / math.sqrt(128.0)
NEG = -30000.0
N_EXP = 16384


class Tk:
    def __init__(self, t, name=""):
        self.t = t
        self.name = name
        self.lw = None
        self.rd = []

    def __getitem__(self, k):
        return self.t[k]


class TkView:
    def __init__(self, base, fn):
        self.base = base
        self.fn = fn
        self.name = base.name

    def __getitem__(self, k):
        return self.fn(self.base.t[:])[k]

    @property
    def lw(self):
        return self.base.lw

    @lw.setter
    def lw(self, v):
        self.base.lw = v

    @property
    def rd(self):
        return self.base.rd

    @rd.setter
    def rd(self, v):
        self.base.rd = v


class Op:
    __slots__ = ("eng", "fn", "deps", "dma", "stream", "sidx", "awaited", "mile", "idx")

    def __init__(self, eng, fn, dma=False, stream=None):
        self.eng = eng
        self.fn = fn
        self.deps = []
        self.dma = dma
        self.stream = stream
        self.sidx = 0
        self.awaited = False
        self.mile = 0
        self.idx = 0


class Sched:
    ENGS = ("tensor", "vector", "scalar", "gpsimd", "sync")

    def __init__(self, nc, es):
        self.nc = nc
        self.es = es
        self.es0 = es
        self.ops = {e: [] for e in self.ENGS}
        self.streams = {}
        self.nops = 0

    def tile(self, name, shape, dt):
        return Tk(self.es.enter_context(self.nc.sbuf_tensor(name, list(shape), dt)), name)

    def psum(self, name, shape=(128, 512), dt=F32):
        return Tk(self.es.enter_context(self.nc.psum_tensor(name, list(shape), dt)), name)

    def dram(self, name, shape, dt, kind="Internal"):
        t = self.nc.dram_tensor(name, list(shape), dt, kind=kind)
        return Tk(t.ap(), name)

    def _add(self, op, reads, writes):
        deps = []
        for t in reads:
            if t.lw is not None:
                deps.append(t.lw)
        for t in writes:
            if t.lw is not None and (t.lw.dma or op.dma or t.lw.eng != op.eng):
                deps.append(t.lw)
            deps.extend(r for r in t.rd if r.dma or op.dma or r.eng != op.eng)
        seen = set()
        for d in deps:
            if d is op or id(d) in seen:
                continue
            seen.add(id(d))
            if (not d.dma) and d.eng == op.eng and op.eng == "tensor" and not op.dma:
                continue
            op.deps.append(d)
            d.awaited = True
        for t in reads:
            t.rd = [r for r in t.rd if r.dma or r.eng != op.eng or op.dma] + [op]
        for t in writes:
            t.lw = op
            t.rd = []
        op.idx = self.nops
        self.nops += 1
        self.ops[op.eng].append(op)
        return op

    def op(self, eng, fn, reads=(), writes=()):
        return self._add(Op(eng, fn), reads, writes)

    def dma(self, eng, out, in_, reads=(), writes=(), stream=None, fn=None):
        if stream is None:
            stream = "dma_" + (writes[0].name if writes else "x")
        if fn is None:
            fn = lambda e, out=out, in_=in_: e.dma_start(out=out, in_=in_)
        op = Op(eng, fn, dma=True, stream=stream)
        st = self.streams.setdefault(stream, [])
        if st:
            op.deps.append(st[-1])
            st[-1].awaited = True
        st.append(op)
        op.sidx = len(st)
        return self._add(op, reads, writes)

    def flush(self, final_streams=()):
        nc = self.nc
        if not hasattr(self, "prog"):
            self.prog = {e: self.es0.enter_context(nc.semaphore("prog_" + e)) for e in self.ENGS}
            self.ssem = {}
            self.mcount = {e: 0 for e in self.ENGS}
            self.waited = {e: {} for e in self.ENGS}
            self.first_flush = True
        prog, ssem = self.prog, self.ssem
        for s in self.streams:
            if s not in ssem:
                ssem[s] = self.es0.enter_context(nc.semaphore("s_" + s))
        for e in self.ENGS:
            comp = [o for o in self.ops[e] if not o.dma]
            if comp:
                comp[-1].awaited = True
            m = self.mcount[e]
            pending = []
            for o in comp:
                pending.append(o)
                if o.awaited:
                    m += 1
                    for p in pending:
                        p.mile = m
                    pending = []
            self.mcount[e] = m
        barrier = {}
        if not self.first_flush:
            for e in self.ENGS:
                if self.bar_m[e] > 0:
                    barrier[("p", e)] = self.bar_m[e]
            for s, n in self.bar_s.items():
                if n > 0:
                    barrier[("s", s)] = 16 * n
        streams = self.streams
        block = self.es.enter_context(nc.Block())

        def run(ename, eng):
            waited = self.waited[ename]

            def do_wait(key, val):
                if waited.get(key, 0) >= val:
                    return
                waited[key] = val
                sem = ssem[key[1]] if key[0] == "s" else prog[key[1]]
                eng.wait_ge(sem, val)

            for key, val in barrier.items():
                do_wait(key, val)
            for o in self.ops[ename]:
                need = {}
                for d in o.deps:
                    if d.dma:
                        key = ("s", d.stream)
                        val = 16 * d.sidx
                    else:
                        key = ("p", d.eng)
                        val = d.mile
                    if val > need.get(key, 0):
                        need[key] = val
                for key, val in need.items():
                    do_wait(key, val)
                ins = o.fn(eng)
                if o.dma:
                    ins.then_inc(ssem[o.stream], 16)
                elif o.awaited:
                    ins.then_inc(prog[ename], 1)
            if ename == "sync":
                for s in final_streams:
                    eng.wait_ge(ssem[s], 16 * len(streams[s]))

        @block.tensor
        def _(e):
            run("tensor", e)

        @block.vector
        def _(e):
            run("vector", e)

        @block.scalar
        def _(e):
            run("scalar", e)

        @block.gpsimd
        def _(e):
            run("gpsimd", e)

        @block.sync
        def _(e):
            run("sync", e)

        self.bar_m = dict(self.mcount)
        self.bar_s = {s: len(v) for s, v in self.streams.items()}
        self.first_flush = False
        self.ops = {e: [] for e in self.ENGS}


def build_nc(NJ, debug=False):
    NXB = NCORE * NJ
    NKB = NXB + 1
    T_ALL = N_META + NXB * 128
    TO = NJ * 128
    NMT = NJ // 4
    assert NJ % 4 == 0

    nc = bass.Bass("TRN2", target_bir_lowering=False)
    es = ExitStack()
    es.enter_context(nc.allow_low_precision("bf16 matmul operands by design; fp32 accumulation"))
    S = Sched(nc, es)

    def ext(name, shape, dt=F32, kind="ExternalInput"):
        return Tk(nc.dram_tensor(name, list(shape), dt, kind=kind).ap(), name)

    xT_all = ext("xT_all", [D, T_ALL])
    xT_own = ext("xT_own", [D, TO])
    x_own = ext("x_own", [TO, D])
    w_in = ext("w_in", [D, 7176])
    w_out = ext("w_out", [D, D])
    w_q = ext("w_q", [D, D])
    skT = ext("skT", [128, 16, 128])
    u_t = ext("peer_u", [N_EXP, D])
    v_t = ext("peer_v", [N_EXP, D])
    vecs = ext("vecs", [128, 64])
    g2row = ext("g2row", [128, D])
    consts = ext("consts", [128, 4, 128])
    maskadd = ext("maskadd", [128, 2, 8, 128])
    onehot_c = ext("onehot_c", [1, 8])
    out_own = ext("out_own", [TO, D], kind="ExternalOutput")

    kT_s = S.dram("kT_s", [16, 128, T_ALL], BF16)
    v_s = S.dram("v_s", [16, 128, NKB, 128], BF16)
    y0_s = S.dram("y0_s", [8, T_ALL], F32)
    qT_s = S.dram("qT_s", [16, 128, TO], BF16)
    gT_s = S.dram("gT_s", [8, 128, TO], BF16)
    oT_s = S.dram("oT_s", [16, 128, TO], F32)
    uv_b = S.dram("uv_b", [N_EXP, 2 * D], BF16)
    wo_fm = S.dram("wo_fm", [16, 128, KC, 128], BF16)
    wo_tm = S.dram("wo_tm", [8, 128, KC, 256], BF16)
    wq_fm = S.dram("wq_fm", [16, 128, KC, 128], BF16)

    ps = [S.psum("ps%d" % i) for i in range(8)]

    V, A, P, G, Q = "vector", "scalar", "tensor", "gpsimd", "sync"

    def act(out, in_, func, reads, writes, bias=None, scale=None, accum=None, eng=A):
        kw = {}
        if bias is not None:
            kw["bias"] = bias
        if scale is not None:
            kw["scale"] = scale
        if accum is not None:
            kw["accum_out"] = accum
        return S.op(eng, lambda e: e.activation(out=out, in_=in_, func=func, **kw), reads, writes)

    def tt(out, in0, in1, op, reads, writes, eng=V):
        return S.op(eng, lambda e: e.tensor_tensor(out=out, in0=in0, in1=in1, op=op), reads, writes)

    def ts(out, in0, s1, op0, reads, writes, s2=None, op1=None, eng=V):
        if op1 is None:
            return S.op(eng, lambda e: e.tensor_scalar(out=out, in0=in0, scalar1=s1, scalar2=None, op0=op0), reads, writes)
        return S.op(eng, lambda e: e.tensor_scalar(out=out, in0=in0, scalar1=s1, scalar2=s2, op0=op0, op1=op1), reads, writes)

    def stt(out, in0, scalar, in1, op0, op1, reads, writes, accum=None):
        if accum is None:
            return S.op(V, lambda e: e.scalar_tensor_tensor(out=out, in0=in0, scalar=scalar, in1=in1, op0=op0, op1=op1), reads, writes)
        return S.op(V, lambda e: e.scalar_tensor_tensor(out=out, in0=in0, scalar=scalar, in1=in1, op0=op0, op1=op1, accum_out=accum), reads, writes)

    def cp(out, in_, reads, writes, eng=V):
        return S.op(eng, lambda e: e.tensor_copy(out=out, in_=in_), reads, writes)

    def mm(out, lhsT, rhs, start, stop, reads, writes):
        return S.op(P, lambda e: e.matmul(out, lhsT, rhs, start=start, stop=stop), reads, writes)

    def memset(ap, val, writes, eng=V):
        return S.op(eng, lambda e: e.memset(ap, val), (), writes)

    def rstd_from(out_t, out_ap, ps_t, ps_ap, inv_n, tmp_t, tmp_ap):
        act(tmp_ap, ps_ap, AF.Ln, [ps_t], [tmp_t], bias=EPS, scale=inv_n)
        act(out_ap, tmp_ap, AF.Exp, [tmp_t], [out_t], scale=-0.5)

    c32 = S.tile("c32", [128, 4, 128], F32)
    S.dma(Q, c32[:], consts[:], [consts], [c32])
    negtri = S.tile("negtri", [128, 128], BF16)
    onesb = S.tile("onesb", [128, 128], BF16)
    identb = S.tile("identb", [128, 128], BF16)
    zerob = S.tile("zerob", [128, 512], BF16)
    cp(negtri[:], c32[:, 0, :], [c32], [negtri])
    cp(onesb[:], c32[:, 1, :], [c32], [onesb])
    cp(identb[:], c32[:, 2, :], [c32], [identb])
    memset(zerob[:], 0.0, [zerob])
    vec = S.tile("vec", [128, 64], F32)
    S.dma(Q, vec[:], vecs[:], [vecs], [vec])
    vec2 = S.tile("vec2", [128, 2], F32)
    ts(vec2[:, 0:1], vec[:, 32:33], SCALE, ALU.mult, [vec], [vec2])
    ts(vec2[:, 1:2], vec[:, 50:51], -1.0, ALU.mult, [vec], [vec2])
    maskb = S.tile("maskb", [128, 2, 8, 128], BF16)
    ohc = S.tile("ohc", [128, 8], F32)
    S.dma(Q, ohc[64:65, :], onehot_c[:], [onehot_c], [ohc])
    ncf_cols = S.tile("ncf_cols", [128, 8, NKB], F32)
    cmid = S.tile("cmid", [128, 8, NJ], F32)
    cfull = S.tile("cfull", [128, 8, NJ], F32)

    NHT = 4 * (N_EXP // 128)
    conv_state = [0, 0]

    def conv_load(cst32):
        n_ = conv_state[0]
        if n_ >= NHT:
            return
        conv_state[0] += 1
        src = u_t if n_ < NHT // 2 else v_t
        rt = (n_ % (NHT // 2)) // 2
        hf = n_ % 2
        a = cst32[n_ % len(cst32)]
        S.dma(Q, a[:], src[rt * 128:(rt + 1) * 128, hf * 1024:(hf + 1) * 1024], [src], [a], stream=a.name)

    g2_tile = [None]

    def conv_finish(cst32, cst16, on_act=False):
        n_ = conv_state[1]
        if n_ >= conv_state[0]:
            return
        conv_state[1] += 1
        coff = 0 if n_ < NHT // 2 else D
        rt = (n_ % (NHT // 2)) // 2
        hf = n_ % 2
        a, b = cst32[n_ % len(cst32)], cst16[n_ % len(cst16)]
        if on_act:
            act(b[:], a[:], AF.Copy, [a], [b])
        else:
            cp(b[:], a[:], [a], [b])
        S.dma(Q, uv_b[rt * 128:(rt + 1) * 128, coff + hf * 1024:coff + (hf + 1) * 1024], b[:], [b], [uv_b], stream=b.name)

    def conv_steps(k, cst32, cst16, lookahead=3, on_act=False):
        for _ in range(k):
            while conv_state[0] < min(NHT, conv_state[1] + lookahead):
                conv_load(cst32)
            conv_finish(cst32, cst16, on_act)

    def conv_drain(cst32, cst16, on_act=False):
        while conv_state[1] < conv_state[0]:
            conv_finish(cst32, cst16, on_act)

    def kcol(slot):
        if slot == 0:
            return 0, N_META
        return N_META + 128 * (slot - 1), 128

    with ExitStack() as es0:
        S.es = es0
        m32 = S.tile("m32", [128, 2, 8, 128], F32)
        S.dma(Q, m32[:], maskadd[:], [maskadd], [m32])
        cp(maskb[:], m32[:], [m32], [maskb])
        S.flush()

    with ExitStack() as es1:
        S.es = es1
        GK = 256
        wk = S.tile("wk", [128, KC, 2048], BF16)
        wf = S.tile("wf", [128, KC, 8], BF16)
        wld = [S.tile("wldk%d" % i, [128, 1024], F32) for i in range(2)]
        n = 0
        for (dst, coff, scol) in ((wk, 0, 1024), (wk, 1024, 4096)):
            for kc in range(KC):
                a = wld[n % 2]
                S.dma(Q, a[:], w_in[kc * 128:(kc + 1) * 128, scol:scol + 1024], [w_in], [a])
                ts(dst[:, kc, coff:coff + 1024], a[:], vec[:, kc:kc + 1], ALU.mult, [a, vec], [dst],
                   eng=(V if n % 2 else G))
                n += 1
        wfl = S.tile("wfl", [128, KC, 8], F32)
        S.dma(Q, wfl[:], None, [w_in], [wfl],
              fn=lambda e: e.dma_start(out=wfl[:], in_=w_in[:, 7168:7176].rearrange("(kc p) c -> p kc c", p=128)))
        for kc in range(KC):
            ts(wf[:, kc, :], wfl[:, kc, :], vec[:, kc:kc + 1], ALU.mult, [wfl, vec], [wf])
        xs = [S.tile("xsk%d" % i, [128, KC, GK], F32) for i in range(2)]
        xbk2 = [S.tile("xbk%d" % i, [128, KC, GK], BF16) for i in range(2)]
        sqx2 = [S.tile("sqx%d" % i, [128, KC, GK], BF16) for i in range(2)]
        lnt = S.tile("lnt", [128, GK], F32)
        lnt2 = [S.tile("lnt2_%d" % i, [128, GK], F32) for i in range(2)]
        rsk = S.tile("rsk", [128, GK], F32)
        kst2 = [S.tile("kstk%d" % i, [128, 16, GK], BF16) for i in range(2)]
        kf = [S.tile("kf%d" % i, [128, GK], F32) for i in range(2)]
        sqk = [S.tile("sqk%d" % i, [128, GK], BF16) for i in range(2)]
        rk = [S.tile("rk%d" % i, [128, GK], F32) for i in range(2)]
        yst = [S.tile("yst%d" % i, [8, GK], F32) for i in range(2)]
        xTv = xT_all[:].rearrange("(kc p) t -> p kc t", p=128)
        groups = [(0, N_META)] + [(N_META + GK * i, GK) for i in range(NXB * 128 // GK)]
        def k_prologue(gi):
            t0, Gn = groups[gi]
            x_ = xs[gi % 2]
            S.dma(Q, x_[:, :, 0:Gn], xTv[:, :, t0:t0 + Gn], [xT_all], [x_], stream="xsk%d" % (gi % 2))
            cp(xbk2[gi % 2][:, :, 0:Gn], x_[:, :, 0:Gn], [x_], [xbk2[gi % 2]], eng=G)
            act(sqx2[gi % 2][:, :, 0:Gn], x_[:, :, 0:Gn], AF.Square, [x_], [sqx2[gi % 2]])

        k_prologue(0)
        for gi, (t0, Gn) in enumerate(groups):
            xbk, sqx, kst = xbk2[gi % 2], sqx2[gi % 2], kst2[gi % 2]
            for kc in range(KC):
                mm(ps[0][:, 0:Gn], onesb[:], sqx[:, kc, 0:Gn], kc == 0, kc == KC - 1, [onesb, sqx], [ps[0]])
            rstd_from(rsk, rsk[:, 0:Gn], ps[0], ps[0][:, 0:Gn], 1.0 / D, lnt, lnt[:, 0:Gn])
            for kc in range(KC):
                mm(ps[1][0:8, 0:Gn], wf[:, kc, :], xbk[:, kc, 0:Gn], kc == 0, kc == KC - 1, [wf, xbk], [ps[1]])
            ys = yst[gi % 2]
            tt(ys[:, 0:Gn], ps[1][0:8, 0:Gn], rsk[0:8, 0:Gn], ALU.mult, [ps[1], rsk], [ys])
            S.dma(A, y0_s[:, t0:t0 + Gn], ys[:, 0:Gn], [ys], [y0_s], stream="yst%d" % (gi % 2))
            if gi + 1 < len(groups):
                k_prologue(gi + 1)

            def fox_norm(h):
                kf_, sqk_, rk_ = kf[h % 2], sqk[h % 2], rk[h % 2]
                pn = ps[6 + h % 2]
                mm(pn[:, 0:Gn], onesb[:], sqk_[:, 0:Gn], True, True, [onesb, sqk_], [pn])
                rstd_from(rk_, rk_[:, 0:Gn], pn, pn[:, 0:Gn], 1.0 / 128, lnt2[h % 2], lnt2[h % 2][:, 0:Gn])
                stt(kst[:, h, 0:Gn], kf_[:, 0:Gn], vec[:, 33:34], rk_[:, 0:Gn], ALU.mult, ALU.mult, [kf_, vec, rk_], [kst])

            order = [8, 9, 0, 10, 1, 11, 2, 12, 3, 13, 4, 14, 5, 15, 6, 7]
            pend = []
            for oi, h in enumerate(order):
                pk = ps[2 + oi % 4]
                for kc in range(KC):
                    mm(pk[:, 0:Gn], wk[:, kc, h * 128:(h + 1) * 128], xbk[:, kc, 0:Gn], kc == 0, kc == KC - 1, [wk, xbk], [pk])
                if h < 8:
                    tt(kst[:, h, 0:Gn], pk[:, 0:Gn], rsk[:, 0:Gn], ALU.mult, [pk, rsk], [kst])
                else:
                    kf_, sqk_ = kf[h % 2], sqk[h % 2]
                    tt(kf_[:, 0:Gn], pk[:, 0:Gn], rsk[:, 0:Gn], ALU.mult, [pk, rsk], [kf_])
                    act(sqk_[:, 0:Gn], kf_[:, 0:Gn], AF.Square, [kf_], [sqk_])
                    pend.append((oi, h))
                while pend and pend[0][0] <= oi - 1:
                    fox_norm(pend.pop(0)[1])
            while pend:
                fox_norm(pend.pop(0)[1])
            S.dma(A, kT_s[:, :, t0:t0 + Gn].rearrange("h d t -> d h t"), kst[:, :, 0:Gn], [kst], [kT_s], stream="kstk%d" % (gi % 2))
        S.flush()

    with ExitStack() as es1v:
        S.es = es1v
        wv = S.tile("wv", [128, KC, 2048], BF16)
        stg32 = [S.tile("stg32_%d" % i, [128, 1024], F32) for i in range(2)]
        wld = stg32
        n = 0
        for (dst, coff, scol) in ((wv, 0, 2048), (wv, 1024, 5120)):
            for kc in range(KC):
                a = wld[n % 2]
                S.dma(Q, a[:], w_in[kc * 128:(kc + 1) * 128, scol:scol + 1024], [w_in], [a], stream="stg32_%d" % (n % 2))
                ts(dst[:, kc, coff:coff + 1024], a[:], vec[:, kc:kc + 1], ALU.mult, [a, vec], [dst],
                   eng=(V if n % 2 else G))
                n += 1
        xs = [S.tile("xs%d" % i, [128, KC, 128], F32) for i in range(3)]
        xb = [S.tile("xb%d" % i, [128, KC, 128], BF16) for i in range(2)]
        sq = [S.tile("sq%d" % i, [128, KC, 128], BF16) for i in range(2)]
        lntv = [S.tile("lntv%d" % i, [128, 1], F32) for i in range(2)]
        rcol = [S.tile("rcol%d" % i, [128, 1], F32) for i in range(2)]
        vst = [S.tile("vst%d" % i, [128, 2048], BF16) for i in range(2)]
        cstB32 = [S.tile("cstB32_%d" % i, [128, 1024], F32) for i in range(4)]
        cstB16 = [S.tile("cstB16_%d" % i, [128, 1024], BF16) for i in range(2)]

        wjobs = [(src, dst, gcol, kc, hf) for (src, dst, gcol) in ((w_out, wo_fm, None), (w_q, wq_fm, 16))
                 for kc in range(KC) for hf in range(2)]
        wstate = [0, 0]

        def w_load():
            n_ = wstate[0]
            if n_ >= len(wjobs):
                return
            wstate[0] += 1
            src, dst, gcol, kc, hf = wjobs[n_]
            a = cstB32[n_ % 4]
            S.dma(Q, a[:], src[kc * 128:(kc + 1) * 128, hf * 1024:(hf + 1) * 1024], [src], [a], stream=a.name)

        def w_finish():
            n_ = wstate[1]
            if n_ >= wstate[0]:
                return
            wstate[1] += 1
            src, dst, gcol, kc, hf = wjobs[n_]
            a, b = cstB32[n_ % 4], cstB16[n_ % 2]
            if gcol is None:
                cp(b[:], a[:], [a], [b])
            else:
                ts(b[:], a[:], vec[:, gcol + kc:gcol + kc + 1], ALU.mult, [a, vec], [b])
            S.dma(Q, dst[hf * 8:(hf + 1) * 8, :, kc, :].rearrange("n p c -> p n c"),
                  b[:].rearrange("p (n c) -> p n c", c=128), [b], [dst], stream=b.name)
            if gcol is None:
                S.dma(Q, wo_tm[hf * 4:(hf + 1) * 4, :, kc, :].rearrange("g p c -> p g c"),
                      b[:].rearrange("p (g c) -> p g c", c=256), [b], [wo_tm], stream=b.name + "t")

        def w_step():
            while wstate[0] < min(len(wjobs), wstate[1] + 3):
                w_load()
            w_finish()

        def v_prologue(slot):
            t0, Gn = kcol(slot)
            x_ = xs[slot % 3]
            S.dma(Q, x_[:, :, 0:Gn], xTv[:, :, t0:t0 + Gn], [xT_all], [x_], stream="xs%d" % (slot % 3))
            cp(xb[slot % 2][:, :, 0:Gn], x_[:, :, 0:Gn], [x_], [xb[slot % 2]], eng=G)
            act(sq[slot % 2][:, :, 0:Gn], x_[:, :, 0:Gn], AF.Square, [x_], [sq[slot % 2]])

        v_prologue(0)
        for slot in range(NKB):
            t0, Gn = kcol(slot)
            b2 = slot % 2
            xb_, sq_, rc_, vs_ = xb[b2], sq[b2], rcol[b2], vst[b2]
            for kc in range(KC):
                mm(ps[b2][0:Gn, 0:1], sq_[:, kc, 0:Gn], onesb[:, 0:1], kc == 0, kc == KC - 1, [onesb, sq_], [ps[b2]])
            rstd_from(rc_, rc_[0:Gn, :], ps[b2], ps[b2][0:Gn, 0:1], 1.0 / D, lntv[b2], lntv[b2][0:Gn, 0:1])
            if slot + 1 < NKB:
                v_prologue(slot + 1)
            if slot % 2 == 0:
                w_step()
            for cg in range(4):
                pv = ps[2 + (slot * 4 + cg) % 6]
                for kc in range(KC):
                    mm(pv[0:Gn, :], xb_[:, kc, 0:Gn], wv[:, kc, cg * 512:(cg + 1) * 512], kc == 0, kc == KC - 1, [xb_, wv], [pv])
                act(vs_[0:Gn, cg * 512:(cg + 1) * 512], pv[0:Gn, :], AF.Copy, [pv, rc_], [vs_], scale=rc_[0:Gn, 0:1])
            S.dma(A, v_s[:, 0:Gn, slot, :].rearrange("h t d -> t h d"), vs_[0:Gn, :].rearrange("t (h d) -> t h d", h=16),
                  [vs_], [v_s], stream="vst%d" % b2)
        while wstate[1] < len(wjobs):
            w_step()
        S.flush()

    with ExitStack() as es2:
        S.es = es2
        wqg = S.tile("wqg", [128, KC, 3072], BF16)
        wld = [S.tile("wld2_%d" % i, [128, 1024], F32) for i in range(2)]
        n = 0
        for (coff, scol) in ((0, 0), (1024, 3072), (2048, 6144)):
            for kc in range(KC):
                a = wld[n % 2]
                S.dma(Q, a[:], w_in[kc * 128:(kc + 1) * 128, scol:scol + 1024], [w_in], [a])
                ts(wqg[:, kc, coff:coff + 1024], a[:], vec[:, kc:kc + 1], ALU.mult, [a, vec], [wqg],
                   eng=(V if n % 2 else G))
                n += 1
        x2 = S.tile("x2", [128, KC, 512], F32)
        xb2 = S.tile("xb2", [128, KC, 512], BF16)
        sq2 = S.tile("sq2", [128, KC, 512], BF16)
        ln2 = S.tile("ln2", [128, 512], F32)
        rs2 = S.tile("rs2", [128, 512], F32)
        qf = [S.tile("qf%d" % i, [128, 512], F32) for i in range(2)]
        qsq = [S.tile("qsq%d" % i, [128, 512], BF16) for i in range(2)]
        rq = [S.tile("rq%d" % i, [128, 512], F32) for i in range(2)]
        qst = [S.tile("qst%d" % i, [128, 512], BF16) for i in range(3)]
        xTo = xT_own[:].rearrange("(kc p) t -> p kc t", p=128)
        for gi in range(TO // 512):
            c0 = gi * 512
            S.dma(Q, x2[:], xTo[:, :, c0:c0 + 512], [xT_own], [x2])
            cp(xb2[:], x2[:], [x2], [xb2], eng=G)
            act(sq2[:], x2[:], AF.Square, [x2], [sq2])
            for kc in range(KC):
                mm(ps[0][:], onesb[:], sq2[:, kc, :], kc == 0, kc == KC - 1, [onesb, sq2], [ps[0]])
            rstd_from(rs2, rs2[:], ps[0], ps[0][:], 1.0 / D, ln2, ln2[:])
            for cc in range(24):
                pq = ps[2 + cc % 4]
                for kc in range(KC):
                    mm(pq[:], wqg[:, kc, cc * 128:(cc + 1) * 128], xb2[:, kc, :], kc == 0, kc == KC - 1, [wqg, xb2], [pq])
                o_ = qst[cc % 3]
                if cc < 8:
                    stt(o_[:], pq[:], SCALE, rs2[:], ALU.mult, ALU.mult, [pq, rs2], [o_])
                    S.dma(A, qT_s[cc, :, c0:c0 + 512], o_[:], [o_], [qT_s], stream="qst%d" % (cc % 3))
                elif cc < 16:
                    f_, s_, r_ = qf[cc % 2], qsq[cc % 2], rq[cc % 2]
                    tt(f_[:], pq[:], rs2[:], ALU.mult, [pq, rs2], [f_])
                    act(s_[:], f_[:], AF.Square, [f_], [s_])
                    mm(ps[1][:], onesb[:], s_[:], True, True, [onesb, s_], [ps[1]])
                    rstd_from(r_, r_[:], ps[1], ps[1][:], 1.0 / 128, ln2, ln2[:])
                    stt(o_[:], f_[:], vec2[:, 0:1], r_[:], ALU.mult, ALU.mult, [f_, vec2, r_], [o_])
                    S.dma(A, qT_s[cc, :, c0:c0 + 512], o_[:], [o_], [qT_s], stream="qst%d" % (cc % 3))
                else:
                    f_ = qf[cc % 2]
                    tt(f_[:], pq[:], rs2[:], ALU.mult, [pq, rs2], [f_])
                    act(f_[:], f_[:], AF.Exp, [f_], [f_], scale=-1.0)
                    ts(f_[:], f_[:], 1.0, ALU.add, [f_], [f_])
                    S.op(V, lambda e, o=o_, f=f_: e.reciprocal(out=o[:], in_=f[:]), [f_], [o_])
                    S.dma(A, gT_s[cc - 16, :, c0:c0 + 512], o_[:], [o_], [gT_s], stream="qst%d" % (cc % 3))
        S.flush()

    with ExitStack() as es2b:
        S.es = es2b
        yl = S.tile("yl", [8, T_ALL], F32)
        ncf = S.tile("ncf", [8, T_ALL], F32)
        one8 = S.tile("one8", [8, 1], F32)
        id32 = S.tile("id32", [8, 8], F32)
        memset(one8[:], 1.0, [one8])
        cp(id32[:], c32[0:8, 2, 0:8], [c32], [id32])
        S.dma(Q, yl[:], y0_s[:], [y0_s], [yl])
        act(yl[:], yl[:], AF.Exp, [yl, vec2], [yl], bias=vec2[0:8, 1:2], scale=-1.0)
        act(yl[:], yl[:], AF.Ln, [yl], [yl], bias=1.0)
        CH = 2048
        pos = 0
        while pos < T_ALL:
            n_ = min(CH, T_ALL - pos)
            init = 0.0 if pos == 0 else ncf[:, pos - 1:pos]
            S.op(V, lambda e, pos=pos, n_=n_, init=init: e.tensor_tensor_scan(
                out=ncf[:, pos:pos + n_], data0=one8[:, 0:1].to_broadcast([8, n_]), data1=yl[:, pos:pos + n_],
                initial=init, op0=ALU.mult, op1=ALU.add), [yl, one8, ncf], [ncf])
            pos += n_
        for s0 in range(0, NKB, 64):
            ns = min(64, NKB - s0)
            pt = ps[(s0 // 64) % 2]
            for si in range(ns):
                t0, Gn = kcol(s0 + si)
                S.op(P, lambda e, pt=pt, si=si, t0=t0, Gn=Gn: e.transpose(pt[0:Gn, si * 8:si * 8 + 8], ncf[0:8, t0:t0 + Gn], id32[:]),
                     [ncf, id32], [pt])
            if s0 == 0:
                cp(ncf_cols[0:16, :, 0:1].rearrange("p h s -> p s h"), pt[0:16, 0:8].rearrange("p (s h) -> p s h", h=8),
                   [pt], [ncf_cols])
                cp(ncf_cols[:, :, 1:ns].rearrange("p h s -> p s h"), pt[:, 8:ns * 8].rearrange("p (s h) -> p s h", h=8),
                   [pt], [ncf_cols])
            else:
                cp(ncf_cols[:, :, s0:s0 + ns].rearrange("p h s -> p s h"), pt[:, 0:ns * 8].rearrange("p (s h) -> p s h", h=8),
                   [pt], [ncf_cols])
        tmpc = S.tile("tmpc", [128, 8, NJ, 8], F32)
        tt(tmpc[64:65], ncf_cols[64:65, :, 1:1 + NXB].rearrange("p h (j c) -> p h j c", c=8),
           ohc[64:65, :].unsqueeze(1).unsqueeze(1).to_broadcast([1, 8, NJ, 8]), ALU.mult, [ncf_cols, ohc], [tmpc])
        S.op(V, lambda e: e.tensor_reduce(out=cmid[64:65], in_=tmpc[64:65], axis=AX.X, op=ALU.add), [tmpc], [cmid])
        mm(ps[2][:, 0:8 * NJ], c32[64:65, 1, :], cmid[64:65].rearrange("p h j -> p (h j)"), True, True, [c32, cmid], [ps[2]])
        cp(cfull[:].rearrange("p h j -> p (h j)"), ps[2][:, 0:8 * NJ], [ps[2]], [cfull])
        S.flush()

    with ExitStack() as es3:
        S.es = es3
        KT = [S.tile("KT%d" % i, [128, T_ALL], BF16) for i in range(2)]
        VV = [S.tile("VV%d" % i, [128, NKB, 128], BF16) for i in range(2)]
        QT = [S.tile("QT%d" % i, [128, TO], BF16) for i in range(2)]
        crow = [S.tile("crow%d" % i, [128, NJ, 128], BF16) for i in range(2)]
        Et = [S.tile("Et%d" % i, [128, 512], F32) for i in range(2)]
        Lt = [S.tile("Lt%d" % i, [128, 512], BF16) for i in range(2)]
        Tt = [S.tile("Tt%d" % i, [128, 512], F32) for i in range(2)]
        Wt = [S.tile("Wt%d" % i, [128, 512], BF16) for i in range(3)]
        carry = [S.tile("carry%d" % i, [128, 512], F32) for i in range(2)]
        osb = [S.tile("osb%d" % i, [128, 512], F32) for i in range(2)]
        rden = S.tile("rden", [128, 512], F32)
        dacc = [S.tile("dacc%d" % i, [128, 512], F32) for i in range(4)]
        dhi = S.tile("dhi", [128, 512], BF16)
        dlo = S.tile("dlo", [128, 512], BF16)
        dtmp = S.tile("dtmp", [128, 512], F32)
        B1 = [ps[0], ps[1]]
        B2 = [ps[2], ps[3]]
        B3 = [ps[4], ps[5]]
        PO, PD = ps[6], ps[7]

        cstC32 = [S.tile("cstC32_%d" % i, [128, 1024], F32) for i in range(2)]
        cstC16 = [S.tile("cstC16_%d" % i, [128, 1024], BF16) for i in range(2)]
        fox_ctr = [0]

        def steps_for(m):
            st = []
            for g in range(4 * m + 3, -1, -1):
                r = max(0, g - 4 * m)
                for i in range(7, -1, -1):
                    st.append(dict(slot=1 + 8 * g + i, c0=128 * r, mask=(i if g >= 4 * m else None)))
            st.append(dict(slot=0, c0=0, mask=None))
            return st

        nmt = 0
        for h in range(16):
            hb = h % 2
            KT_, VV_, QT_ = KT[hb], VV[hb], QT[hb]
            is_sb = h < 8
            S.dma(Q, KT_[:], kT_s[h], [kT_s], [KT_])
            S.dma(Q, VV_[:], v_s[h], [v_s], [VV_])
            S.dma(Q, QT_[:], qT_s[h], [qT_s], [QT_])
            cr_ = crow[hb]
            if not is_sb:
                hf = h - 8
                ts(cr_[:], cfull[:, hf, :].unsqueeze(2).to_broadcast([128, NJ, 128]), -1.0 / 128, ALU.mult, [cfull], [cr_])
            for m in range(NMT):
                steps = steps_for(m)
                ns = len(steps)
                car = carry[nmt % 2]
                ob = osb[nmt % 2]
                nmt += 1
                q0 = 512 * m
                if is_sb:
                    memset(car[:], 0.0, [car], eng=G)
                mm(PO[:], zerob[:, 0:128], zerob[:], True, False, [zerob], [PO])
                da = dacc[nmt % 2]
                da2 = dacc[2 + nmt % 2]
                if not is_sb:
                    memset(da[:], 0.0, [da], eng=G)
                    memset(da2[:], 0.0, [da2], eng=G)

                def pe1(s):
                    d = steps[s]
                    t0, kp = kcol(d["slot"])
                    c0 = d["c0"]
                    b1 = B1[s % 2]
                    msk = d["mask"]
                    kl = KT_[:, t0:t0 + kp]
                    qr = QT_[:, q0 + c0:q0 + 512]
                    if is_sb:
                        b2_ = B2[s % 2]
                        for bb in (b1, b2_):
                            last = (msk is None) and (bb is b1)
                            mm(bb[0:kp, c0:512], kl, qr, True, last, [KT_, QT_], [bb])
                            if msk is not None:
                                mm(bb[0:kp, c0:c0 + 128], identb[:], maskb[:, 0, msk, :], False, bb is b1, [identb, maskb], [bb])
                    else:
                        mm(b1[0:kp, c0:512], kl, qr, True, False, [KT_, QT_], [b1])
                        mm(b1[0:kp, c0:512], onesb[:, 0:kp],
                           cr_[:, 4 * m:4 * m + 4, :].rearrange("p j t -> p (j t)")[:, c0:512],
                           False, msk is None, [onesb, cr_], [b1])
                        if msk is not None:
                            mm(b1[0:kp, c0:c0 + 128], identb[:], maskb[:, 1, msk, :], False, True, [identb, maskb], [b1])

                def act1(s):
                    d = steps[s]
                    t0, kp = kcol(d["slot"])
                    c0 = d["c0"]
                    b1 = B1[s % 2]
                    if is_sb:
                        e_, l_ = Et[s % 2], Lt[s % 2]
                        act(e_[0:kp, c0:512], b1[0:kp, c0:512], AF.Exp, [b1], [e_])
                        act(l_[0:kp, c0:512], e_[0:kp, c0:512], AF.Ln, [e_], [l_], bias=1.0)
                    else:
                        w_ = Wt[s % 3]
                        act(w_[0:kp, c0:512], b1[0:kp, c0:512], AF.Exp, [b1, ncf_cols], [w_],
                            bias=ncf_cols[0:kp, h - 8, d["slot"]:d["slot"] + 1])

                def pe2(s):
                    d = steps[s]
                    t0, kp = kcol(d["slot"])
                    c0 = d["c0"]
                    l_ = Lt[s % 2]
                    mm(B2[s % 2][0:kp, c0:512], negtri[0:kp, 0:kp], l_[0:kp, c0:512], False, True, [negtri, l_], [B2[s % 2]])
                    mm(B3[s % 2][:, c0:512], onesb[0:kp, :], l_[0:kp, c0:512], True, True, [onesb, l_], [B3[s % 2]])

                def dve1(s):
                    d = steps[s]
                    t0, kp = kcol(d["slot"])
                    c0 = d["c0"]
                    t_ = Tt[s % 2]
                    tt(t_[0:kp, c0:512], B2[s % 2][0:kp, c0:512], car[0:kp, c0:512], ALU.subtract, [B2[s % 2], car], [t_])
                    tt(car[:, c0:512], B3[s % 2][:, c0:512], car[:, c0:512], ALU.add, [B3[s % 2], car], [car])

                def act3(s):
                    d = steps[s]
                    t0, kp = kcol(d["slot"])
                    c0 = d["c0"]
                    act(Wt[s % 3][0:kp, c0:512], Tt[s % 2][0:kp, c0:512], AF.Exp, [Tt[s % 2]], [Wt[s % 3]])

                def pe3(s):
                    d = steps[s]
                    t0, kp = kcol(d["slot"])
                    c0 = d["c0"]
                    w_ = Wt[s % 3]
                    last = s == ns - 1
                    mm(PO[:, c0:512], VV_[0:kp, d["slot"], :], w_[0:kp, c0:512], False, last, [VV_, w_], [PO])
                    if not is_sb:
                        dd = da if s % 2 == 0 else da2
                        tt(dd[0:kp, c0:512], dd[0:kp, c0:512], w_[0:kp, c0:512], ALU.add, [dd, w_], [dd])

                if is_sb:
                    for t in range(ns + 2):
                        fox_ctr[0] += 1
                        if fox_ctr[0] % 5 == 0:
                            conv_steps(1, cstC32, cstC16, lookahead=2, on_act=False)
                        if t < ns:
                            pe1(t)
                            act1(t)
                        if 0 <= t - 1 < ns:
                            pe2(t - 1)
                            dve1(t - 1)
                            act3(t - 1)
                        if 0 <= t - 2 < ns:
                            pe3(t - 2)
                    act(ob[:], PO[:], AF.Copy, [PO], [ob])
                else:
                    for t in range(ns + 1):
                        fox_ctr[0] += 1
                        if fox_ctr[0] % 5 == 0:
                            conv_steps(1, cstC32, cstC16, lookahead=2, on_act=(fox_ctr[0] % 10 == 0))
                        if t < ns:
                            pe1(t)
                            act1(t)
                        if 0 <= t - 1 < ns:
                            pe3(t - 1)
                    tt(da[:], da[:], da2[:], ALU.add, [da, da2], [da])
                    cp(dhi[:], da[:], [da], [dhi])
                    tt(dtmp[:], da[:], dhi[:], ALU.subtract, [da, dhi], [dtmp])
                    cp(dlo[:], dtmp[:], [dtmp], [dlo])
                    mm(PD[:], onesb[:], dhi[:], True, False, [onesb, dhi], [PD])
                    mm(PD[:], onesb[:], dlo[:], False, True, [onesb, dlo], [PD])
                    S.op(V, lambda e: e.reciprocal(out=rden[:], in_=PD[:]), [PD], [rden])
                    tt(ob[:], PO[:], rden[:], ALU.mult, [PO, rden], [ob])
                S.dma(G, oT_s[h, :, q0:q0 + 512], ob[:], [ob], [oT_s], stream="ost%d" % ((nmt - 1) % 2))
        conv_steps(NHT, cstC32, cstC16, lookahead=2, on_act=True)
        conv_drain(cstC32, cstC16, on_act=True)
        S.flush()

    S.es = es
    skb = S.tile("skb", [128, 16, 128], BF16)
    with ExitStack() as es35:
        S.es = es35
        skl = S.tile("skl", [128, 16, 128], F32)
        S.dma(Q, skl[:], skT[:], [skT], [skl])
        cp(skb[:], skl[:], [skl], [skb])
        S.flush()
    with ExitStack() as es4:
        S.es = es4
        TC = 256
        NTB = TC // 128
        iota16 = S.tile("iota16", [128, 16], F32)
        cp(iota16[:], c32[:, 3, 0:16], [c32], [iota16])
        ol = [S.tile("ol%d" % i, [128, TC], F32) for i in range(3)]
        osq = [S.tile("osq%d" % i, [128, TC], BF16) for i in range(2)]
        gl = [S.tile("gl%d" % i, [128, TC], BF16) for i in range(2)]
        lnr = S.tile("lnr", [128, TC], F32)
        rsn = [S.tile("rsn%d" % i, [128, TC], F32) for i in range(2)]
        MT = S.tile("MT", [128, KC, TC], BF16)
        mtmp = S.tile("mtmp", [128, TC], F32)
        wos = [S.tile("wos%d" % i, [128, KC, 128], BF16) for i in range(2)]
        wot = [S.tile("wot%d" % i, [128, KC, 256], BF16) for i in range(1)] * 2
        xtl = [S.tile("xtl%d" % i, [128, TC], F32) for i in range(2)]
        hnT = S.tile("hnT", [128, KC, TC], BF16)
        H1 = S.tile("H1", [128, NTB, D], F32)
        xol = [S.tile("xol%d" % i, [128, 256], F32) for i in range(2)]
        qTt = S.tile("qTt", [128, 16, TC], BF16)
        junk = S.tile("junk", [128, D], BF16)
        ss2 = S.tile("ss2", [128, 4], F32)
        r2c = S.tile("r2c", [128, 4], F32)
        Ssc = S.tile("Ssc", [128, 16, 128], F32)
        scr = S.tile("scr", [128, 2048], F32)
        Ss2 = TkView(scr, lambda ap: ap.rearrange("p (a n) -> p a n", a=16))
        TOPV = S.tile("TOPV", [128, 16, 16], F32)
        TOPI = S.tile("TOPI", [128, 16, 16], U32)
        TOPF = S.tile("TOPF", [128, 16, 16], F32)
        CS = TkView(Ssc, lambda ap: ap.rearrange("p a n -> p (a n)").rearrange("p (h c) -> p h c", h=8))
        CS2 = TkView(scr, lambda ap: ap.rearrange("p (a n) -> p a n", a=8))
        BV = S.tile("BV", [128, 8, 16], F32)
        BJ = S.tile("BJ", [128, 8, 16], U32)
        K1 = S.tile("K1", [128, 8, 16], U32)
        K2 = S.tile("K2", [128, 8, 16], U32)
        K1f = S.tile("K1f", [128, 8, 16], F32)
        K2f = S.tile("K2f", [128, 8, 16], F32)
        OH = TkView(scr, lambda ap: ap.rearrange("p (a b c) -> p a b c", a=8, b=16))
        I0f = S.tile("I0f", [128, 8, 16], F32)
        I1f = S.tile("I1f", [128, 8, 16], F32)
        IDXf = S.tile("IDXf", [128, 128], F32)
        IDX = [S.tile("IDX%d" % i, [128, 128], I32) for i in range(2)]
        nbv = S.tile("nbv", [128, 8], F32)
        Eg = S.tile("Eg", [128, 8, 16], F32)
        Zg = S.tile("Zg", [128, 8], F32)
        Ag = S.tile("Ag", [128, 128], F32)
        Wg = S.tile("Wg", [128, 128], F32)
        NR = 7
        UR = [S.tile("UR%d" % i, [128, 2 * D], BF16) for i in range(NR)]
        Gs = S.tile("Gs", [128, 128], F32)
        dg = [S.tile("dg%d" % i, [128, 128], BF16) for i in range(4)]
        H1b = [H1, S.tile("H1b", [128, NTB, D], F32)]
        Egb = [Eg, S.tile("Egb", [128, 8, 16], F32)]
        junk2 = S.tile("junk2", [128, D], BF16)
        g2b = S.tile("g2b", [128, D], BF16)
        S.dma(Q, H1[:, 0, :], g2row[:], [g2row], [H1])
        cp(g2b[:], H1[:, 0, :], [H1], [g2b])
        HG = [S.tile("HG%d" % i, [128, D], BF16) for i in range(2)]
        nrow_c = [0]

        def chunk_work(tc):
            c0 = tc * TC
            H1_ = H1b[tc % 2]
            for grp in range(2):
                pss = ps[4 + grp]
                for hh in range(8):
                    h = grp * 8 + hh
                    o_ = ol[h % 3]
                    s_ = osq[h % 2]
                    S.dma(Q, o_[:], oT_s[h, :, c0:c0 + TC], [oT_s], [o_], stream="ol%d" % (h % 3))
                    act(s_[:], o_[:], AF.Square, [o_], [s_])
                    mm(pss[:, 0:TC], onesb[:], s_[:], hh == 0, hh == 7, [onesb, s_], [pss])
                    yield
                rstd_from(rsn[grp], rsn[grp][:], pss, pss[:, 0:TC], 1.0 / 1024, lnr, lnr[:])
                yield
            for h in range(16):
                o_ = ol[h % 3]
                S.dma(Q, o_[:], oT_s[h, :, c0:c0 + TC], [oT_s], [o_], stream="ol%d" % (h % 3))
                if h < 8:
                    stt(MT[:, h, :], o_[:], vec[:, 34 + h:35 + h], rsn[0][:], ALU.mult, ALU.mult, [o_, vec, rsn[0]], [MT])
                else:
                    g_ = gl[h % 2]
                    S.dma(Q, g_[:], gT_s[h - 8, :, c0:c0 + TC], [gT_s], [g_], stream="gl%d" % (h % 2))
                    stt(mtmp[:], o_[:], vec[:, 34 + h:35 + h], rsn[1][:], ALU.mult, ALU.mult, [o_, vec, rsn[1]], [mtmp])
                    tt(MT[:, h, :], mtmp[:], g_[:], ALU.mult, [mtmp, g_], [MT])
                yield
            for n in range(KC):
                w_ = wos[n % 2]
                S.dma(Q, w_[:], wo_fm[n], [wo_fm], [w_], stream="wos%d" % (n % 2))
                x_ = xtl[n % 2]
                S.dma(Q, x_[:], xT_own[n * 128:(n + 1) * 128, c0:c0 + TC], [xT_own], [x_], stream="xtl%d" % (n % 2))
                pp = ps[6 + n % 2]
                for kc in range(KC):
                    mm(pp[:, 0:TC], w_[:, kc, :], MT[:, kc, :], kc == 0, kc == KC - 1, [w_, MT], [pp])
                tt(hnT[:, n, :], pp[:, 0:TC], x_[:], ALU.add, [pp, x_], [hnT])
                yield
            for ng in range(8):
                w_ = wot[ng % 2]
                S.dma(Q, w_[:], wo_tm[ng], [wo_tm], [w_], stream="wot0")
                for tb in range(NTB):
                    x_ = xol[(ng * NTB + tb) % 2]
                    S.dma(Q, x_[:, 0:256], x_own[c0 + tb * 128:c0 + (tb + 1) * 128, ng * 256:(ng + 1) * 256], [x_own], [x_],
                          stream="xol%d" % ((ng * NTB + tb) % 2))
                    pp = ps[4 + (ng * NTB + tb) % 2]
                    for kc in range(KC):
                        mm(pp[:, 0:256], MT[:, kc, tb * 128:(tb + 1) * 128], w_[:, kc, :], kc == 0, kc == KC - 1, [MT, w_], [pp])
                    tt(H1_[:, tb, ng * 256:(ng + 1) * 256], pp[:, 0:256], x_[:, 0:256], ALU.add, [pp, x_], [H1_])
                yield
            for hp in range(16):
                w_ = wos[hp % 2]
                S.dma(Q, w_[:], wq_fm[hp], [wq_fm], [w_], stream="wos%d" % (hp % 2))
                pp = ps[6 + hp % 2]
                for kc in range(KC):
                    mm(pp[:, 0:TC], w_[:, kc, :], hnT[:, kc, :], kc == 0, kc == KC - 1, [w_, hnT], [pp])
                act(qTt[:, hp, :], pp[:, 0:TC], AF.Copy, [pp], [qTt])
                yield

        def prep_block(tc, tb):
            bi = tc * NTB + tb
            H1_, Eg_, idx_ = H1b[tc % 2], Egb[bi % 2], IDX[bi % 2]
            act(junk2[:], H1_[:, tb, :], AF.Square, [H1_], [junk2, ss2], accum=ss2[:, tb:tb + 1])
            rstd_from(r2c, r2c[:, tb:tb + 1], ss2, ss2[:, tb:tb + 1], 1.0 / D, lnr, lnr[:, 0:1])
            tt(HG[bi % 2][:], H1_[:, tb, :], g2b[:], ALU.mult, [H1_, g2b], [HG[bi % 2]])
            yield
            for b4 in range(4):
                pp = ps[4 + b4]
                for q4 in range(4):
                    hp = b4 * 4 + q4
                    mm(pp[:, q4 * 128:(q4 + 1) * 128], qTt[:, hp, tb * 128:(tb + 1) * 128], skb[:, hp, :], True, True,
                       [qTt, skb], [pp])
                act(Ssc[:, b4 * 4:b4 * 4 + 4, :], pp[:].rearrange("p (a n) -> p a n", a=4), AF.Copy,
                    [pp, r2c], [Ssc], scale=r2c[:, tb:tb + 1])
                yield
            for hp in range(16):
                S.op(V, lambda e, hp=hp: e.max(out=TOPV[:, hp, 0:8], in_=Ssc[:, hp, :]), [Ssc], [TOPV])
                S.op(V, lambda e, hp=hp: e.max_index(out=TOPI[:, hp, 0:8], in_max=TOPV[:, hp, 0:8], in_values=Ssc[:, hp, :]),
                     [Ssc, TOPV], [TOPI])
                S.op(V, lambda e, hp=hp: e.match_replace(out=Ss2[:, hp, :], in_to_replace=TOPV[:, hp, 0:8],
                                                         in_values=Ssc[:, hp, :], imm_value=-1e30), [Ssc, TOPV], [Ss2])
                S.op(V, lambda e, hp=hp: e.max(out=TOPV[:, hp, 8:16], in_=Ss2[:, hp, :]), [Ss2], [TOPV])
                S.op(V, lambda e, hp=hp: e.max_index(out=TOPI[:, hp, 8:16], in_max=TOPV[:, hp, 8:16], in_values=Ss2[:, hp, :]),
                     [Ss2, TOPV], [TOPI])
                yield
            cp(TOPF[:], TOPI[:], [TOPI], [TOPF])
            tv = TOPV[:].rearrange("p (h two) k -> p h two k", two=2)
            tf = TOPF[:].rearrange("p (h two) k -> p h two k", two=2)
            tt(CS[:].rearrange("p h (a b) -> p h a b", a=16),
               tv[:, :, 0, :].unsqueeze(3).to_broadcast([128, 8, 16, 16]),
               tv[:, :, 1, :].unsqueeze(2).to_broadcast([128, 8, 16, 16]), ALU.add, [TOPV], [CS])
            yield
            for hh in range(8):
                S.op(V, lambda e, hh=hh: e.max(out=BV[:, hh, 0:8], in_=CS[:, hh, :]), [CS], [BV])
                S.op(V, lambda e, hh=hh: e.max_index(out=BJ[:, hh, 0:8], in_max=BV[:, hh, 0:8], in_values=CS[:, hh, :]),
                     [CS, BV], [BJ])
                S.op(V, lambda e, hh=hh: e.match_replace(out=CS2[:, hh, :], in_to_replace=BV[:, hh, 0:8],
                                                         in_values=CS[:, hh, :], imm_value=-1e30), [CS, BV], [CS2])
                S.op(V, lambda e, hh=hh: e.max(out=BV[:, hh, 8:16], in_=CS2[:, hh, :]), [CS2], [BV])
                S.op(V, lambda e, hh=hh: e.max_index(out=BJ[:, hh, 8:16], in_max=BV[:, hh, 8:16], in_values=CS2[:, hh, :]),
                     [CS2, BV], [BJ])
                yield
            ts(K1[:], BJ[:], 4, ALU.logical_shift_right, [BJ], [K1])
            ts(K2[:], BJ[:], 15, ALU.bitwise_and, [BJ], [K2])
            cp(K1f[:], K1[:], [K1], [K1f])
            cp(K2f[:], K2[:], [K2], [K2f])
            yield
            iob = iota16[:].unsqueeze(1).unsqueeze(1).to_broadcast([128, 8, 16, 16])
            for (kf_, two, of_) in ((K1f, 0, I0f), (K2f, 1, I1f)):
                tt(OH[:], kf_[:].unsqueeze(3).to_broadcast([128, 8, 16, 16]), iob, ALU.is_equal, [kf_, iota16], [OH])
                yield
                tt(OH[:], OH[:], tf[:, :, two, :].unsqueeze(2).to_broadcast([128, 8, 16, 16]), ALU.mult, [OH, TOPF], [OH], eng=G)
                yield
                S.op(V, lambda e, of_=of_: e.tensor_reduce(out=of_[:], in_=OH[:], axis=AX.X, op=ALU.add), [OH], [of_])
                yield
            stt(IDXf[:], I0f[:].rearrange("p h k -> p (h k)"), 128.0, I1f[:].rearrange("p h k -> p (h k)"),
                ALU.mult, ALU.add, [I0f, I1f], [IDXf])
            cp(idx_[:], IDXf[:], [IDXf], [idx_])
            ts(nbv[:], BV[:, :, 0], -1.0, ALU.mult, [BV], [nbv])
            for hh in range(8):
                act(Eg_[:, hh, :], BV[:, hh, :], AF.Exp, [BV, nbv], [Eg_, Zg], bias=nbv[:, hh:hh + 1], accum=Zg[:, hh:hh + 1])
            S.op(V, lambda e: e.reciprocal(out=Zg[:], in_=Zg[:]), [Zg], [Zg])
            tt(Eg_[:], Eg_[:], Zg[:].unsqueeze(2).to_broadcast([128, 8, 16]), ALU.mult, [Eg_, Zg], [Eg_])
            yield

        def gather_block(tc, tb, filler):
            bi = tc * NTB + tb
            r0 = tc * TC + tb * 128
            H1_, Eg_, idx_ = H1b[tc % 2], Egb[bi % 2], IDX[bi % 2]
            egf = Eg_[:].rearrange("p h k -> p (h k)")
            rows = {}

            def second_half(sl):
                u_ = rows.pop(sl)
                act(Wg[:, sl:sl + 1], Gs[:, sl:sl + 1], AF.Copy, [Gs, Eg_], [Wg], scale=egf[:, sl:sl + 1])
                d_ = dg[sl % 4]
                act(d_[:], identb[:], AF.Copy, [identb, Wg], [d_], scale=Wg[:, sl:sl + 1])
                for n4 in range(4):
                    mm(ps[n4][:], d_[:], u_[:, D + n4 * 512:D + (n4 + 1) * 512], sl == 0, sl == 127, [d_, u_], [ps[n4]])

            for sl in range(128):
                u_ = UR[nrow_c[0] % NR]
                nrow_c[0] += 1
                rows[sl] = u_
                S.dma(G, None, None, [idx_, uv_b], [u_], stream=u_.name,
                      fn=lambda e, u_=u_, idx_=idx_, sl=sl: e.indirect_dma_start(
                          out=u_[:], out_offset=None, in_=uv_b[:],
                          in_offset=bass.IndirectOffsetOnAxis(ap=idx_[:, sl:sl + 1], axis=0)))
                stt(junk[:], u_[:, 0:D], 1.0, HG[bi % 2][:], ALU.mult, ALU.mult, [u_, HG[bi % 2]], [junk, Ag], accum=Ag[:, sl:sl + 1])
                act(Gs[:, sl:sl + 1], Ag[:, sl:sl + 1], AF.Gelu, [Ag, r2c], [Gs], scale=r2c[:, tb:tb + 1])
                if sl >= 1:
                    second_half(sl - 1)
                if sl >= 2:
                    next(filler, None)
            second_half(127)
            for _ in filler:
                pass
            for n4 in range(4):
                tt(H1_[:, tb, n4 * 512:(n4 + 1) * 512], ps[n4][:], H1_[:, tb, n4 * 512:(n4 + 1) * 512], ALU.add,
                   [ps[n4], H1_], [H1_])
            S.dma(Q, out_own[r0:r0 + 128, :], H1_[:, tb, :], [H1_], [out_own], stream="outst")

        blocks = [(tc, tb) for tc in range(TO // TC) for tb in range(NTB)]

        def filler_for(nxt):
            if nxt is None:
                return
            if nxt[1] == 0:
                yield from chunk_work(nxt[0])
            yield from prep_block(*nxt)

        for _ in filler_for(blocks[0]):
            pass
        for bi, (tc, tb) in enumerate(blocks):
            nxt = blocks[bi + 1] if bi + 1 < len(blocks) else None
            gather_block(tc, tb, filler_for(nxt))
        S.flush(final_streams=["outst"])

    S.es = es
    es.close()
    return nc


def make_in_maps(inputs, NJ):
    f = lambda a: np.ascontiguousarray(np.asarray(a, dtype=np.float32))
    x = f(inputs["x"])[0]
    S_ = x.shape[0]
    assert S_ == NCORE * NJ * 128
    meta = f(inputs["meta_tokens"])
    xT_all = np.ascontiguousarray(np.concatenate([meta, x], axis=0).T)
    w_in = f(inputs["w_in"])[0]
    w_out = f(inputs["w_out"])[0]
    w_q = f(inputs["peer_w_query"])[0]
    sk = f(inputs["peer_sub_keys"])[0]
    skT = np.ascontiguousarray(sk.reshape(16, 128, 128).transpose(2, 0, 1))
    u = f(inputs["peer_u"])[0]
    v = f(inputs["peer_v"])[0]
    vecs = np.zeros((128, 64), np.float32)
    vecs[:, 0:16] = f(inputs["norm_mix"])[0].reshape(16, 128).T
    vecs[:, 16:32] = f(inputs["norm_ffn"])[0].reshape(16, 128).T
    vecs[:, 32] = f(inputs["fox_q_gain"])[0]
    vecs[:, 33] = f(inputs["fox_k_gain"])[0]
    vecs[:, 34:42] = f(inputs["sb_out_gain"])[0].reshape(8, 128).T
    vecs[:, 42:50] = f(inputs["fox_out_gain"])[0].reshape(8, 128).T
    vecs[0:8, 50] = f(inputs["b_forget"])[0]
    g2row = np.ascontiguousarray(np.broadcast_to(f(inputs["norm_ffn"])[0][None, :], (128, D)))
    consts = np.zeros((128, 4, 128), np.float32)
    kk = np.arange(128)
    consts[:, 0, :] = np.where(kk[:, None] >= kk[None, :], -1.0, 0.0)
    consts[:, 1, :] = 1.0
    consts[:, 2, :] = np.eye(128, dtype=np.float32)
    consts[:, 3, :] = kk[None, :].astype(np.float32)
    maps = []
    for c in range(NCORE):
        blocks = [c + NCORE * j for j in range(NJ)]
        rows = np.concatenate([np.arange(b * 128, (b + 1) * 128) for b in blocks])
        x_own = np.ascontiguousarray(x[rows])
        xT_own = np.ascontiguousarray(x_own.T)
        mk = np.zeros((128, 2, 8, 128), np.float32)
        for i in range(8):
            if i > c:
                mk[:, :, i, :] = NEG
            elif i == c:
                mk[:, 0, i, :] = np.where(kk[:, None] < kk[None, :], 0.0, NEG)
                mk[:, 1, i, :] = np.where(kk[:, None] <= kk[None, :], 0.0, NEG)
        oh = np.zeros((1, 8), np.float32)
        oh[0, c] = 1.0
        maps.append({
            "xT_all": xT_all, "xT_own": xT_own, "x_own": x_own, "w_in": w_in, "w_out": w_out, "w_q": w_q,
            "skT": skT, "peer_u": u, "peer_v": v, "vecs": vecs, "g2row": g2row, "consts": consts,
            "maskadd": mk, "onehot_c": oh,
        })
    return maps


def assemble(results, NJ):
    S_ = NCORE * NJ * 128
    out = np.zeros((1, S_, D), np.float32)
    for c in range(NCORE):
        o = np.asarray(results[c]["out_own"], dtype=np.float32)
        for j in range(NJ):
            b = c + NCORE * j
            out[0, b * 128:(b + 1) * 128] = o[j * 128:(j + 1) * 128]
    return out


_NC_CACHE = {}


def kernel(**inputs):
    NJ = 16
    if NJ not in _NC_CACHE:
        _NC_CACHE[NJ] = build_nc(NJ)
    nc = _NC_CACHE[NJ]
    maps = make_in_maps(inputs, NJ)
    res = run_bass_kernel_spmd(nc, maps, core_ids=list(range(NCORE)))
    return assemble(res.results, NJ)
```

```python
import math
from contextlib import ExitStack

import numpy as np
import concourse.bass as bass
import concourse.mybir as mybir
from concourse.bass_utils import run_bass_kernel_spmd

F32 = mybir.dt.float32
BF16 = mybir.dt.bfloat16
I32 = mybir.dt.int32
U32 = mybir.dt.uint32
AF = mybir.ActivationFunctionType
ALU = mybir.AluOpType
AX = mybir.AxisListType

D = 2048
KC = 16
NCORE = 8
N_META = 16
EPS = 1e-6
SCALE = 1.0 / math.sqrt(128.0)
NEG = -30000.0
N_EXP = 16384


class Tk:
    def __init__(self, t, name=""):
        self.t = t
        self.name = name
        self.lw = None
        self.rd = []

    def __getitem__(self, k):
        return self.t[k]


class TkView:
    def __init__(self, base, fn):
        self.base = base
        self.fn = fn
        self.name = base.name

    def __getitem__(self, k):
        return self.fn(self.base.t[:])[k]

    @property
    def lw(self):
        return self.base.lw

    @lw.setter
    def lw(self, v):
        self.base.lw = v

    @property
    def rd(self):
        return self.base.rd

    @rd.setter
    def rd(self, v):
        self.base.rd = v


class Op:
    __slots__ = ("eng", "fn", "deps", "dma", "stream", "sidx", "awaited", "mile", "idx")

    def __init__(self, eng, fn, dma=False, stream=None):
        self.eng = eng
        self.fn = fn
        self.deps = []
        self.dma = dma
        self.stream = stream
        self.sidx = 0
        self.awaited = False
        self.mile = 0
        self.idx = 0


class Sched:
    ENGS = ("tensor", "vector", "scalar", "gpsimd", "sync")

    def __init__(self, nc, es):
        self.nc = nc
        self.es = es
        self.es0 = es
        self.ops = {e: [] for e in self.ENGS}
        self.streams = {}
        self.nops = 0

    def tile(self, name, shape, dt):
        return Tk(self.es.enter_context(self.nc.sbuf_tensor(name, list(shape), dt)), name)

    def psum(self, name, shape=(128, 512), dt=F32):
        return Tk(self.es.enter_context(self.nc.psum_tensor(name, list(shape), dt)), name)

    def dram(self, name, shape, dt, kind="Internal"):
        t = self.nc.dram_tensor(name, list(shape), dt, kind=kind)
        return Tk(t.ap(), name)

    def _add(self, op, reads, writes):
        deps = []
        for t in reads:
            if t.lw is not None:
                deps.append(t.lw)
        for t in writes:
            if t.lw is not None and (t.lw.dma or op.dma or t.lw.eng != op.eng):
                deps.append(t.lw)
            deps.extend(r for r in t.rd if r.dma or op.dma or r.eng != op.eng)
        seen = set()
        for d in deps:
            if d is op or id(d) in seen:
                continue
            seen.add(id(d))
            if (not d.dma) and d.eng == op.eng and op.eng == "tensor" and not op.dma:
                continue
            op.deps.append(d)
            d.awaited = True
        for t in reads:
            t.rd = [r for r in t.rd if r.dma or r.eng != op.eng or op.dma] + [op]
        for t in writes:
            t.lw = op
            t.rd = []
        op.idx = self.nops
        self.nops += 1
        self.ops[op.eng].append(op)
        return op

    def op(self, eng, fn, reads=(), writes=()):
        return self._add(Op(eng, fn), reads, writes)

    def dma(self, eng, out, in_, reads=(), writes=(), stream=None, fn=None):
        if stream is None:
            stream = "dma_" + (writes[0].name if writes else "x")
        if fn is None:
            fn = lambda e, out=out, in_=in_: e.dma_start(out=out, in_=in_)
        op = Op(eng, fn, dma=True, stream=stream)
        st = self.streams.setdefault(stream, [])
        if st:
            op.deps.append(st[-1])
            st[-1].awaited = True
        st.append(op)
        op.sidx = len(st)
        return self._add(op, reads, writes)

    def flush(self, final_streams=()):
        nc = self.nc
        if not hasattr(self, "prog"):
            self.prog = {e: self.es0.enter_context(nc.semaphore("prog_" + e)) for e in self.ENGS}
            self.ssem = {}
            self.mcount = {e: 0 for e in self.ENGS}
            self.waited = {e: {} for e in self.ENGS}
            self.first_flush = True
        prog, ssem = self.prog, self.ssem
        for s in self.streams:
            if s not in ssem:
                ssem[s] = self.es0.enter_context(nc.semaphore("s_" + s))
        for e in self.ENGS:
            comp = [o for o in self.ops[e] if not o.dma]
            if comp:
                comp[-1].awaited = True
            m = self.mcount[e]
            pending = []
            for o in comp:
                pending.append(o)
                if o.awaited:
                    m += 1
                    for p in pending:
                        p.mile = m
                    pending = []
            self.mcount[e] = m
        barrier = {}
        if not self.first_flush:
            for e in self.ENGS:
                if self.bar_m[e] > 0:
                    barrier[("p", e)] = self.bar_m[e]
            for s, n in self.bar_s.items():
                if n > 0:
                    barrier[("s", s)] = 16 * n
        streams = self.streams
        block = self.es.enter_context(nc.Block())

        def run(ename, eng):
            waited = self.waited[ename]

            def do_wait(key, val):
                if waited.get(key, 0) >= val:
                    return
                waited[key] = val
                sem = ssem[key[1]] if key[0] == "s" else prog[key[1]]
                eng.wait_ge(sem, val)

            for key, val in barrier.items():
                do_wait(key, val)
            for o in self.ops[ename]:
                need = {}
                for d in o.deps:
                    if d.dma:
                        key = ("s", d.stream)
                        val = 16 * d.sidx
                    else:
                        key = ("p", d.eng)
                        val = d.mile
                    if val > need.get(key, 0):
                        need[key] = val
                for key, val in need.items():
                    do_wait(key, val)
                ins = o.fn(eng)
                if o.dma:
                    ins.then_inc(ssem[o.stream], 16)
                elif o.awaited:
                    ins.then_inc(prog[ename], 1)
            if ename == "sync":
                for s in final_streams:
                    eng.wait_ge(ssem[s], 16 * len(streams[s]))

        @block.tensor
        def _(e):
            run("tensor", e)

        @block.vector
        def _(e):
            run("vector", e)

        @block.scalar
        def _(e):
            run("scalar", e)

        @block.gpsimd
        def _(e):
            run("gpsimd", e)

        @block.sync
        def _(e):
            run("sync", e)

        self.bar_m = dict(self.mcount)
        self.bar_s = {s: len(v) for s, v in self.streams.items()}
        self.first_flush = False
        self.ops = {e: [] for e in self.ENGS}


def build_nc(NJ, debug=False):
    NXB = NCORE * NJ
    NKB = NXB + 1
    T_ALL = N_META + NXB * 128
    TO = NJ * 128
    NMT = NJ // 4
    assert NJ % 4 == 0

    nc = bass.Bass("TRN2", target_bir_lowering=False)
    es = ExitStack()
    es.enter_context(nc.allow_low_precision("bf16 matmul operands by design; fp32 accumulation"))
    S = Sched(nc, es)

    def ext(name, shape, dt=F32, kind="ExternalInput"):
        return Tk(nc.dram_tensor(name, list(shape), dt, kind=kind).ap(), name)

    xT_all = ext("xT_all", [D, T_ALL])
    xT_own = ext("xT_own", [D, TO])
    x_own = ext("x_own", [TO, D])
    w_in = ext("w_in", [D, 7176])
    w_out = ext("w_out", [D, D])
    w_q = ext("w_q", [D, D])
    skT = ext("skT", [128, 16, 128])
    u_t = ext("peer_u", [N_EXP, D])
    v_t = ext("peer_v", [N_EXP, D])
    vecs = ext("vecs", [128, 64])
    g2row = ext("g2row", [128, D])
    consts = ext("consts", [128, 4, 128])
    maskadd = ext("maskadd", [128, 2, 8, 128])
    onehot_c = ext("onehot_c", [1, 8])
    out_own = ext("out_own", [TO, D], kind="ExternalOutput")

    kT_s = S.dram("kT_s", [16, 128, T_ALL], BF16)
    v_s = S.dram("v_s", [16, 128, NKB, 128], BF16)
    y0_s = S.dram("y0_s", [8, T_ALL], F32)
    qT_s = S.dram("qT_s", [16, 128, TO], BF16)
    gT_s = S.dram("gT_s", [8, 128, TO], BF16)
    oT_s = S.dram("oT_s", [16, 128, TO], F32)
    uv_b = S.dram("uv_b", [N_EXP, 2 * D], BF16)
    wo_fm = S.dram("wo_fm", [16, 128, KC, 128], BF16)
    wo_tm = S.dram("wo_tm", [8, 128, KC, 256], BF16)
    wq_fm = S.dram("wq_fm", [16, 128, KC, 128], BF16)

    ps = [S.psum("ps%d" % i) for i in range(8)]

    V, A, P, G, Q = "vector", "scalar", "tensor", "gpsimd", "sync"

    def act(out, in_, func, reads, writes, bias=None, scale=None, accum=None, eng=A):
        kw = {}
        if bias is not None:
            kw["bias"] = bias
        if scale is not None:
            kw["scale"] = scale
        if accum is not None:
            kw["accum_out"] = accum
        return S.op(eng, lambda e: e.activation(out=out, in_=in_, func=func, **kw), reads, writes)

    def tt(out, in0, in1, op, reads, writes, eng=V):
        return S.op(eng, lambda e: e.tensor_tensor(out=out, in0=in0, in1=in1, op=op), reads, writes)

    def ts(out, in0, s1, op0, reads, writes, s2=None, op1=None, eng=V):
        if op1 is None:
            return S.op(eng, lambda e: e.tensor_scalar(out=out, in0=in0, scalar1=s1, scalar2=None, op0=op0), reads, writes)
        return S.op(eng, lambda e: e.tensor_scalar(out=out, in0=in0, scalar1=s1, scalar2=s2, op0=op0, op1=op1), reads, writes)

    def stt(out, in0, scalar, in1, op0, op1, reads, writes, accum=None):
        if accum is None:
            return S.op(V, lambda e: e.scalar_tensor_tensor(out=out, in0=in0, scalar=scalar, in1=in1, op0=op0, op1=op1), reads, writes)
        return S.op(V, lambda e: e.scalar_tensor_tensor(out=out, in0=in0, scalar=scalar, in1=in1, op0=op0, op1=op1, accum_out=accum), reads, writes)

    def cp(out, in_, reads, writes, eng=V):
        return S.op(eng, lambda e: e.tensor_copy(out=out, in_=in_), reads, writes)

    def mm(out, lhsT, rhs, start, stop, reads, writes):
        return S.op(P, lambda e: e.matmul(out, lhsT, rhs, start=start, stop=stop), reads, writes)

    def memset(ap, val, writes, eng=V):
        return S.op(eng, lambda e: e.memset(ap, val), (), writes)

    def rstd_from(out_t, out_ap, ps_t, ps_ap, inv_n, tmp_t, tmp_ap):
        act(tmp_ap, ps_ap, AF.Ln, [ps_t], [tmp_t], bias=EPS, scale=inv_n)
        act(out_ap, tmp_ap, AF.Exp, [tmp_t], [out_t], scale=-0.5)

    c32 = S.tile("c32", [128, 4, 128], F32)
    S.dma(Q, c32[:], consts[:], [consts], [c32])
    negtri = S.tile("negtri", [128, 128], BF16)
    onesb = S.tile("onesb", [128, 128], BF16)
    identb = S.tile("identb", [128, 128], BF16)
    zerob = S.tile("zerob", [128, 512], BF16)
    cp(negtri[:], c32[:, 0, :], [c32], [negtri])
    cp(onesb[:], c32[:, 1, :], [c32], [onesb])
    cp(identb[:], c32[:, 2, :], [c32], [identb])
    memset(zerob[:], 0.0, [zerob])
    vec = S.tile("vec", [128, 64], F32)
    S.dma(Q, vec[:], vecs[:], [vecs], [vec])
    vec2 = S.tile("vec2", [128, 2], F32)
    ts(vec2[:, 0:1], vec[:, 32:33], SCALE, ALU.mult, [vec], [vec2])
    ts(vec2[:, 1:2], vec[:, 50:51], -1.0, ALU.mult, [vec], [vec2])
    maskb = S.tile("maskb", [128, 2, 8, 128], BF16)
    ohc = S.tile("ohc", [128, 8], F32)
    S.dma(Q, ohc[64:65, :], onehot_c[:], [onehot_c], [ohc])
    ncf_cols = S.tile("ncf_cols", [128, 8, NKB], F32)
    cmid = S.tile("cmid", [128, 8, NJ], F32)
    cfull = S.tile("cfull", [128, 8, NJ], F32)

    NHT = 4 * (N_EXP // 128)
    conv_state = [0, 0]

    def conv_load(cst32):
        n_ = conv_state[0]
        if n_ >= NHT:
            return
        conv_state[0] += 1
        src = u_t if n_ < NHT // 2 else v_t
        rt = (n_ % (NHT // 2)) // 2
        hf = n_ % 2
        a = cst32[n_ % len(cst32)]
        S.dma(Q, a[:], src[rt * 128:(rt + 1) * 128, hf * 1024:(hf + 1) * 1024], [src], [a], stream=a.name)

    g2_tile = [None]

    def conv_finish(cst32, cst16, on_act=False):
        n_ = conv_state[1]
        if n_ >= conv_state[0]:
            return
        conv_state[1] += 1
        coff = 0 if n_ < NHT // 2 else D
        rt = (n_ % (NHT // 2)) // 2
        hf = n_ % 2
        a, b = cst32[n_ % len(cst32)], cst16[n_ % len(cst16)]
        if on_act:
            act(b[:], a[:], AF.Copy, [a], [b])
        else:
            cp(b[:], a[:], [a], [b])
        S.dma(Q, uv_b[rt * 128:(rt + 1) * 128, coff + hf * 1024:coff + (hf + 1) * 1024], b[:], [b], [uv_b], stream=b.name)

    def conv_steps(k, cst32, cst16, lookahead=3, on_act=False):
        for _ in range(k):
            while conv_state[0] < min(NHT, conv_state[1] + lookahead):
                conv_load(cst32)
            conv_finish(cst32, cst16, on_act)

    def conv_drain(cst32, cst16, on_act=False):
        while conv_state[1] < conv_state[0]:
            conv_finish(cst32, cst16, on_act)

    def kcol(slot):
        if slot == 0:
            return 0, N_META
        return N_META + 128 * (slot - 1), 128

    with ExitStack() as es0:
        S.es = es0
        m32 = S.tile("m32", [128, 2, 8, 128], F32)
        S.dma(Q, m32[:], maskadd[:], [maskadd], [m32])
        cp(maskb[:], m32[:], [m32], [maskb])
        S.flush()

    with ExitStack() as es1:
        S.es = es1
        GK = 256
        wk = S.tile("wk", [128, KC, 2048], BF16)
        wf = S.tile("wf", [128, KC, 8], BF16)
        wld = [S.tile("wldk%d" % i, [128, 1024], F32) for i in range(2)]
        n = 0
        for (dst, coff, scol) in ((wk, 0, 1024), (wk, 1024, 4096)):
            for kc in range(KC):
                a = wld[n % 2]
                S.dma(Q, a[:], w_in[kc * 128:(kc + 1) * 128, scol:scol + 1024], [w_in], [a])
                if n % 2:
                    ts(dst[:, kc, coff:coff + 1024], a[:], vec[:, kc:kc + 1], ALU.mult, [a, vec], [dst])
                else:
                    act(dst[:, kc, coff:coff + 1024], a[:], AF.Copy, [a, vec], [dst], scale=vec[:, kc:kc + 1])
                n += 1
        wfl = S.tile("wfl", [128, KC, 8], F32)
        S.dma(Q, wfl[:], None, [w_in], [wfl],
              fn=lambda e: e.dma_start(out=wfl[:], in_=w_in[:, 7168:7176].rearrange("(kc p) c -> p kc c", p=128)))
        for kc in range(KC):
            ts(wf[:, kc, :], wfl[:, kc, :], vec[:, kc:kc + 1], ALU.mult, [wfl, vec], [wf])
        xs = [S.tile("xsk%d" % i, [128, KC, GK], F32) for i in range(2)]
        xbk2 = [S.tile("xbk%d" % i, [128, KC, GK], BF16) for i in range(2)]
        sqx2 = [S.tile("sqx%d" % i, [128, KC, GK], BF16) for i in range(2)]
        lnt = S.tile("lnt", [128, GK], F32)
        lnt2 = [S.tile("lnt2_%d" % i, [128, GK], F32) for i in range(2)]
        rsk = S.tile("rsk", [128, GK], F32)
        kst2 = [S.tile("kstk%d" % i, [128, 16, GK], BF16) for i in range(2)]
        kf = [S.tile("kf%d" % i, [128, GK], F32) for i in range(2)]
        sqk = [S.tile("sqk%d" % i, [128, GK], BF16) for i in range(2)]
        rk = [S.tile("rk%d" % i, [128, GK], F32) for i in range(2)]
        yst = [S.tile("yst%d" % i, [8, GK], F32) for i in range(2)]
        xTv = xT_all[:].rearrange("(kc p) t -> p kc t", p=128)
        groups = [(0, N_META)] + [(N_META + GK * i, GK) for i in range(NXB * 128 // GK)]
        def k_prologue(gi):
            t0, Gn = groups[gi]
            x_ = xs[gi % 2]
            S.dma(Q, x_[:, :, 0:Gn], xTv[:, :, t0:t0 + Gn], [xT_all], [x_], stream="xsk%d" % (gi % 2))
            cp(xbk2[gi % 2][:, :, 0:Gn], x_[:, :, 0:Gn], [x_], [xbk2[gi % 2]], eng=G)
            act(sqx2[gi % 2][:, :, 0:Gn], x_[:, :, 0:Gn], AF.Square, [x_], [sqx2[gi % 2]])

        k_prologue(0)
        for gi, (t0, Gn) in enumerate(groups):
            xbk, sqx, kst = xbk2[gi % 2], sqx2[gi % 2], kst2[gi % 2]
            for kc in range(KC):
                mm(ps[0][:, 0:Gn], onesb[:], sqx[:, kc, 0:Gn], kc == 0, kc == KC - 1, [onesb, sqx], [ps[0]])
            rstd_from(rsk, rsk[:, 0:Gn], ps[0], ps[0][:, 0:Gn], 1.0 / D, lnt, lnt[:, 0:Gn])
            for kc in range(KC):
                mm(ps[1][0:8, 0:Gn], wf[:, kc, :], xbk[:, kc, 0:Gn], kc == 0, kc == KC - 1, [wf, xbk], [ps[1]])
            ys = yst[gi % 2]
            tt(ys[:, 0:Gn], ps[1][0:8, 0:Gn], rsk[0:8, 0:Gn], ALU.mult, [ps[1], rsk], [ys])
            S.dma(A, y0_s[:, t0:t0 + Gn], ys[:, 0:Gn], [ys], [y0_s], stream="yst%d" % (gi % 2))
            if gi + 1 < len(groups):
                k_prologue(gi + 1)

            def fox_norm(h):
                kf_, sqk_, rk_ = kf[h % 2], sqk[h % 2], rk[h % 2]
                pn = ps[6 + h % 2]
                mm(pn[:, 0:Gn], onesb[:], sqk_[:, 0:Gn], True, True, [onesb, sqk_], [pn])
                rstd_from(rk_, rk_[:, 0:Gn], pn, pn[:, 0:Gn], 1.0 / 128, lnt2[h % 2], lnt2[h % 2][:, 0:Gn])
                stt(kst[:, h, 0:Gn], kf_[:, 0:Gn], vec[:, 33:34], rk_[:, 0:Gn], ALU.mult, ALU.mult, [kf_, vec, rk_], [kst])

            order = [8, 9, 0, 10, 1, 11, 2, 12, 3, 13, 4, 14, 5, 15, 6, 7]
            pend = []
            for oi, h in enumerate(order):
                pk = ps[2 + oi % 4]
                for kc in range(KC):
                    mm(pk[:, 0:Gn], wk[:, kc, h * 128:(h + 1) * 128], xbk[:, kc, 0:Gn], kc == 0, kc == KC - 1, [wk, xbk], [pk])
                if h < 8:
                    tt(kst[:, h, 0:Gn], pk[:, 0:Gn], rsk[:, 0:Gn], ALU.mult, [pk, rsk], [kst])
                else:
                    kf_, sqk_ = kf[h % 2], sqk[h % 2]
                    tt(kf_[:, 0:Gn], pk[:, 0:Gn], rsk[:, 0:Gn], ALU.mult, [pk, rsk], [kf_])
                    act(sqk_[:, 0:Gn], kf_[:, 0:Gn], AF.Square, [kf_], [sqk_])
                    pend.append((oi, h))
                while pend and pend[0][0] <= oi - 1:
                    fox_norm(pend.pop(0)[1])
            while pend:
                fox_norm(pend.pop(0)[1])
            S.dma(A, kT_s[:, :, t0:t0 + Gn].rearrange("h d t -> d h t"), kst[:, :, 0:Gn], [kst], [kT_s], stream="kstk%d" % (gi % 2))
        S.flush()

    with ExitStack() as es1v:
        S.es = es1v
        wv = S.tile("wv", [128, KC, 2048], BF16)
        stg32 = [S.tile("stg32_%d" % i, [128, 1024], F32) for i in range(2)]
        wld = stg32
        n = 0
        for (dst, coff, scol) in ((wv, 0, 2048), (wv, 1024, 5120)):
            for kc in range(KC):
                a = wld[n % 2]
                S.dma(Q, a[:], w_in[kc * 128:(kc + 1) * 128, scol:scol + 1024], [w_in], [a], stream="stg32_%d" % (n % 2))
                if n % 2:
                    ts(dst[:, kc, coff:coff + 1024], a[:], vec[:, kc:kc + 1], ALU.mult, [a, vec], [dst])
                else:
                    act(dst[:, kc, coff:coff + 1024], a[:], AF.Copy, [a, vec], [dst], scale=vec[:, kc:kc + 1])
                n += 1
        xs = [S.tile("xs%d" % i, [128, KC, 128], F32) for i in range(3)]
        xb = [S.tile("xb%d" % i, [128, KC, 128], BF16) for i in range(2)]
        sq = [S.tile("sq%d" % i, [128, KC, 128], BF16) for i in range(2)]
        lntv = [S.tile("lntv%d" % i, [128, 1], F32) for i in range(2)]
        rcol = [S.tile("rcol%d" % i, [128, 1], F32) for i in range(2)]
        vst = [S.tile("vst%d" % i, [128, 2048], BF16) for i in range(2)]
        cstB32 = [S.tile("cstB32_%d" % i, [128, 1024], F32) for i in range(4)]
        cstB16 = [S.tile("cstB16_%d" % i, [128, 1024], BF16) for i in range(2)]

        wjobs = [(src, dst, gcol, kc, hf) for (src, dst, gcol) in ((w_out, wo_fm, None), (w_q, wq_fm, 16))
                 for kc in range(KC) for hf in range(2)]
        wstate = [0, 0]

        def w_load():
            n_ = wstate[0]
            if n_ >= len(wjobs):
                return
            wstate[0] += 1
            src, dst, gcol, kc, hf = wjobs[n_]
            a = cstB32[n_ % 4]
            S.dma(Q, a[:], src[kc * 128:(kc + 1) * 128, hf * 1024:(hf + 1) * 1024], [src], [a], stream=a.name)

        def w_finish():
            n_ = wstate[1]
            if n_ >= wstate[0]:
                return
            wstate[1] += 1
            src, dst, gcol, kc, hf = wjobs[n_]
            a, b = cstB32[n_ % 4], cstB16[n_ % 2]
            if gcol is None:
                cp(b[:], a[:], [a], [b])
            else:
                ts(b[:], a[:], vec[:, gcol + kc:gcol + kc + 1], ALU.mult, [a, vec], [b])
            S.dma(Q, dst[hf * 8:(hf + 1) * 8, :, kc, :].rearrange("n p c -> p n c"),
                  b[:].rearrange("p (n c) -> p n c", c=128), [b], [dst], stream=b.name)
            if gcol is None:
                S.dma(Q, wo_tm[hf * 4:(hf + 1) * 4, :, kc, :].rearrange("g p c -> p g c"),
                      b[:].rearrange("p (g c) -> p g c", c=256), [b], [wo_tm], stream=b.name + "t")

        def w_step():
            while wstate[0] < min(len(wjobs), wstate[1] + 3):
                w_load()
            w_finish()

        def v_prologue(slot):
            t0, Gn = kcol(slot)
            x_ = xs[slot % 3]
            S.dma(Q, x_[:, :, 0:Gn], xTv[:, :, t0:t0 + Gn], [xT_all], [x_], stream="xs%d" % (slot % 3))
            cp(xb[slot % 2][:, :, 0:Gn], x_[:, :, 0:Gn], [x_], [xb[slot % 2]], eng=G)
            act(sq[slot % 2][:, :, 0:Gn], x_[:, :, 0:Gn], AF.Square, [x_], [sq[slot % 2]])

        v_prologue(0)
        for slot in range(NKB):
            t0, Gn = kcol(slot)
            b2 = slot % 2
            xb_, sq_, rc_, vs_ = xb[b2], sq[b2], rcol[b2], vst[b2]
            for kc in range(KC):
                mm(ps[b2][0:Gn, 0:1], sq_[:, kc, 0:Gn], onesb[:, 0:1], kc == 0, kc == KC - 1, [onesb, sq_], [ps[b2]])
            rstd_from(rc_, rc_[0:Gn, :], ps[b2], ps[b2][0:Gn, 0:1], 1.0 / D, lntv[b2], lntv[b2][0:Gn, 0:1])
            if slot + 1 < NKB:
                v_prologue(slot + 1)
            if slot % 2 == 0:
                w_step()
            for cg in range(4):
                pv = ps[2 + (slot * 4 + cg) % 6]
                for kc in range(KC):
                    mm(pv[0:Gn, :], xb_[:, kc, 0:Gn], wv[:, kc, cg * 512:(cg + 1) * 512], kc == 0, kc == KC - 1, [xb_, wv], [pv])
                act(vs_[0:Gn, cg * 512:(cg + 1) * 512], pv[0:Gn, :], AF.Copy, [pv, rc_], [vs_], scale=rc_[0:Gn, 0:1])
            S.dma(A, v_s[:, 0:Gn, slot, :].rearrange("h t d -> t h d"), vs_[0:Gn, :].rearrange("t (h d) -> t h d", h=16),
                  [vs_], [v_s], stream="vst%d" % b2)
        while wstate[1] < len(wjobs):
            w_step()
        S.flush()

    with ExitStack() as es2:
        S.es = es2
        wqg = S.tile("wqg", [128, KC, 3072], BF16)
        wld = [S.tile("wld2_%d" % i, [128, 1024], F32) for i in range(2)]
        n = 0
        for (coff, scol) in ((0, 0), (1024, 3072), (2048, 6144)):
            for kc in range(KC):
                a = wld[n % 2]
                S.dma(Q, a[:], w_in[kc * 128:(kc + 1) * 128, scol:scol + 1024], [w_in], [a])
                if n % 2:
                    ts(wqg[:, kc, coff:coff + 1024], a[:], vec[:, kc:kc + 1], ALU.mult, [a, vec], [wqg])
                else:
                    act(wqg[:, kc, coff:coff + 1024], a[:], AF.Copy, [a, vec], [wqg], scale=vec[:, kc:kc + 1])
                n += 1
        x2 = S.tile("x2", [128, KC, 512], F32)
        xb2 = S.tile("xb2", [128, KC, 512], BF16)
        sq2 = S.tile("sq2", [128, KC, 512], BF16)
        ln2 = S.tile("ln2", [128, 512], F32)
        rs2 = S.tile("rs2", [128, 512], F32)
        qf = [S.tile("qf%d" % i, [128, 512], F32) for i in range(2)]
        qsq = [S.tile("qsq%d" % i, [128, 512], BF16) for i in range(2)]
        rq = [S.tile("rq%d" % i, [128, 512], F32) for i in range(2)]
        qst = [S.tile("qst%d" % i, [128, 512], BF16) for i in range(3)]
        xTo = xT_own[:].rearrange("(kc p) t -> p kc t", p=128)
        for gi in range(TO // 512):
            c0 = gi * 512
            S.dma(Q, x2[:], xTo[:, :, c0:c0 + 512], [xT_own], [x2])
            cp(xb2[:], x2[:], [x2], [xb2], eng=G)
            act(sq2[:], x2[:], AF.Square, [x2], [sq2])
            for kc in range(KC):
                mm(ps[0][:], onesb[:], sq2[:, kc, :], kc == 0, kc == KC - 1, [onesb, sq2], [ps[0]])
            rstd_from(rs2, rs2[:], ps[0], ps[0][:], 1.0 / D, ln2, ln2[:])
            for cc in range(24):
                pq = ps[2 + cc % 4]
                for kc in range(KC):
                    mm(pq[:], wqg[:, kc, cc * 128:(cc + 1) * 128], xb2[:, kc, :], kc == 0, kc == KC - 1, [wqg, xb2], [pq])
                o_ = qst[cc % 3]
                if cc < 8:
                    stt(o_[:], pq[:], SCALE, rs2[:], ALU.mult, ALU.mult, [pq, rs2], [o_])
                    S.dma(A, qT_s[cc, :, c0:c0 + 512], o_[:], [o_], [qT_s], stream="qst%d" % (cc % 3))
                elif cc < 16:
                    f_, s_, r_ = qf[cc % 2], qsq[cc % 2], rq[cc % 2]
                    tt(f_[:], pq[:], rs2[:], ALU.mult, [pq, rs2], [f_])
                    act(s_[:], f_[:], AF.Square, [f_], [s_])
                    mm(ps[1][:], onesb[:], s_[:], True, True, [onesb, s_], [ps[1]])
                    rstd_from(r_, r_[:], ps[1], ps[1][:], 1.0 / 128, ln2, ln2[:])
                    stt(o_[:], f_[:], vec2[:, 0:1], r_[:], ALU.mult, ALU.mult, [f_, vec2, r_], [o_])
                    S.dma(A, qT_s[cc, :, c0:c0 + 512], o_[:], [o_], [qT_s], stream="qst%d" % (cc % 3))
                else:
                    f_ = qf[cc % 2]
                    tt(f_[:], pq[:], rs2[:], ALU.mult, [pq, rs2], [f_])
                    act(f_[:], f_[:], AF.Exp, [f_], [f_], scale=-1.0)
                    ts(f_[:], f_[:], 1.0, ALU.add, [f_], [f_])
                    S.op(V, lambda e, o=o_, f=f_: e.reciprocal(out=o[:], in_=f[:]), [f_], [o_])
                    S.dma(A, gT_s[cc - 16, :, c0:c0 + 512], o_[:], [o_], [gT_s], stream="qst%d" % (cc % 3))
        S.flush()

    with ExitStack() as es2b:
        S.es = es2b
        yl = S.tile("yl", [8, T_ALL], F32)
        ncf = S.tile("ncf", [8, T_ALL], F32)
        one8 = S.tile("one8", [8, 1], F32)
        id32 = S.tile("id32", [8, 8], F32)
        memset(one8[:], 1.0, [one8])
        cp(id32[:], c32[0:8, 2, 0:8], [c32], [id32])
        S.dma(Q, yl[:], y0_s[:], [y0_s], [yl])
        act(yl[:], yl[:], AF.Exp, [yl, vec2], [yl], bias=vec2[0:8, 1:2], scale=-1.0)
        act(yl[:], yl[:], AF.Ln, [yl], [yl], bias=1.0)
        CH = 2048
        pos = 0
        while pos < T_ALL:
            n_ = min(CH, T_ALL - pos)
            init = 0.0 if pos == 0 else ncf[:, pos - 1:pos]
            S.op(V, lambda e, pos=pos, n_=n_, init=init: e.tensor_tensor_scan(
                out=ncf[:, pos:pos + n_], data0=one8[:, 0:1].to_broadcast([8, n_]), data1=yl[:, pos:pos + n_],
                initial=init, op0=ALU.mult, op1=ALU.add), [yl, one8, ncf], [ncf])
            pos += n_
        for s0 in range(0, NKB, 64):
            ns = min(64, NKB - s0)
            pt = ps[(s0 // 64) % 2]
            for si in range(ns):
                t0, Gn = kcol(s0 + si)
                S.op(P, lambda e, pt=pt, si=si, t0=t0, Gn=Gn: e.transpose(pt[0:Gn, si * 8:si * 8 + 8], ncf[0:8, t0:t0 + Gn], id32[:]),
                     [ncf, id32], [pt])
            if s0 == 0:
                cp(ncf_cols[0:16, :, 0:1].rearrange("p h s -> p s h"), pt[0:16, 0:8].rearrange("p (s h) -> p s h", h=8),
                   [pt], [ncf_cols])
                cp(ncf_cols[:, :, 1:ns].rearrange("p h s -> p s h"), pt[:, 8:ns * 8].rearrange("p (s h) -> p s h", h=8),
                   [pt], [ncf_cols])
            else:
                cp(ncf_cols[:, :, s0:s0 + ns].rearrange("p h s -> p s h"), pt[:, 0:ns * 8].rearrange("p (s h) -> p s h", h=8),
                   [pt], [ncf_cols])
        tmpc = S.tile("tmpc", [128, 8, NJ, 8], F32)
        tt(tmpc[64:65], ncf_cols[64:65, :, 1:1 + NXB].rearrange("p h (j c) -> p h j c", c=8),
           ohc[64:65, :].unsqueeze(1).unsqueeze(1).to_broadcast([1, 8, NJ, 8]), ALU.mult, [ncf_cols, ohc], [tmpc])
        S.op(V, lambda e: e.tensor_reduce(out=cmid[64:65], in_=tmpc[64:65], axis=AX.X, op=ALU.add), [tmpc], [cmid])
        mm(ps[2][:, 0:8 * NJ], c32[64:65, 1, :], cmid[64:65].rearrange("p h j -> p (h j)"), True, True, [c32, cmid], [ps[2]])
        cp(cfull[:].rearrange("p h j -> p (h j)"), ps[2][:, 0:8 * NJ], [ps[2]], [cfull])
        S.flush()

    with ExitStack() as es3:
        S.es = es3
        KT = [S.tile("KT%d" % i, [128, T_ALL], BF16) for i in range(2)]
        VV = [S.tile("VV%d" % i, [128, NKB, 128], BF16) for i in range(2)]
        QT = [S.tile("QT%d" % i, [128, TO], BF16) for i in range(2)]
        crow = [S.tile("crow%d" % i, [128, NJ, 128], BF16) for i in range(2)]
        Et = [S.tile("Et%d" % i, [128, 512], F32) for i in range(2)]
        Lt = [S.tile("Lt%d" % i, [128, 512], BF16) for i in range(2)]
        Tt = [S.tile("Tt%d" % i, [128, 512], F32) for i in range(2)]
        Wt = [S.tile("Wt%d" % i, [128, 512], BF16) for i in range(3)]
        carry = [S.tile("carry%d" % i, [128, 512], F32) for i in range(2)]
        osb = [S.tile("osb%d" % i, [128, 512], F32) for i in range(2)]
        rden = S.tile("rden", [128, 512], F32)
        dacc = [S.tile("dacc%d" % i, [128, 512], F32) for i in range(4)]
        dhi = S.tile("dhi", [128, 512], BF16)
        dlo = S.tile("dlo", [128, 512], BF16)
        dtmp = S.tile("dtmp", [128, 512], F32)
        B1 = [ps[0], ps[1]]
        B2 = [ps[2], ps[3]]
        B3 = [ps[4], ps[5]]
        PO, PD = ps[6], ps[7]

        cstC32 = [S.tile("cstC32_%d" % i, [128, 1024], F32) for i in range(2)]
        cstC16 = [S.tile("cstC16_%d" % i, [128, 1024], BF16) for i in range(2)]
        fox_ctr = [0]

        def steps_for(m):
            st = []
            for g in range(4 * m + 3, -1, -1):
                r = max(0, g - 4 * m)
                for i in range(7, -1, -1):
                    st.append(dict(slot=1 + 8 * g + i, c0=128 * r, mask=(i if g >= 4 * m else None)))
            st.append(dict(slot=0, c0=0, mask=None))
            return st

        nmt = 0
        for h in range(16):
            hb = h % 2
            KT_, VV_, QT_ = KT[hb], VV[hb], QT[hb]
            is_sb = h < 8
            S.dma(Q, KT_[:], kT_s[h], [kT_s], [KT_])
            S.dma(Q, VV_[:], v_s[h], [v_s], [VV_])
            S.dma(Q, QT_[:], qT_s[h], [qT_s], [QT_])
            cr_ = crow[hb]
            if not is_sb:
                hf = h - 8
                ts(cr_[:], cfull[:, hf, :].unsqueeze(2).to_broadcast([128, NJ, 128]), -1.0 / 128, ALU.mult, [cfull], [cr_])
            for m in range(NMT):
                steps = steps_for(m)
                ns = len(steps)
                car = carry[nmt % 2]
                ob = osb[nmt % 2]
                nmt += 1
                q0 = 512 * m
                if is_sb:
                    memset(car[:], 0.0, [car], eng=G)
                mm(PO[:], zerob[:, 0:128], zerob[:], True, False, [zerob], [PO])
                da = dacc[nmt % 2]
                da2 = dacc[2 + nmt % 2]
                if not is_sb:
                    memset(da[:], 0.0, [da], eng=G)
                    memset(da2[:], 0.0, [da2], eng=G)

                def pe1(s):
                    d = steps[s]
                    t0, kp = kcol(d["slot"])
                    c0 = d["c0"]
                    b1 = B1[s % 2]
                    msk = d["mask"]
                    kl = KT_[:, t0:t0 + kp]
                    qr = QT_[:, q0 + c0:q0 + 512]
                    if is_sb:
                        b2_ = B2[s % 2]
                        for bb in (b1, b2_):
                            last = (msk is None) and (bb is b1)
                            mm(bb[0:kp, c0:512], kl, qr, True, last, [KT_, QT_], [bb])
                            if msk is not None:
                                mm(bb[0:kp, c0:c0 + 128], identb[:], maskb[:, 0, msk, :], False, bb is b1, [identb, maskb], [bb])
                    else:
                        mm(b1[0:kp, c0:512], kl, qr, True, False, [KT_, QT_], [b1])
                        mm(b1[0:kp, c0:512], onesb[:, 0:kp],
                           cr_[:, 4 * m:4 * m + 4, :].rearrange("p j t -> p (j t)")[:, c0:512],
                           False, msk is None, [onesb, cr_], [b1])
                        if msk is not None:
                            mm(b1[0:kp, c0:c0 + 128], identb[:], maskb[:, 1, msk, :], False, True, [identb, maskb], [b1])

                def act1(s):
                    d = steps[s]
                    t0, kp = kcol(d["slot"])
                    c0 = d["c0"]
                    b1 = B1[s % 2]
                    if is_sb:
                        e_, l_ = Et[s % 2], Lt[s % 2]
                        act(e_[0:kp, c0:512], b1[0:kp, c0:512], AF.Exp, [b1], [e_])
                        act(l_[0:kp, c0:512], e_[0:kp, c0:512], AF.Ln, [e_], [l_], bias=1.0)
                    else:
                        w_ = Wt[s % 3]
                        act(w_[0:kp, c0:512], b1[0:kp, c0:512], AF.Exp, [b1, ncf_cols], [w_],
                            bias=ncf_cols[0:kp, h - 8, d["slot"]:d["slot"] + 1])

                def pe2(s):
                    d = steps[s]
                    t0, kp = kcol(d["slot"])
                    c0 = d["c0"]
                    l_ = Lt[s % 2]
                    mm(B2[s % 2][0:kp, c0:512], negtri[0:kp, 0:kp], l_[0:kp, c0:512], False, True, [negtri, l_], [B2[s % 2]])
                    mm(B3[s % 2][:, c0:512], onesb[0:kp, :], l_[0:kp, c0:512], True, True, [onesb, l_], [B3[s % 2]])

                def dve1(s):
                    d = steps[s]
                    t0, kp = kcol(d["slot"])
                    c0 = d["c0"]
                    t_ = Tt[s % 2]
                    tt(t_[0:kp, c0:512], B2[s % 2][0:kp, c0:512], car[0:kp, c0:512], ALU.subtract, [B2[s % 2], car], [t_])
                    tt(car[:, c0:512], B3[s % 2][:, c0:512], car[:, c0:512], ALU.add, [B3[s % 2], car], [car])

                def act3(s):
                    d = steps[s]
                    t0, kp = kcol(d["slot"])
                    c0 = d["c0"]
                    act(Wt[s % 3][0:kp, c0:512], Tt[s % 2][0:kp, c0:512], AF.Exp, [Tt[s % 2]], [Wt[s % 3]])

                def pe3(s):
                    d = steps[s]
                    t0, kp = kcol(d["slot"])
                    c0 = d["c0"]
                    w_ = Wt[s % 3]
                    last = s == ns - 1
                    mm(PO[:, c0:512], VV_[0:kp, d["slot"], :], w_[0:kp, c0:512], False, last, [VV_, w_], [PO])
                    if not is_sb:
                        dd = da if s % 2 == 0 else da2
                        tt(dd[0:kp, c0:512], dd[0:kp, c0:512], w_[0:kp, c0:512], ALU.add, [dd, w_], [dd])

                if is_sb:
                    for t in range(ns + 2):
                        fox_ctr[0] += 1
                        if fox_ctr[0] % 5 == 0:
                            conv_steps(1, cstC32, cstC16, lookahead=2, on_act=False)
                        if t < ns:
                            pe1(t)
                            act1(t)
                        if 0 <= t - 1 < ns:
                            pe2(t - 1)
                            dve1(t - 1)
                            act3(t - 1)
                        if 0 <= t - 2 < ns:
                            pe3(t - 2)
                    act(ob[:], PO[:], AF.Copy, [PO], [ob])
                else:
                    for t in range(ns + 1):
                        fox_ctr[0] += 1
                        if fox_ctr[0] % 5 == 0:
                            conv_steps(1, cstC32, cstC16, lookahead=2, on_act=(fox_ctr[0] % 10 == 0))
                        if t < ns:
                            pe1(t)
                            act1(t)
                        if 0 <= t - 1 < ns:
                            pe3(t - 1)
                    tt(da[:], da[:], da2[:], ALU.add, [da, da2], [da])
                    cp(dhi[:], da[:], [da], [dhi])
                    tt(dtmp[:], da[:], dhi[:], ALU.subtract, [da, dhi], [dtmp])
                    cp(dlo[:], dtmp[:], [dtmp], [dlo])
                    mm(PD[:], onesb[:], dhi[:], True, False, [onesb, dhi], [PD])
                    mm(PD[:], onesb[:], dlo[:], False, True, [onesb, dlo], [PD])
                    S.op(V, lambda e: e.reciprocal(out=rden[:], in_=PD[:]), [PD], [rden])
                    tt(ob[:], PO[:], rden[:], ALU.mult, [PO, rden], [ob])
                S.dma(G, oT_s[h, :, q0:q0 + 512], ob[:], [ob], [oT_s], stream="ost%d" % ((nmt - 1) % 2))
        conv_steps(NHT, cstC32, cstC16, lookahead=2, on_act=True)
        conv_drain(cstC32, cstC16, on_act=True)
        S.flush()

    S.es = es
    skb = S.tile("skb", [128, 16, 128], BF16)
    with ExitStack() as es35:
        S.es = es35
        skl = S.tile("skl", [128, 16, 128], F32)
        S.dma(Q, skl[:], skT[:], [skT], [skl])
        cp(skb[:], skl[:], [skl], [skb])
        S.flush()
    with ExitStack() as es4:
        S.es = es4
        TC = 256
        NTB = TC // 128
        iota16 = S.tile("iota16", [128, 16], F32)
        cp(iota16[:], c32[:, 3, 0:16], [c32], [iota16])
        ol = [S.tile("ol%d" % i, [128, TC], F32) for i in range(3)]
        osq = [S.tile("osq%d" % i, [128, TC], BF16) for i in range(2)]
        gl = [S.tile("gl%d" % i, [128, TC], BF16) for i in range(2)]
        lnr = S.tile("lnr", [128, TC], F32)
        rsn = [S.tile("rsn%d" % i, [128, TC], F32) for i in range(2)]
        MT = S.tile("MT", [128, KC, TC], BF16)
        mtmp = S.tile("mtmp", [128, TC], F32)
        wos = [S.tile("wos%d" % i, [128, KC, 128], BF16) for i in range(2)]
        wot = [S.tile("wot%d" % i, [128, KC, 256], BF16) for i in range(1)] * 2
        xtl = [S.tile("xtl%d" % i, [128, TC], F32) for i in range(2)]
        hnT = S.tile("hnT", [128, KC, TC], BF16)
        H1 = S.tile("H1", [128, NTB, D], F32)
        xol = [S.tile("xol%d" % i, [128, 256], F32) for i in range(2)]
        qTt = S.tile("qTt", [128, 16, TC], BF16)
        junk = S.tile("junk", [128, D], BF16)
        ss2 = S.tile("ss2", [128, 4], F32)
        r2c = S.tile("r2c", [128, 4], F32)
        Ssc = S.tile("Ssc", [128, 16, 128], F32)
        scr = S.tile("scr", [128, 2048], F32)
        Ss2 = TkView(scr, lambda ap: ap.rearrange("p (a n) -> p a n", a=16))
        TOPV = S.tile("TOPV", [128, 16, 16], F32)
        TOPI = S.tile("TOPI", [128, 16, 16], U32)
        TOPF = S.tile("TOPF", [128, 16, 16], F32)
        CS = TkView(Ssc, lambda ap: ap.rearrange("p a n -> p (a n)").rearrange("p (h c) -> p h c", h=8))
        CS2 = TkView(scr, lambda ap: ap.rearrange("p (a n) -> p a n", a=8))
        BV = S.tile("BV", [128, 8, 16], F32)
        BJ = S.tile("BJ", [128, 8, 16], U32)
        K1 = S.tile("K1", [128, 8, 16], U32)
        K2 = S.tile("K2", [128, 8, 16], U32)
        K1f = S.tile("K1f", [128, 8, 16], F32)
        K2f = S.tile("K2f", [128, 8, 16], F32)
        OH = TkView(scr, lambda ap: ap.rearrange("p (a b c) -> p a b c", a=8, b=16))
        I0f = S.tile("I0f", [128, 8, 16], F32)
        I1f = S.tile("I1f", [128, 8, 16], F32)
        IDXf = S.tile("IDXf", [128, 128], F32)
        IDX = [S.tile("IDX%d" % i, [128, 128], I32) for i in range(2)]
        nbv = S.tile("nbv", [128, 8], F32)
        Eg = S.tile("Eg", [128, 8, 16], F32)
        Zg = S.tile("Zg", [128, 8], F32)
        Ag = S.tile("Ag", [128, 128], F32)
        Wg = S.tile("Wg", [128, 128], F32)
        NR = 7
        UR = [S.tile("UR%d" % i, [128, 2 * D], BF16) for i in range(NR)]
        Gs = S.tile("Gs", [128, 128], F32)
        dg = [S.tile("dg%d" % i, [128, 128], BF16) for i in range(4)]
        H1b = [H1, S.tile("H1b", [128, NTB, D], F32)]
        Egb = [Eg, S.tile("Egb", [128, 8, 16], F32)]
        junk2 = S.tile("junk2", [128, D], BF16)
        g2b = S.tile("g2b", [128, D], BF16)
        S.dma(Q, H1[:, 0, :], g2row[:], [g2row], [H1])
        cp(g2b[:], H1[:, 0, :], [H1], [g2b])
        HG = [S.tile("HG%d" % i, [128, D], BF16) for i in range(2)]
        nrow_c = [0]

        def chunk_work(tc):
            c0 = tc * TC
            H1_ = H1b[tc % 2]
            for grp in range(2):
                pss = ps[4 + grp]
                for hh in range(8):
                    h = grp * 8 + hh
                    o_ = ol[h % 3]
                    s_ = osq[h % 2]
                    S.dma(Q, o_[:], oT_s[h, :, c0:c0 + TC], [oT_s], [o_], stream="ol%d" % (h % 3))
                    act(s_[:], o_[:], AF.Square, [o_], [s_])
                    mm(pss[:, 0:TC], onesb[:], s_[:], hh == 0, hh == 7, [onesb, s_], [pss])
                    yield
                rstd_from(rsn[grp], rsn[grp][:], pss, pss[:, 0:TC], 1.0 / 1024, lnr, lnr[:])
                yield
            for h in range(16):
                o_ = ol[h % 3]
                S.dma(Q, o_[:], oT_s[h, :, c0:c0 + TC], [oT_s], [o_], stream="ol%d" % (h % 3))
                if h < 8:
                    stt(MT[:, h, :], o_[:], vec[:, 34 + h:35 + h], rsn[0][:], ALU.mult, ALU.mult, [o_, vec, rsn[0]], [MT])
                else:
                    g_ = gl[h % 2]
                    S.dma(Q, g_[:], gT_s[h - 8, :, c0:c0 + TC], [gT_s], [g_], stream="gl%d" % (h % 2))
                    stt(mtmp[:], o_[:], vec[:, 34 + h:35 + h], rsn[1][:], ALU.mult, ALU.mult, [o_, vec, rsn[1]], [mtmp])
                    tt(MT[:, h, :], mtmp[:], g_[:], ALU.mult, [mtmp, g_], [MT])
                yield
            for n in range(KC):
                w_ = wos[n % 2]
                S.dma(Q, w_[:], wo_fm[n], [wo_fm], [w_], stream="wos%d" % (n % 2))
                x_ = xtl[n % 2]
                S.dma(Q, x_[:], xT_own[n * 128:(n + 1) * 128, c0:c0 + TC], [xT_own], [x_], stream="xtl%d" % (n % 2))
                pp = ps[6 + n % 2]
                for kc in range(KC):
                    mm(pp[:, 0:TC], w_[:, kc, :], MT[:, kc, :], kc == 0, kc == KC - 1, [w_, MT], [pp])
                tt(hnT[:, n, :], pp[:, 0:TC], x_[:], ALU.add, [pp, x_], [hnT])
                yield
            for ng in range(8):
                w_ = wot[ng % 2]
                S.dma(Q, w_[:], wo_tm[ng], [wo_tm], [w_], stream="wot0")
                for tb in range(NTB):
                    x_ = xol[(ng * NTB + tb) % 2]
                    S.dma(Q, x_[:, 0:256], x_own[c0 + tb * 128:c0 + (tb + 1) * 128, ng * 256:(ng + 1) * 256], [x_own], [x_],
                          stream="xol%d" % ((ng * NTB + tb) % 2))
                    pp = ps[4 + (ng * NTB + tb) % 2]
                    for kc in range(KC):
                        mm(pp[:, 0:256], MT[:, kc, tb * 128:(tb + 1) * 128], w_[:, kc, :], kc == 0, kc == KC - 1, [MT, w_], [pp])
                    tt(H1_[:, tb, ng * 256:(ng + 1) * 256], pp[:, 0:256], x_[:, 0:256], ALU.add, [pp, x_], [H1_])
                yield
            for hp in range(16):
                w_ = wos[hp % 2]
                S.dma(Q, w_[:], wq_fm[hp], [wq_fm], [w_], stream="wos%d" % (hp % 2))
                pp = ps[6 + hp % 2]
                for kc in range(KC):
                    mm(pp[:, 0:TC], w_[:, kc, :], hnT[:, kc, :], kc == 0, kc == KC - 1, [w_, hnT], [pp])
                act(qTt[:, hp, :], pp[:, 0:TC], AF.Copy, [pp], [qTt])
                yield

        def prep_block(tc, tb):
            bi = tc * NTB + tb
            H1_, Eg_, idx_ = H1b[tc % 2], Egb[bi % 2], IDX[bi % 2]
            act(junk2[:], H1_[:, tb, :], AF.Square, [H1_], [junk2, ss2], accum=ss2[:, tb:tb + 1])
            rstd_from(r2c, r2c[:, tb:tb + 1], ss2, ss2[:, tb:tb + 1], 1.0 / D, lnr, lnr[:, 0:1])
            tt(HG[bi % 2][:], H1_[:, tb, :], g2b[:], ALU.mult, [H1_, g2b], [HG[bi % 2]])
            yield
            for b4 in range(4):
                pp = ps[4 + b4]
                for q4 in range(4):
                    hp = b4 * 4 + q4
                    mm(pp[:, q4 * 128:(q4 + 1) * 128], qTt[:, hp, tb * 128:(tb + 1) * 128], skb[:, hp, :], True, True,
                       [qTt, skb], [pp])
                act(Ssc[:, b4 * 4:b4 * 4 + 4, :], pp[:].rearrange("p (a n) -> p a n", a=4), AF.Copy,
                    [pp, r2c], [Ssc], scale=r2c[:, tb:tb + 1])
                yield
            for hp in range(16):
                S.op(V, lambda e, hp=hp: e.max(out=TOPV[:, hp, 0:8], in_=Ssc[:, hp, :]), [Ssc], [TOPV])
                S.op(V, lambda e, hp=hp: e.max_index(out=TOPI[:, hp, 0:8], in_max=TOPV[:, hp, 0:8], in_values=Ssc[:, hp, :]),
                     [Ssc, TOPV], [TOPI])
                S.op(V, lambda e, hp=hp: e.match_replace(out=Ss2[:, hp, :], in_to_replace=TOPV[:, hp, 0:8],
                                                         in_values=Ssc[:, hp, :], imm_value=-1e30), [Ssc, TOPV], [Ss2])
                S.op(V, lambda e, hp=hp: e.max(out=TOPV[:, hp, 8:16], in_=Ss2[:, hp, :]), [Ss2], [TOPV])
                S.op(V, lambda e, hp=hp: e.max_index(out=TOPI[:, hp, 8:16], in_max=TOPV[:, hp, 8:16], in_values=Ss2[:, hp, :]),
                     [Ss2, TOPV], [TOPI])
                yield
            cp(TOPF[:], TOPI[:], [TOPI], [TOPF])
            tv = TOPV[:].rearrange("p (h two) k -> p h two k", two=2)
            tf = TOPF[:].rearrange("p (h two) k -> p h two k", two=2)
            tt(CS[:].rearrange("p h (a b) -> p h a b", a=16),
               tv[:, :, 0, :].unsqueeze(3).to_broadcast([128, 8, 16, 16]),
               tv[:, :, 1, :].unsqueeze(2).to_broadcast([128, 8, 16, 16]), ALU.add, [TOPV], [CS])
            yield
            for hh in range(8):
                S.op(V, lambda e, hh=hh: e.max(out=BV[:, hh, 0:8], in_=CS[:, hh, :]), [CS], [BV])
                S.op(V, lambda e, hh=hh: e.max_index(out=BJ[:, hh, 0:8], in_max=BV[:, hh, 0:8], in_values=CS[:, hh, :]),
                     [CS, BV], [BJ])
                S.op(V, lambda e, hh=hh: e.match_replace(out=CS2[:, hh, :], in_to_replace=BV[:, hh, 0:8],
                                                         in_values=CS[:, hh, :], imm_value=-1e30), [CS, BV], [CS2])
                S.op(V, lambda e, hh=hh: e.max(out=BV[:, hh, 8:16], in_=CS2[:, hh, :]), [CS2], [BV])
                S.op(V, lambda e, hh=hh: e.max_index(out=BJ[:, hh, 8:16], in_max=BV[:, hh, 8:16], in_values=CS2[:, hh, :]),
                     [CS2, BV], [BJ])
                yield
            ts(K1[:], BJ[:], 4, ALU.logical_shift_right, [BJ], [K1])
            ts(K2[:], BJ[:], 15, ALU.bitwise_and, [BJ], [K2])
            cp(K1f[:], K1[:], [K1], [K1f])
            cp(K2f[:], K2[:], [K2], [K2f])
            yield
            iob = iota16[:].unsqueeze(1).unsqueeze(1).to_broadcast([128, 8, 16, 16])
            for (kf_, two, of_) in ((K1f, 0, I0f), (K2f, 1, I1f)):
                tt(OH[:], kf_[:].unsqueeze(3).to_broadcast([128, 8, 16, 16]), iob, ALU.is_equal, [kf_, iota16], [OH])
                yield
                tt(OH[:], OH[:], tf[:, :, two, :].unsqueeze(2).to_broadcast([128, 8, 16, 16]), ALU.mult, [OH, TOPF], [OH], eng=G)
                yield
                S.op(V, lambda e, of_=of_: e.tensor_reduce(out=of_[:], in_=OH[:], axis=AX.X, op=ALU.add), [OH], [of_])
                yield
            stt(IDXf[:], I0f[:].rearrange("p h k -> p (h k)"), 128.0, I1f[:].rearrange("p h k -> p (h k)"),
                ALU.mult, ALU.add, [I0f, I1f], [IDXf])
            cp(idx_[:], IDXf[:], [IDXf], [idx_])
            ts(nbv[:], BV[:, :, 0], -1.0, ALU.mult, [BV], [nbv])
            for hh in range(8):
                act(Eg_[:, hh, :], BV[:, hh, :], AF.Exp, [BV, nbv], [Eg_, Zg], bias=nbv[:, hh:hh + 1], accum=Zg[:, hh:hh + 1])
            S.op(V, lambda e: e.reciprocal(out=Zg[:], in_=Zg[:]), [Zg], [Zg])
            tt(Eg_[:], Eg_[:], Zg[:].unsqueeze(2).to_broadcast([128, 8, 16]), ALU.mult, [Eg_, Zg], [Eg_])
            yield

        def gather_block(tc, tb, filler):
            bi = tc * NTB + tb
            r0 = tc * TC + tb * 128
            H1_, Eg_, idx_ = H1b[tc % 2], Egb[bi % 2], IDX[bi % 2]
            egf = Eg_[:].rearrange("p h k -> p (h k)")
            rows = {}

            def second_half(sl):
                u_ = rows.pop(sl)
                act(Wg[:, sl:sl + 1], Gs[:, sl:sl + 1], AF.Copy, [Gs, Eg_], [Wg], scale=egf[:, sl:sl + 1])
                d_ = dg[sl % 4]
                act(d_[:], identb[:], AF.Copy, [identb, Wg], [d_], scale=Wg[:, sl:sl + 1])
                for n4 in range(4):
                    mm(ps[n4][:], d_[:], u_[:, D + n4 * 512:D + (n4 + 1) * 512], sl == 0, sl == 127, [d_, u_], [ps[n4]])

            for sl in range(128):
                u_ = UR[nrow_c[0] % NR]
                nrow_c[0] += 1
                rows[sl] = u_
                S.dma(G, None, None, [idx_, uv_b], [u_], stream=u_.name,
                      fn=lambda e, u_=u_, idx_=idx_, sl=sl: e.indirect_dma_start(
                          out=u_[:], out_offset=None, in_=uv_b[:],
                          in_offset=bass.IndirectOffsetOnAxis(ap=idx_[:, sl:sl + 1], axis=0)))
                stt(junk[:], u_[:, 0:D], 1.0, HG[bi % 2][:], ALU.mult, ALU.mult, [u_, HG[bi % 2]], [junk, Ag], accum=Ag[:, sl:sl + 1])
                act(Gs[:, sl:sl + 1], Ag[:, sl:sl + 1], AF.Gelu, [Ag, r2c], [Gs], scale=r2c[:, tb:tb + 1])
                if sl >= 1:
                    second_half(sl - 1)
                if sl >= 2:
                    next(filler, None)
            second_half(127)
            for _ in filler:
                pass
            for n4 in range(4):
                tt(H1_[:, tb, n4 * 512:(n4 + 1) * 512], ps[n4][:], H1_[:, tb, n4 * 512:(n4 + 1) * 512], ALU.add,
                   [ps[n4], H1_], [H1_])
            S.dma(Q, out_own[r0:r0 + 128, :], H1_[:, tb, :], [H1_], [out_own], stream="outst")

        blocks = [(tc, tb) for tc in range(TO // TC) for tb in range(NTB)]

        def filler_for(nxt):
            if nxt is None:
                return
            if nxt[1] == 0:
                yield from chunk_work(nxt[0])
            yield from prep_block(*nxt)

        for _ in filler_for(blocks[0]):
            pass
        for bi, (tc, tb) in enumerate(blocks):
            nxt = blocks[bi + 1] if bi + 1 < len(blocks) else None
            gather_block(tc, tb, filler_for(nxt))
        S.flush(final_streams=["outst"])

    S.es = es
    es.close()
    return nc


def make_in_maps(inputs, NJ):
    f = lambda a: np.ascontiguousarray(np.asarray(a, dtype=np.float32))
    x = f(inputs["x"])[0]
    S_ = x.shape[0]
    assert S_ == NCORE * NJ * 128
    meta = f(inputs["meta_tokens"])
    xT_all = np.ascontiguousarray(np.concatenate([meta, x], axis=0).T)
    w_in = f(inputs["w_in"])[0]
    w_out = f(inputs["w_out"])[0]
    w_q = f(inputs["peer_w_query"])[0]
    sk = f(inputs["peer_sub_keys"])[0]
    skT = np.ascontiguousarray(sk.reshape(16, 128, 128).transpose(2, 0, 1))
    u = f(inputs["peer_u"])[0]
    v = f(inputs["peer_v"])[0]
    vecs = np.zeros((128, 64), np.float32)
    vecs[:, 0:16] = f(inputs["norm_mix"])[0].reshape(16, 128).T
    vecs[:, 16:32] = f(inputs["norm_ffn"])[0].reshape(16, 128).T
    vecs[:, 32] = f(inputs["fox_q_gain"])[0]
    vecs[:, 33] = f(inputs["fox_k_gain"])[0]
    vecs[:, 34:42] = f(inputs["sb_out_gain"])[0].reshape(8, 128).T
    vecs[:, 42:50] = f(inputs["fox_out_gain"])[0].reshape(8, 128).T
    vecs[0:8, 50] = f(inputs["b_forget"])[0]
    g2row = np.ascontiguousarray(np.broadcast_to(f(inputs["norm_ffn"])[0][None, :], (128, D)))
    consts = np.zeros((128, 4, 128), np.float32)
    kk = np.arange(128)
    consts[:, 0, :] = np.where(kk[:, None] >= kk[None, :], -1.0, 0.0)
    consts[:, 1, :] = 1.0
    consts[:, 2, :] = np.eye(128, dtype=np.float32)
    consts[:, 3, :] = kk[None, :].astype(np.float32)
    maps = []
    for c in range(NCORE):
        blocks = [c + NCORE * j for j in range(NJ)]
        rows = np.concatenate([np.arange(b * 128, (b + 1) * 128) for b in blocks])
        x_own = np.ascontiguousarray(x[rows])
        xT_own = np.ascontiguousarray(x_own.T)
        mk = np.zeros((128, 2, 8, 128), np.float32)
        for i in range(8):
            if i > c:
                mk[:, :, i, :] = NEG
            elif i == c:
                mk[:, 0, i, :] = np.where(kk[:, None] < kk[None, :], 0.0, NEG)
                mk[:, 1, i, :] = np.where(kk[:, None] <= kk[None, :], 0.0, NEG)
        oh = np.zeros((1, 8), np.float32)
        oh[0, c] = 1.0
        maps.append({
            "xT_all": xT_all, "xT_own": xT_own, "x_own": x_own, "w_in": w_in, "w_out": w_out, "w_q": w_q,
            "skT": skT, "peer_u": u, "peer_v": v, "vecs": vecs, "g2row": g2row, "consts": consts,
            "maskadd": mk, "onehot_c": oh,
        })
    return maps


def assemble(results, NJ):
    S_ = NCORE * NJ * 128
    out = np.zeros((1, S_, D), np.float32)
    for c in range(NCORE):
        o = np.asarray(results[c]["out_own"], dtype=np.float32)
        for j in range(NJ):
            b = c + NCORE * j
            out[0, b * 128:(b + 1) * 128] = o[j * 128:(j + 1) * 128]
    return out


_NC_CACHE = {}


def kernel(**inputs):
    NJ = 16
    if NJ not in _NC_CACHE:
        _NC_CACHE[NJ] = build_nc(NJ)
    nc = _NC_CACHE[NJ]
    maps = make_in_maps(inputs, NJ)
    res = run_bass_kernel_spmd(nc, maps, core_ids=list(range(NCORE)))
    return assemble(res.results, NJ)
```

```python
import math
from contextlib import ExitStack

import numpy as np
import concourse.bass as bass
import concourse.mybir as mybir
from concourse.bass_utils import run_bass_kernel_spmd

F32 = mybir.dt.float32
BF16 = mybir.dt.bfloat16
I32 = mybir.dt.int32
U32 = mybir.dt.uint32
AF = mybir.ActivationFunctionType
ALU = mybir.AluOpType
AX = mybir.AxisListType

D = 2048
KC = 16
NCORE = 8
N_META = 16
EPS = 1e-6
SCALE = 1.0 / math.sqrt(128.0)
NEG = -30000.0
N_EXP = 16384


class Tk:
    def __init__(self, t, name=""):
        self.t = t
        self.name = name
        self.lw = None
        self.rd = []

    def __getitem__(self, k):
        return self.t[k]


class TkView:
    def __init__(self, base, fn):
        self.base = base
        self.fn = fn
        self.name = base.name

    def __getitem__(self, k):
        return self.fn(self.base.t[:])[k]

    @property
    def lw(self):
        return self.base.lw

    @lw.setter
    def lw(self, v):
        self.base.lw = v

    @property
    def rd(self):
        return self.base.rd

    @rd.setter
    def rd(self, v):
        self.base.rd = v


class Op:
    __slots__ = ("eng", "fn", "deps", "dma", "stream", "sidx", "awaited", "mile", "idx")

    def __init__(self, eng, fn, dma=False, stream=None):
        self.eng = eng
        self.fn = fn
        self.deps = []
        self.dma = dma
        self.stream = stream
        self.sidx = 0
        self.awaited = False
        self.mile = 0
        self.idx = 0


class Sched:
    ENGS = ("tensor", "vector", "scalar", "gpsimd", "sync")

    def __init__(self, nc, es):
        self.nc = nc
        self.es = es
        self.es0 = es
        self.ops = {e: [] for e in self.ENGS}
        self.streams = {}
        self.nops = 0

    def tile(self, name, shape, dt):
        return Tk(self.es.enter_context(self.nc.sbuf_tensor(name, list(shape), dt)), name)

    def psum(self, name, shape=(128, 512), dt=F32):
        return Tk(self.es.enter_context(self.nc.psum_tensor(name, list(shape), dt)), name)

    def dram(self, name, shape, dt, kind="Internal"):
        t = self.nc.dram_tensor(name, list(shape), dt, kind=kind)
        return Tk(t.ap(), name)

    def _add(self, op, reads, writes):
        deps = []
        for t in reads:
            if t.lw is not None:
                deps.append(t.lw)
        for t in writes:
            if t.lw is not None and (t.lw.dma or op.dma or t.lw.eng != op.eng):
                deps.append(t.lw)
            deps.extend(r for r in t.rd if r.dma or op.dma or r.eng != op.eng)
        seen = set()
        for d in deps:
            if d is op or id(d) in seen:
                continue
            seen.add(id(d))
            if (not d.dma) and d.eng == op.eng and op.eng == "tensor" and not op.dma:
                continue
            op.deps.append(d)
            d.awaited = True
        for t in reads:
            t.rd = [r for r in t.rd if r.dma or r.eng != op.eng or op.dma] + [op]
        for t in writes:
            t.lw = op
            t.rd = []
        op.idx = self.nops
        self.nops += 1
        self.ops[op.eng].append(op)
        return op

    def op(self, eng, fn, reads=(), writes=()):
        return self._add(Op(eng, fn), reads, writes)

    def dma(self, eng, out, in_, reads=(), writes=(), stream=None, fn=None):
        if stream is None:
            stream = "dma_" + (writes[0].name if writes else "x")
        if fn is None:
            fn = lambda e, out=out, in_=in_: e.dma_start(out=out, in_=in_)
        op = Op(eng, fn, dma=True, stream=stream)
        st = self.streams.setdefault(stream, [])
        if st:
            op.deps.append(st[-1])
            st[-1].awaited = True
        st.append(op)
        op.sidx = len(st)
        return self._add(op, reads, writes)

    def flush(self, final_streams=()):
        nc = self.nc
        if not hasattr(self, "prog"):
            self.prog = {e: self.es0.enter_context(nc.semaphore("prog_" + e)) for e in self.ENGS}
            self.ssem = {}
            self.mcount = {e: 0 for e in self.ENGS}
            self.waited = {e: {} for e in self.ENGS}
            self.first_flush = True
        prog, ssem = self.prog, self.ssem
        for s in self.streams:
            if s not in ssem:
                ssem[s] = self.es0.enter_context(nc.semaphore("s_" + s))
        for e in self.ENGS:
            comp = [o for o in self.ops[e] if not o.dma]
            if comp:
                comp[-1].awaited = True
            m = self.mcount[e]
            pending = []
            for o in comp:
                pending.append(o)
                if o.awaited:
                    m += 1
                    for p in pending:
                        p.mile = m
                    pending = []
            self.mcount[e] = m
        barrier = {}
        if not self.first_flush:
            for e in self.ENGS:
                if self.bar_m[e] > 0:
                    barrier[("p", e)] = self.bar_m[e]
            for s, n in self.bar_s.items():
                if n > 0:
                    barrier[("s", s)] = 16 * n
        streams = self.streams
        block = self.es.enter_context(nc.Block())

        def run(ename, eng):
            waited = self.waited[ename]

            def do_wait(key, val):
                if waited.get(key, 0) >= val:
                    return
                waited[key] = val
                sem = ssem[key[1]] if key[0] == "s" else prog[key[1]]
                eng.wait_ge(sem, val)

            for key, val in barrier.items():
                do_wait(key, val)
            for o in self.ops[ename]:
                need = {}
                for d in o.deps:
                    if d.dma:
                        key = ("s", d.stream)
                        val = 16 * d.sidx
                    else:
                        key = ("p", d.eng)
                        val = d.mile
                    if val > need.get(key, 0):
                        need[key] = val
                for key, val in need.items():
                    do_wait(key, val)
                ins = o.fn(eng)
                if o.dma:
                    ins.then_inc(ssem[o.stream], 16)
                elif o.awaited:
                    ins.then_inc(prog[ename], 1)
            if ename == "sync":
                for s in final_streams:
                    eng.wait_ge(ssem[s], 16 * len(streams[s]))

        @block.tensor
        def _(e):
            run("tensor", e)

        @block.vector
        def _(e):
            run("vector", e)

        @block.scalar
        def _(e):
            run("scalar", e)

        @block.gpsimd
        def _(e):
            run("gpsimd", e)

        @block.sync
        def _(e):
            run("sync", e)

        self.bar_m = dict(self.mcount)
        self.bar_s = {s: len(v) for s, v in self.streams.items()}
        self.first_flush = False
        self.ops = {e: [] for e in self.ENGS}


def build_nc(NJ, debug=False):
    NXB = NCORE * NJ
    NKB = NXB + 1
    T_ALL = N_META + NXB * 128
    TO = NJ * 128
    NMT = NJ // 4
    assert NJ % 4 == 0

    nc = bass.Bass("TRN2", target_bir_lowering=False)
    es = ExitStack()
    es.enter_context(nc.allow_low_precision("bf16 matmul operands by design; fp32 accumulation"))
    S = Sched(nc, es)

    def ext(name, shape, dt=F32, kind="ExternalInput"):
        return Tk(nc.dram_tensor(name, list(shape), dt, kind=kind).ap(), name)

    xT_all = ext("xT_all", [D, T_ALL])
    xT_own = ext("xT_own", [D, TO])
    x_own = ext("x_own", [TO, D])
    w_in = ext("w_in", [D, 7176])
    w_out = ext("w_out", [D, D])
    w_q = ext("w_q", [D, D])
    skT = ext("skT", [128, 16, 128])
    u_t = ext("peer_u", [N_EXP, D])
    v_t = ext("peer_v", [N_EXP, D])
    vecs = ext("vecs", [128, 64])
    g2row = ext("g2row", [128, D])
    consts = ext("consts", [128, 4, 128])
    maskadd = ext("maskadd", [128, 2, 8, 128])
    onehot_c = ext("onehot_c", [1, 8])
    out_own = ext("out_own", [TO, D], kind="ExternalOutput")

    kT_s = S.dram("kT_s", [16, 128, T_ALL], BF16)
    v_s = S.dram("v_s", [16, 128, NKB, 128], BF16)
    y0_s = S.dram("y0_s", [8, T_ALL], F32)
    qT_s = S.dram("qT_s", [16, 128, TO], BF16)
    gT_s = S.dram("gT_s", [8, 128, TO], BF16)
    oT_s = S.dram("oT_s", [16, 128, TO], F32)
    uv_b = S.dram("uv_b", [N_EXP, 2 * D], BF16)
    wo_fm = S.dram("wo_fm", [16, 128, KC, 128], BF16)
    wo_tm = S.dram("wo_tm", [8, 128, KC, 256], BF16)
    wq_fm = S.dram("wq_fm", [16, 128, KC, 128], BF16)

    ps = [S.psum("ps%d" % i) for i in range(8)]

    V, A, P, G, Q = "vector", "scalar", "tensor", "gpsimd", "sync"

    def act(out, in_, func, reads, writes, bias=None, scale=None, accum=None, eng=A):
        kw = {}
        if bias is not None:
            kw["bias"] = bias
        if scale is not None:
            kw["scale"] = scale
        if accum is not None:
            kw["accum_out"] = accum
        return S.op(eng, lambda e: e.activation(out=out, in_=in_, func=func, **kw), reads, writes)

    def tt(out, in0, in1, op, reads, writes, eng=V):
        return S.op(eng, lambda e: e.tensor_tensor(out=out, in0=in0, in1=in1, op=op), reads, writes)

    def ts(out, in0, s1, op0, reads, writes, s2=None, op1=None, eng=V):
        if op1 is None:
            return S.op(eng, lambda e: e.tensor_scalar(out=out, in0=in0, scalar1=s1, scalar2=None, op0=op0), reads, writes)
        return S.op(eng, lambda e: e.tensor_scalar(out=out, in0=in0, scalar1=s1, scalar2=s2, op0=op0, op1=op1), reads, writes)

    def stt(out, in0, scalar, in1, op0, op1, reads, writes, accum=None):
        if accum is None:
            return S.op(V, lambda e: e.scalar_tensor_tensor(out=out, in0=in0, scalar=scalar, in1=in1, op0=op0, op1=op1), reads, writes)
        return S.op(V, lambda e: e.scalar_tensor_tensor(out=out, in0=in0, scalar=scalar, in1=in1, op0=op0, op1=op1, accum_out=accum), reads, writes)

    def cp(out, in_, reads, writes, eng=V):
        return S.op(eng, lambda e: e.tensor_copy(out=out, in_=in_), reads, writes)

    def mm(out, lhsT, rhs, start, stop, reads, writes):
        return S.op(P, lambda e: e.matmul(out, lhsT, rhs, start=start, stop=stop), reads, writes)

    def memset(ap, val, writes, eng=V):
        return S.op(eng, lambda e: e.memset(ap, val), (), writes)

    def rstd_from(out_t, out_ap, ps_t, ps_ap, inv_n, tmp_t, tmp_ap):
        act(tmp_ap, ps_ap, AF.Ln, [ps_t], [tmp_t], bias=EPS, scale=inv_n)
        act(out_ap, tmp_ap, AF.Exp, [tmp_t], [out_t], scale=-0.5)

    c32 = S.tile("c32", [128, 4, 128], F32)
    S.dma(Q, c32[:], consts[:], [consts], [c32])
    negtri = S.tile("negtri", [128, 128], BF16)
    onesb = S.tile("onesb", [128, 128], BF16)
    identb = S.tile("identb", [128, 128], BF16)
    zerob = S.tile("zerob", [128, 512], BF16)
    cp(negtri[:], c32[:, 0, :], [c32], [negtri])
    cp(onesb[:], c32[:, 1, :], [c32], [onesb])
    cp(identb[:], c32[:, 2, :], [c32], [identb])
    memset(zerob[:], 0.0, [zerob])
    vec = S.tile("vec", [128, 64], F32)
    S.dma(Q, vec[:], vecs[:], [vecs], [vec])
    vec2 = S.tile("vec2", [128, 2], F32)
    ts(vec2[:, 0:1], vec[:, 32:33], SCALE, ALU.mult, [vec], [vec2])
    ts(vec2[:, 1:2], vec[:, 50:51], -1.0, ALU.mult, [vec], [vec2])
    maskb = S.tile("maskb", [128, 2, 8, 128], BF16)
    ohc = S.tile("ohc", [128, 8], F32)
    S.dma(Q, ohc[64:65, :], onehot_c[:], [onehot_c], [ohc])
    ncf_cols = S.tile("ncf_cols", [128, 8, NKB], F32)
    cmid = S.tile("cmid", [128, 8, NJ], F32)
    cfull = S.tile("cfull", [128, 8, NJ], F32)

    NHT = 4 * (N_EXP // 128)
    conv_state = [0, 0]

    def conv_load(cst32):
        n_ = conv_state[0]
        if n_ >= NHT:
            return
        conv_state[0] += 1
        src = u_t if n_ < NHT // 2 else v_t
        rt = (n_ % (NHT // 2)) // 2
        hf = n_ % 2
        a = cst32[n_ % len(cst32)]
        S.dma(Q, a[:], src[rt * 128:(rt + 1) * 128, hf * 1024:(hf + 1) * 1024], [src], [a], stream=a.name)

    g2_tile = [None]

    def conv_finish(cst32, cst16, on_act=False):
        n_ = conv_state[1]
        if n_ >= conv_state[0]:
            return
        conv_state[1] += 1
        coff = 0 if n_ < NHT // 2 else D
        rt = (n_ % (NHT // 2)) // 2
        hf = n_ % 2
        a, b = cst32[n_ % len(cst32)], cst16[n_ % len(cst16)]
        if on_act:
            act(b[:], a[:], AF.Copy, [a], [b])
        else:
            cp(b[:], a[:], [a], [b])
        S.dma(Q, uv_b[rt * 128:(rt + 1) * 128, coff + hf * 1024:coff + (hf + 1) * 1024], b[:], [b], [uv_b], stream=b.name)

    def conv_steps(k, cst32, cst16, lookahead=3, on_act=False):
        for _ in range(k):
            while conv_state[0] < min(NHT, conv_state[1] + lookahead):
                conv_load(cst32)
            conv_finish(cst32, cst16, on_act)

    def conv_drain(cst32, cst16, on_act=False):
        while conv_state[1] < conv_state[0]:
            conv_finish(cst32, cst16, on_act)

    def kcol(slot):
        if slot == 0:
            return 0, N_META
        return N_META + 128 * (slot - 1), 128

    with ExitStack() as es0:
        S.es = es0
        m32 = S.tile("m32", [128, 2, 8, 128], F32)
        S.dma(Q, m32[:], maskadd[:], [maskadd], [m32])
        cp(maskb[:], m32[:], [m32], [maskb])
        S.flush()

    with ExitStack() as es1:
        S.es = es1
        GK = 256
        wk = S.tile("wk", [128, KC, 2048], BF16)
        wf = S.tile("wf", [128, KC, 8], BF16)
        wld = [S.tile("wldk%d" % i, [128, 1024], F32) for i in range(2)]
        n = 0
        for (dst, coff, scol) in ((wk, 0, 1024), (wk, 1024, 4096)):
            for kc in range(KC):
                a = wld[n % 2]
                S.dma(Q, a[:], w_in[kc * 128:(kc + 1) * 128, scol:scol + 1024], [w_in], [a])
                if n % 2:
                    ts(dst[:, kc, coff:coff + 1024], a[:], vec[:, kc:kc + 1], ALU.mult, [a, vec], [dst])
                else:
                    act(dst[:, kc, coff:coff + 1024], a[:], AF.Copy, [a, vec], [dst], scale=vec[:, kc:kc + 1])
                n += 1
        wfl = S.tile("wfl", [128, KC, 8], F32)
        S.dma(Q, wfl[:], None, [w_in], [wfl],
              fn=lambda e: e.dma_start(out=wfl[:], in_=w_in[:, 7168:7176].rearrange("(kc p) c -> p kc c", p=128)))
        for kc in range(KC):
            ts(wf[:, kc, :], wfl[:, kc, :], vec[:, kc:kc + 1], ALU.mult, [wfl, vec], [wf])
        xs = [S.tile("xsk%d" % i, [128, KC, GK], F32) for i in range(2)]
        xbk2 = [S.tile("xbk%d" % i, [128, KC, GK], BF16) for i in range(2)]
        sqx2 = [S.tile("sqx%d" % i, [128, KC, GK], BF16) for i in range(2)]
        lnt = S.tile("lnt", [128, GK], F32)
        lnt2 = [S.tile("lnt2_%d" % i, [128, GK], F32) for i in range(2)]
        rsk = S.tile("rsk", [128, GK], F32)
        kst2 = [S.tile("kstk%d" % i, [128, 16, GK], BF16) for i in range(2)]
        kf = [S.tile("kf%d" % i, [128, GK], F32) for i in range(2)]
        sqk = [S.tile("sqk%d" % i, [128, GK], BF16) for i in range(2)]
        rk = [S.tile("rk%d" % i, [128, GK], F32) for i in range(2)]
        yst = [S.tile("yst%d" % i, [8, GK], F32) for i in range(2)]
        xTv = xT_all[:].rearrange("(kc p) t -> p kc t", p=128)
        groups = [(0, N_META)] + [(N_META + GK * i, GK) for i in range(NXB * 128 // GK)]
        def k_prologue(gi):
            t0, Gn = groups[gi]
            x_ = xs[gi % 2]
            S.dma(Q, x_[:, :, 0:Gn], xTv[:, :, t0:t0 + Gn], [xT_all], [x_], stream="xsk%d" % (gi % 2))
            cp(xbk2[gi % 2][:, :, 0:Gn], x_[:, :, 0:Gn], [x_], [xbk2[gi % 2]], eng=G)
            act(sqx2[gi % 2][:, :, 0:Gn], x_[:, :, 0:Gn], AF.Square, [x_], [sqx2[gi % 2]])

        k_prologue(0)
        for gi, (t0, Gn) in enumerate(groups):
            xbk, sqx, kst = xbk2[gi % 2], sqx2[gi % 2], kst2[gi % 2]
            for kc in range(KC):
                mm(ps[0][:, 0:Gn], onesb[:], sqx[:, kc, 0:Gn], kc == 0, kc == KC - 1, [onesb, sqx], [ps[0]])
            rstd_from(rsk, rsk[:, 0:Gn], ps[0], ps[0][:, 0:Gn], 1.0 / D, lnt, lnt[:, 0:Gn])
            for kc in range(KC):
                mm(ps[1][0:8, 0:Gn], wf[:, kc, :], xbk[:, kc, 0:Gn], kc == 0, kc == KC - 1, [wf, xbk], [ps[1]])
            ys = yst[gi % 2]
            tt(ys[:, 0:Gn], ps[1][0:8, 0:Gn], rsk[0:8, 0:Gn], ALU.mult, [ps[1], rsk], [ys])
            S.dma(A, y0_s[:, t0:t0 + Gn], ys[:, 0:Gn], [ys], [y0_s], stream="yst%d" % (gi % 2))
            if gi + 1 < len(groups):
                k_prologue(gi + 1)

            def fox_norm(h):
                kf_, sqk_, rk_ = kf[h % 2], sqk[h % 2], rk[h % 2]
                pn = ps[6 + h % 2]
                mm(pn[:, 0:Gn], onesb[:], sqk_[:, 0:Gn], True, True, [onesb, sqk_], [pn])
                rstd_from(rk_, rk_[:, 0:Gn], pn, pn[:, 0:Gn], 1.0 / 128, lnt2[h % 2], lnt2[h % 2][:, 0:Gn])
                stt(kst[:, h, 0:Gn], kf_[:, 0:Gn], vec[:, 33:34], rk_[:, 0:Gn], ALU.mult, ALU.mult, [kf_, vec, rk_], [kst])

            order = [8, 9, 0, 10, 1, 11, 2, 12, 3, 13, 4, 14, 5, 15, 6, 7]
            pend = []
            for oi, h in enumerate(order):
                pk = ps[2 + oi % 4]
                for kc in range(KC):
                    mm(pk[:, 0:Gn], wk[:, kc, h * 128:(h + 1) * 128], xbk[:, kc, 0:Gn], kc == 0, kc == KC - 1, [wk, xbk], [pk])
                if h < 8:
                    tt(kst[:, h, 0:Gn], pk[:, 0:Gn], rsk[:, 0:Gn], ALU.mult, [pk, rsk], [kst])
                else:
                    kf_, sqk_ = kf[h % 2], sqk[h % 2]
                    tt(kf_[:, 0:Gn], pk[:, 0:Gn], rsk[:, 0:Gn], ALU.mult, [pk, rsk], [kf_])
                    act(sqk_[:, 0:Gn], kf_[:, 0:Gn], AF.Square, [kf_], [sqk_])
                    pend.append((oi, h))
                while pend and pend[0][0] <= oi - 1:
                    fox_norm(pend.pop(0)[1])
            while pend:
                fox_norm(pend.pop(0)[1])
            S.dma(A, kT_s[:, :, t0:t0 + Gn].rearrange("h d t -> d h t"), kst[:, :, 0:Gn], [kst], [kT_s], stream="kstk%d" % (gi % 2))
        S.flush()

    with ExitStack() as es1v:
        S.es = es1v
        wv = S.tile("wv", [128, KC, 2048], BF16)
        stg32 = [S.tile("stg32_%d" % i, [128, 1024], F32) for i in range(2)]
        wld = stg32
        n = 0
        for (dst, coff, scol) in ((wv, 0, 2048), (wv, 1024, 5120)):
            for kc in range(KC):
                a = wld[n % 2]
                S.dma(Q, a[:], w_in[kc * 128:(kc + 1) * 128, scol:scol + 1024], [w_in], [a], stream="stg32_%d" % (n % 2))
                if n % 2:
                    ts(dst[:, kc, coff:coff + 1024], a[:], vec[:, kc:kc + 1], ALU.mult, [a, vec], [dst])
                else:
                    act(dst[:, kc, coff:coff + 1024], a[:], AF.Copy, [a, vec], [dst], scale=vec[:, kc:kc + 1])
                n += 1
        xs = [S.tile("xs%d" % i, [128, KC, 128], F32) for i in range(3)]
        xb = [S.tile("xb%d" % i, [128, KC, 128], BF16) for i in range(2)]
        sq = [S.tile("sq%d" % i, [128, KC, 128], BF16) for i in range(2)]
        lntv = [S.tile("lntv%d" % i, [128, 1], F32) for i in range(2)]
        rcol = [S.tile("rcol%d" % i, [128, 1], F32) for i in range(2)]
        vst = [S.tile("vst%d" % i, [128, 2048], BF16) for i in range(2)]
        cstB32 = [S.tile("cstB32_%d" % i, [128, 1024], F32) for i in range(4)]
        cstB16 = [S.tile("cstB16_%d" % i, [128, 1024], BF16) for i in range(2)]

        wjobs = [(src, dst, gcol, kc, hf) for (src, dst, gcol) in ((w_out, wo_fm, None), (w_q, wq_fm, 16))
                 for kc in range(KC) for hf in range(2)]
        wstate = [0, 0]

        def w_load():
            n_ = wstate[0]
            if n_ >= len(wjobs):
                return
            wstate[0] += 1
            src, dst, gcol, kc, hf = wjobs[n_]
            a = cstB32[n_ % 4]
            S.dma(Q, a[:], src[kc * 128:(kc + 1) * 128, hf * 1024:(hf + 1) * 1024], [src], [a], stream=a.name)

        def w_finish():
            n_ = wstate[1]
            if n_ >= wstate[0]:
                return
            wstate[1] += 1
            src, dst, gcol, kc, hf = wjobs[n_]
            a, b = cstB32[n_ % 4], cstB16[n_ % 2]
            if gcol is None:
                cp(b[:], a[:], [a], [b])
            else:
                ts(b[:], a[:], vec[:, gcol + kc:gcol + kc + 1], ALU.mult, [a, vec], [b])
            S.dma(Q, dst[hf * 8:(hf + 1) * 8, :, kc, :].rearrange("n p c -> p n c"),
                  b[:].rearrange("p (n c) -> p n c", c=128), [b], [dst], stream=b.name)
            if gcol is None:
                S.dma(Q, wo_tm[hf * 4:(hf + 1) * 4, :, kc, :].rearrange("g p c -> p g c"),
                      b[:].rearrange("p (g c) -> p g c", c=256), [b], [wo_tm], stream=b.name + "t")

        def w_step():
            while wstate[0] < min(len(wjobs), wstate[1] + 3):
                w_load()
            w_finish()

        def v_prologue(slot):
            t0, Gn = kcol(slot)
            x_ = xs[slot % 3]
            S.dma(Q, x_[:, :, 0:Gn], xTv[:, :, t0:t0 + Gn], [xT_all], [x_], stream="xs%d" % (slot % 3))
            cp(xb[slot % 2][:, :, 0:Gn], x_[:, :, 0:Gn], [x_], [xb[slot % 2]], eng=G)
            act(sq[slot % 2][:, :, 0:Gn], x_[:, :, 0:Gn], AF.Square, [x_], [sq[slot % 2]])

        v_prologue(0)
        for slot in range(NKB):
            t0, Gn = kcol(slot)
            b2 = slot % 2
            xb_, sq_, rc_, vs_ = xb[b2], sq[b2], rcol[b2], vst[b2]
            for kc in range(KC):
                mm(ps[b2][0:Gn, 0:1], sq_[:, kc, 0:Gn], onesb[:, 0:1], kc == 0, kc == KC - 1, [onesb, sq_], [ps[b2]])
            rstd_from(rc_, rc_[0:Gn, :], ps[b2], ps[b2][0:Gn, 0:1], 1.0 / D, lntv[b2], lntv[b2][0:Gn, 0:1])
            if slot + 1 < NKB:
                v_prologue(slot + 1)
            if slot % 2 == 0:
                w_step()
            for cg in range(4):
                pv = ps[2 + (slot * 4 + cg) % 6]
                for kc in range(KC):
                    mm(pv[0:Gn, :], xb_[:, kc, 0:Gn], wv[:, kc, cg * 512:(cg + 1) * 512], kc == 0, kc == KC - 1, [xb_, wv], [pv])
                act(vs_[0:Gn, cg * 512:(cg + 1) * 512], pv[0:Gn, :], AF.Copy, [pv, rc_], [vs_], scale=rc_[0:Gn, 0:1])
            S.dma(A, v_s[:, 0:Gn, slot, :].rearrange("h t d -> t h d"), vs_[0:Gn, :].rearrange("t (h d) -> t h d", h=16),
                  [vs_], [v_s], stream="vst%d" % b2)
        while wstate[1] < len(wjobs):
            w_step()
        S.flush()

    with ExitStack() as es2:
        S.es = es2
        wqg = S.tile("wqg", [128, KC, 3072], BF16)
        wld = [S.tile("wld2_%d" % i, [128, 1024], F32) for i in range(2)]
        n = 0
        for (coff, scol) in ((0, 0), (1024, 3072), (2048, 6144)):
            for kc in range(KC):
                a = wld[n % 2]
                S.dma(Q, a[:], w_in[kc * 128:(kc + 1) * 128, scol:scol + 1024], [w_in], [a])
                if n % 2:
                    ts(wqg[:, kc, coff:coff + 1024], a[:], vec[:, kc:kc + 1], ALU.mult, [a, vec], [wqg])
                else:
                    act(wqg[:, kc, coff:coff + 1024], a[:], AF.Copy, [a, vec], [wqg], scale=vec[:, kc:kc + 1])
                n += 1
        x2 = S.tile("x2", [128, KC, 512], F32)
        xb2 = S.tile("xb2", [128, KC, 512], BF16)
        sq2 = S.tile("sq2", [128, KC, 512], BF16)
        ln2 = S.tile("ln2", [128, 512], F32)
        rs2 = S.tile("rs2", [128, 512], F32)
        qf = [S.tile("qf%d" % i, [128, 512], F32) for i in range(2)]
        qsq = [S.tile("qsq%d" % i, [128, 512], BF16) for i in range(2)]
        rq = [S.tile("rq%d" % i, [128, 512], F32) for i in range(2)]
        qst = [S.tile("qst%d" % i, [128, 512], BF16) for i in range(3)]
        xTo = xT_own[:].rearrange("(kc p) t -> p kc t", p=128)
        for gi in range(TO // 512):
            c0 = gi * 512
            S.dma(Q, x2[:], xTo[:, :, c0:c0 + 512], [xT_own], [x2])
            cp(xb2[:], x2[:], [x2], [xb2])
            act(sq2[:], x2[:], AF.Square, [x2], [sq2])
            for kc in range(KC):
                mm(ps[0][:], onesb[:], sq2[:, kc, :], kc == 0, kc == KC - 1, [onesb, sq2], [ps[0]])
            rstd_from(rs2, rs2[:], ps[0], ps[0][:], 1.0 / D, ln2, ln2[:])
            for cc in range(24):
                pq = ps[2 + cc % 4]
                for kc in range(KC):
                    mm(pq[:], wqg[:, kc, cc * 128:(cc + 1) * 128], xb2[:, kc, :], kc == 0, kc == KC - 1, [wqg, xb2], [pq])
                o_ = qst[cc % 3]
                if cc < 8:
                    stt(o_[:], pq[:], SCALE, rs2[:], ALU.mult, ALU.mult, [pq, rs2], [o_])
                    S.dma(A, qT_s[cc, :, c0:c0 + 512], o_[:], [o_], [qT_s], stream="qst%d" % (cc % 3))
                elif cc < 16:
                    f_, s_, r_ = qf[cc % 2], qsq[cc % 2], rq[cc % 2]
                    tt(f_[:], pq[:], rs2[:], ALU.mult, [pq, rs2], [f_])
                    act(s_[:], f_[:], AF.Square, [f_], [s_])
                    mm(ps[1][:], onesb[:], s_[:], True, True, [onesb, s_], [ps[1]])
                    rstd_from(r_, r_[:], ps[1], ps[1][:], 1.0 / 128, ln2, ln2[:])
                    stt(o_[:], f_[:], vec2[:, 0:1], r_[:], ALU.mult, ALU.mult, [f_, vec2, r_], [o_])
                    S.dma(A, qT_s[cc, :, c0:c0 + 512], o_[:], [o_], [qT_s], stream="qst%d" % (cc % 3))
                else:
                    f_ = qf[cc % 2]
                    tt(f_[:], pq[:], rs2[:], ALU.mult, [pq, rs2], [f_])
                    act(f_[:], f_[:], AF.Exp, [f_], [f_], scale=-1.0)
                    ts(f_[:], f_[:], 1.0, ALU.add, [f_], [f_])
                    S.op(V, lambda e, o=o_, f=f_: e.reciprocal(out=o[:], in_=f[:]), [f_], [o_])
                    S.dma(A, gT_s[cc - 16, :, c0:c0 + 512], o_[:], [o_], [gT_s], stream="qst%d" % (cc % 3))
        S.flush()

    with ExitStack() as es2b:
        S.es = es2b
        yl = S.tile("yl", [8, T_ALL], F32)
        ncf = S.tile("ncf", [8, T_ALL], F32)
        one8 = S.tile("one8", [8, 1], F32)
        id32 = S.tile("id32", [8, 8], F32)
        memset(one8[:], 1.0, [one8])
        cp(id32[:], c32[0:8, 2, 0:8], [c32], [id32])
        S.dma(Q, yl[:], y0_s[:], [y0_s], [yl])
        act(yl[:], yl[:], AF.Exp, [yl, vec2], [yl], bias=vec2[0:8, 1:2], scale=-1.0)
        act(yl[:], yl[:], AF.Ln, [yl], [yl], bias=1.0)
        CH = 2048
        pos = 0
        while pos < T_ALL:
            n_ = min(CH, T_ALL - pos)
            init = 0.0 if pos == 0 else ncf[:, pos - 1:pos]
            S.op(V, lambda e, pos=pos, n_=n_, init=init: e.tensor_tensor_scan(
                out=ncf[:, pos:pos + n_], data0=one8[:, 0:1].to_broadcast([8, n_]), data1=yl[:, pos:pos + n_],
                initial=init, op0=ALU.mult, op1=ALU.add), [yl, one8, ncf], [ncf])
            pos += n_
        for s0 in range(0, NKB, 64):
            ns = min(64, NKB - s0)
            pt = ps[(s0 // 64) % 2]
            for si in range(ns):
                t0, Gn = kcol(s0 + si)
                S.op(P, lambda e, pt=pt, si=si, t0=t0, Gn=Gn: e.transpose(pt[0:Gn, si * 8:si * 8 + 8], ncf[0:8, t0:t0 + Gn], id32[:]),
                     [ncf, id32], [pt])
            if s0 == 0:
                cp(ncf_cols[0:16, :, 0:1].rearrange("p h s -> p s h"), pt[0:16, 0:8].rearrange("p (s h) -> p s h", h=8),
                   [pt], [ncf_cols])
                cp(ncf_cols[:, :, 1:ns].rearrange("p h s -> p s h"), pt[:, 8:ns * 8].rearrange("p (s h) -> p s h", h=8),
                   [pt], [ncf_cols])
            else:
                cp(ncf_cols[:, :, s0:s0 + ns].rearrange("p h s -> p s h"), pt[:, 0:ns * 8].rearrange("p (s h) -> p s h", h=8),
                   [pt], [ncf_cols])
        tmpc = S.tile("tmpc", [128, 8, NJ, 8], F32)
        tt(tmpc[64:65], ncf_cols[64:65, :, 1:1 + NXB].rearrange("p h (j c) -> p h j c", c=8),
           ohc[64:65, :].unsqueeze(1).unsqueeze(1).to_broadcast([1, 8, NJ, 8]), ALU.mult, [ncf_cols, ohc], [tmpc])
        S.op(V, lambda e: e.tensor_reduce(out=cmid[64:65], in_=tmpc[64:65], axis=AX.X, op=ALU.add), [tmpc], [cmid])
        mm(ps[2][:, 0:8 * NJ], c32[64:65, 1, :], cmid[64:65].rearrange("p h j -> p (h j)"), True, True, [c32, cmid], [ps[2]])
        cp(cfull[:].rearrange("p h j -> p (h j)"), ps[2][:, 0:8 * NJ], [ps[2]], [cfull])
        S.flush()

    with ExitStack() as es3:
        S.es = es3
        KT = [S.tile("KT%d" % i, [128, T_ALL], BF16) for i in range(2)]
        VV = [S.tile("VV%d" % i, [128, NKB, 128], BF16) for i in range(2)]
        QT = [S.tile("QT%d" % i, [128, TO], BF16) for i in range(2)]
        crow = [S.tile("crow%d" % i, [128, NJ, 128], BF16) for i in range(2)]
        Et = [S.tile("Et%d" % i, [128, 512], F32) for i in range(2)]
        Lt = [S.tile("Lt%d" % i, [128, 512], BF16) for i in range(2)]
        Tt = [S.tile("Tt%d" % i, [128, 512], F32) for i in range(2)]
        Wt = [S.tile("Wt%d" % i, [128, 512], BF16) for i in range(3)]
        carry = [S.tile("carry%d" % i, [128, 512], F32) for i in range(2)]
        osb = [S.tile("osb%d" % i, [128, 512], F32) for i in range(2)]
        rden = S.tile("rden", [128, 512], F32)
        dacc = [S.tile("dacc%d" % i, [128, 512], F32) for i in range(4)]
        dhi = S.tile("dhi", [128, 512], BF16)
        dlo = S.tile("dlo", [128, 512], BF16)
        dtmp = S.tile("dtmp", [128, 512], F32)
        B1 = [ps[0], ps[1]]
        B2 = [ps[2], ps[3]]
        B3 = [ps[4], ps[5]]
        PO, PD = ps[6], ps[7]

        cstC32 = [S.tile("cstC32_%d" % i, [128, 1024], F32) for i in range(2)]
        cstC16 = [S.tile("cstC16_%d" % i, [128, 1024], BF16) for i in range(2)]
        fox_ctr = [0]

        def steps_for(m):
            st = []
            for g in range(4 * m + 3, -1, -1):
                r = max(0, g - 4 * m)
                for i in range(7, -1, -1):
                    st.append(dict(slot=1 + 8 * g + i, c0=128 * r, mask=(i if g >= 4 * m else None)))
            st.append(dict(slot=0, c0=0, mask=None))
            return st

        nmt = 0
        for h in range(16):
            hb = h % 2
            KT_, VV_, QT_ = KT[hb], VV[hb], QT[hb]
            is_sb = h < 8
            S.dma(Q, KT_[:], kT_s[h], [kT_s], [KT_])
            S.dma(Q, VV_[:], v_s[h], [v_s], [VV_])
            S.dma(Q, QT_[:], qT_s[h], [qT_s], [QT_])
            cr_ = crow[hb]
            if not is_sb:
                hf = h - 8
                ts(cr_[:], cfull[:, hf, :].unsqueeze(2).to_broadcast([128, NJ, 128]), -1.0 / 128, ALU.mult, [cfull], [cr_])
            for m in range(NMT):
                steps = steps_for(m)
                ns = len(steps)
                car = carry[nmt % 2]
                ob = osb[nmt % 2]
                nmt += 1
                q0 = 512 * m
                if is_sb:
                    memset(car[:], 0.0, [car], eng=G)
                mm(PO[:], zerob[:, 0:128], zerob[:], True, False, [zerob], [PO])
                da = dacc[nmt % 2]
                da2 = dacc[2 + nmt % 2]
                if not is_sb:
                    memset(da[:], 0.0, [da], eng=G)
                    memset(da2[:], 0.0, [da2], eng=G)

                def pe1(s):
                    d = steps[s]
                    t0, kp = kcol(d["slot"])
                    c0 = d["c0"]
                    b1 = B1[s % 2]
                    msk = d["mask"]
                    kl = KT_[:, t0:t0 + kp]
                    qr = QT_[:, q0 + c0:q0 + 512]
                    if is_sb:
                        b2_ = B2[s % 2]
                        for bb in (b1, b2_):
                            last = (msk is None) and (bb is b1)
                            mm(bb[0:kp, c0:512], kl, qr, True, last, [KT_, QT_], [bb])
                            if msk is not None:
                                mm(bb[0:kp, c0:c0 + 128], identb[:], maskb[:, 0, msk, :], False, bb is b1, [identb, maskb], [bb])
                    else:
                        mm(b1[0:kp, c0:512], kl, qr, True, False, [KT_, QT_], [b1])
                        mm(b1[0:kp, c0:512], onesb[:, 0:kp],
                           cr_[:, 4 * m:4 * m + 4, :].rearrange("p j t -> p (j t)")[:, c0:512],
                           False, msk is None, [onesb, cr_], [b1])
                        if msk is not None:
                            mm(b1[0:kp, c0:c0 + 128], identb[:], maskb[:, 1, msk, :], False, True, [identb, maskb], [b1])

                def act1(s):
                    d = steps[s]
                    t0, kp = kcol(d["slot"])
                    c0 = d["c0"]
                    b1 = B1[s % 2]
                    if is_sb:
                        e_, l_ = Et[s % 2], Lt[s % 2]
                        act(e_[0:kp, c0:512], b1[0:kp, c0:512], AF.Exp, [b1], [e_])
                        act(l_[0:kp, c0:512], e_[0:kp, c0:512], AF.Ln, [e_], [l_], bias=1.0)
                    else:
                        w_ = Wt[s % 3]
                        act(w_[0:kp, c0:512], b1[0:kp, c0:512], AF.Exp, [b1, ncf_cols], [w_],
                            bias=ncf_cols[0:kp, h - 8, d["slot"]:d["slot"] + 1])

                def pe2(s):
                    d = steps[s]
                    t0, kp = kcol(d["slot"])
                    c0 = d["c0"]
                    l_ = Lt[s % 2]
                    mm(B2[s % 2][0:kp, c0:512], negtri[0:kp, 0:kp], l_[0:kp, c0:512], False, True, [negtri, l_], [B2[s % 2]])
                    mm(B3[s % 2][:, c0:512], onesb[0:kp, :], l_[0:kp, c0:512], True, True, [onesb, l_], [B3[s % 2]])

                def dve1(s):
                    d = steps[s]
                    t0, kp = kcol(d["slot"])
                    c0 = d["c0"]
                    t_ = Tt[s % 2]
                    tt(t_[0:kp, c0:512], B2[s % 2][0:kp, c0:512], car[0:kp, c0:512], ALU.subtract, [B2[s % 2], car], [t_])
                    tt(car[:, c0:512], B3[s % 2][:, c0:512], car[:, c0:512], ALU.add, [B3[s % 2], car], [car])

                def act3(s):
                    d = steps[s]
                    t0, kp = kcol(d["slot"])
                    c0 = d["c0"]
                    act(Wt[s % 3][0:kp, c0:512], Tt[s % 2][0:kp, c0:512], AF.Exp, [Tt[s % 2]], [Wt[s % 3]])

                def pe3(s):
                    d = steps[s]
                    t0, kp = kcol(d["slot"])
                    c0 = d["c0"]
                    w_ = Wt[s % 3]
                    last = s == ns - 1
                    mm(PO[:, c0:512], VV_[0:kp, d["slot"], :], w_[0:kp, c0:512], False, last, [VV_, w_], [PO])
                    if not is_sb:
                        dd = da if s % 2 == 0 else da2
                        tt(dd[0:kp, c0:512], dd[0:kp, c0:512], w_[0:kp, c0:512], ALU.add, [dd, w_], [dd])

                if is_sb:
                    for t in range(ns + 2):
                        fox_ctr[0] += 1
                        if fox_ctr[0] % 5 == 0:
                            conv_steps(1, cstC32, cstC16, lookahead=2, on_act=False)
                        if t < ns:
                            pe1(t)
                            act1(t)
                        if 0 <= t - 1 < ns:
                            pe2(t - 1)
                            dve1(t - 1)
                            act3(t - 1)
                        if 0 <= t - 2 < ns:
                            pe3(t - 2)
                    act(ob[:], PO[:], AF.Copy, [PO], [ob])
                else:
                    for t in range(ns + 1):
                        fox_ctr[0] += 1
                        if fox_ctr[0] % 5 == 0:
                            conv_steps(1, cstC32, cstC16, lookahead=2, on_act=(fox_ctr[0] % 10 == 0))
                        if t < ns:
                            pe1(t)
                            act1(t)
                        if 0 <= t - 1 < ns:
                            pe3(t - 1)
                    tt(da[:], da[:], da2[:], ALU.add, [da, da2], [da])
                    cp(dhi[:], da[:], [da], [dhi])
                    tt(dtmp[:], da[:], dhi[:], ALU.subtract, [da, dhi], [dtmp])
                    cp(dlo[:], dtmp[:], [dtmp], [dlo])
                    mm(PD[:], onesb[:], dhi[:], True, False, [onesb, dhi], [PD])
                    mm(PD[:], onesb[:], dlo[:], False, True, [onesb, dlo], [PD])
                    S.op(V, lambda e: e.reciprocal(out=rden[:], in_=PD[:]), [PD], [rden])
                    tt(ob[:], PO[:], rden[:], ALU.mult, [PO, rden], [ob])
                S.dma(G, oT_s[h, :, q0:q0 + 512], ob[:], [ob], [oT_s], stream="ost%d" % ((nmt - 1) % 2))
        conv_steps(NHT, cstC32, cstC16, lookahead=2, on_act=True)
        conv_drain(cstC32, cstC16, on_act=True)
        S.flush()

    S.es = es
    skb = S.tile("skb", [128, 16, 128], BF16)
    with ExitStack() as es35:
        S.es = es35
        skl = S.tile("skl", [128, 16, 128], F32)
        S.dma(Q, skl[:], skT[:], [skT], [skl])
        cp(skb[:], skl[:], [skl], [skb])
        S.flush()
    with ExitStack() as es4:
        S.es = es4
        TC = 256
        NTB = TC // 128
        iota16 = S.tile("iota16", [128, 16], F32)
        cp(iota16[:], c32[:, 3, 0:16], [c32], [iota16])
        ol = [S.tile("ol%d" % i, [128, TC], F32) for i in range(3)]
        osq = [S.tile("osq%d" % i, [128, TC], BF16) for i in range(2)]
        gl = [S.tile("gl%d" % i, [128, TC], BF16) for i in range(2)]
        lnr = S.tile("lnr", [128, TC], F32)
        rsn = [S.tile("rsn%d" % i, [128, TC], F32) for i in range(2)]
        MT = S.tile("MT", [128, KC, TC], BF16)
        mtmp = S.tile("mtmp", [128, TC], F32)
        wos = [S.tile("wos%d" % i, [128, KC, 128], BF16) for i in range(2)]
        wot = [S.tile("wot%d" % i, [128, KC, 256], BF16) for i in range(1)] * 2
        xtl = [S.tile("xtl%d" % i, [128, TC], F32) for i in range(2)]
        hnT = S.tile("hnT", [128, KC, TC], BF16)
        H1 = S.tile("H1", [128, NTB, D], F32)
        xol = [S.tile("xol%d" % i, [128, 256], F32) for i in range(2)]
        qTt = S.tile("qTt", [128, 16, TC], BF16)
        junk = S.tile("junk", [128, D], BF16)
        ss2 = S.tile("ss2", [128, 4], F32)
        r2c = S.tile("r2c", [128, 4], F32)
        Ssc = S.tile("Ssc", [128, 16, 128], F32)
        scr = S.tile("scr", [128, 2048], F32)
        Ss2 = TkView(scr, lambda ap: ap.rearrange("p (a n) -> p a n", a=16))
        TOPV = S.tile("TOPV", [128, 16, 16], F32)
        TOPI = S.tile("TOPI", [128, 16, 16], U32)
        TOPF = S.tile("TOPF", [128, 16, 16], F32)
        CS = TkView(Ssc, lambda ap: ap.rearrange("p a n -> p (a n)").rearrange("p (h c) -> p h c", h=8))
        CS2 = TkView(scr, lambda ap: ap.rearrange("p (a n) -> p a n", a=8))
        BV = S.tile("BV", [128, 8, 16], F32)
        BJ = S.tile("BJ", [128, 8, 16], U32)
        K1 = S.tile("K1", [128, 8, 16], U32)
        K2 = S.tile("K2", [128, 8, 16], U32)
        K1f = S.tile("K1f", [128, 8, 16], F32)
        K2f = S.tile("K2f", [128, 8, 16], F32)
        OH = TkView(scr, lambda ap: ap.rearrange("p (a b c) -> p a b c", a=8, b=16))
        I0f = S.tile("I0f", [128, 8, 16], F32)
        I1f = S.tile("I1f", [128, 8, 16], F32)
        IDXf = S.tile("IDXf", [128, 128], F32)
        IDX = [S.tile("IDX%d" % i, [128, 128], I32) for i in range(2)]
        nbv = S.tile("nbv", [128, 8], F32)
        Eg = S.tile("Eg", [128, 8, 16], F32)
        Zg = S.tile("Zg", [128, 8], F32)
        Ag = S.tile("Ag", [128, 128], F32)
        Wg = S.tile("Wg", [128, 128], F32)
        NR = 7
        UR = [S.tile("UR%d" % i, [128, 2 * D], BF16) for i in range(NR)]
        Gs = S.tile("Gs", [128, 128], F32)
        dg = [S.tile("dg%d" % i, [128, 128], BF16) for i in range(4)]
        H1b = [H1, S.tile("H1b", [128, NTB, D], F32)]
        Egb = [Eg, S.tile("Egb", [128, 8, 16], F32)]
        junk2 = S.tile("junk2", [128, D], BF16)
        g2b = S.tile("g2b", [128, D], BF16)
        S.dma(Q, H1[:, 0, :], g2row[:], [g2row], [H1])
        cp(g2b[:], H1[:, 0, :], [H1], [g2b])
        HG = [S.tile("HG%d" % i, [128, D], BF16) for i in range(2)]
        nrow_c = [0]

        def chunk_work(tc):
            c0 = tc * TC
            H1_ = H1b[tc % 2]
            for grp in range(2):
                pss = ps[4 + grp]
                for hh in range(8):
                    h = grp * 8 + hh
                    o_ = ol[h % 3]
                    s_ = osq[h % 2]
                    S.dma(Q, o_[:], oT_s[h, :, c0:c0 + TC], [oT_s], [o_], stream="ol%d" % (h % 3))
                    act(s_[:], o_[:], AF.Square, [o_], [s_])
                    mm(pss[:, 0:TC], onesb[:], s_[:], hh == 0, hh == 7, [onesb, s_], [pss])
                    yield
                rstd_from(rsn[grp], rsn[grp][:], pss, pss[:, 0:TC], 1.0 / 1024, lnr, lnr[:])
                yield
            for h in range(16):
                o_ = ol[h % 3]
                S.dma(Q, o_[:], oT_s[h, :, c0:c0 + TC], [oT_s], [o_], stream="ol%d" % (h % 3))
                if h < 8:
                    stt(MT[:, h, :], o_[:], vec[:, 34 + h:35 + h], rsn[0][:], ALU.mult, ALU.mult, [o_, vec, rsn[0]], [MT])
                else:
                    g_ = gl[h % 2]
                    S.dma(Q, g_[:], gT_s[h - 8, :, c0:c0 + TC], [gT_s], [g_], stream="gl%d" % (h % 2))
                    stt(mtmp[:], o_[:], vec[:, 34 + h:35 + h], rsn[1][:], ALU.mult, ALU.mult, [o_, vec, rsn[1]], [mtmp])
                    tt(MT[:, h, :], mtmp[:], g_[:], ALU.mult, [mtmp, g_], [MT])
                yield
            for n in range(KC):
                w_ = wos[n % 2]
                S.dma(Q, w_[:], wo_fm[n], [wo_fm], [w_], stream="wos%d" % (n % 2))
                x_ = xtl[n % 2]
                S.dma(Q, x_[:], xT_own[n * 128:(n + 1) * 128, c0:c0 + TC], [xT_own], [x_], stream="xtl%d" % (n % 2))
                pp = ps[6 + n % 2]
                for kc in range(KC):
                    mm(pp[:, 0:TC], w_[:, kc, :], MT[:, kc, :], kc == 0, kc == KC - 1, [w_, MT], [pp])
                tt(hnT[:, n, :], pp[:, 0:TC], x_[:], ALU.add, [pp, x_], [hnT])
                yield
            for ng in range(8):
                w_ = wot[ng % 2]
                S.dma(Q, w_[:], wo_tm[ng], [wo_tm], [w_], stream="wot0")
                for tb in range(NTB):
                    x_ = xol[(ng * NTB + tb) % 2]
                    S.dma(Q, x_[:, 0:256], x_own[c0 + tb * 128:c0 + (tb + 1) * 128, ng * 256:(ng + 1) * 256], [x_own], [x_],
                          stream="xol%d" % ((ng * NTB + tb) % 2))
                    pp = ps[4 + (ng * NTB + tb) % 2]
                    for kc in range(KC):
                        mm(pp[:, 0:256], MT[:, kc, tb * 128:(tb + 1) * 128], w_[:, kc, :], kc == 0, kc == KC - 1, [MT, w_], [pp])
                    tt(H1_[:, tb, ng * 256:(ng + 1) * 256], pp[:, 0:256], x_[:, 0:256], ALU.add, [pp, x_], [H1_])
                yield
            for hp in range(16):
                w_ = wos[hp % 2]
                S.dma(Q, w_[:], wq_fm[hp], [wq_fm], [w_], stream="wos%d" % (hp % 2))
                pp = ps[6 + hp % 2]
                for kc in range(KC):
                    mm(pp[:, 0:TC], w_[:, kc, :], hnT[:, kc, :], kc == 0, kc == KC - 1, [w_, hnT], [pp])
                act(qTt[:, hp, :], pp[:, 0:TC], AF.Copy, [pp], [qTt])
                yield

        def prep_block(tc, tb):
            bi = tc * NTB + tb
            H1_, Eg_, idx_ = H1b[tc % 2], Egb[bi % 2], IDX[bi % 2]
            act(junk2[:], H1_[:, tb, :], AF.Square, [H1_], [junk2, ss2], accum=ss2[:, tb:tb + 1])
            rstd_from(r2c, r2c[:, tb:tb + 1], ss2, ss2[:, tb:tb + 1], 1.0 / D, lnr, lnr[:, 0:1])
            tt(HG[bi % 2][:], H1_[:, tb, :], g2b[:], ALU.mult, [H1_, g2b], [HG[bi % 2]])
            yield
            for b4 in range(4):
                pp = ps[4 + b4]
                for q4 in range(4):
                    hp = b4 * 4 + q4
                    mm(pp[:, q4 * 128:(q4 + 1) * 128], qTt[:, hp, tb * 128:(tb + 1) * 128], skb[:, hp, :], True, True,
                       [qTt, skb], [pp])
                act(Ssc[:, b4 * 4:b4 * 4 + 4, :], pp[:].rearrange("p (a n) -> p a n", a=4), AF.Copy,
                    [pp, r2c], [Ssc], scale=r2c[:, tb:tb + 1])
                yield
            for hp in range(16):
                S.op(V, lambda e, hp=hp: e.max(out=TOPV[:, hp, 0:8], in_=Ssc[:, hp, :]), [Ssc], [TOPV])
                S.op(V, lambda e, hp=hp: e.max_index(out=TOPI[:, hp, 0:8], in_max=TOPV[:, hp, 0:8], in_values=Ssc[:, hp, :]),
                     [Ssc, TOPV], [TOPI])
                S.op(V, lambda e, hp=hp: e.match_replace(out=Ss2[:, hp, :], in_to_replace=TOPV[:, hp, 0:8],
                                                         in_values=Ssc[:, hp, :], imm_value=-1e30), [Ssc, TOPV], [Ss2])
                S.op(V, lambda e, hp=hp: e.max(out=TOPV[:, hp, 8:16], in_=Ss2[:, hp, :]), [Ss2], [TOPV])
                S.op(V, lambda e, hp=hp: e.max_index(out=TOPI[:, hp, 8:16], in_max=TOPV[:, hp, 8:16], in_values=Ss2[:, hp, :]),
                     [Ss2, TOPV], [TOPI])
                yield
            cp(TOPF[:], TOPI[:], [TOPI], [TOPF])
            tv = TOPV[:].rearrange("p (h two) k -> p h two k", two=2)
            tf = TOPF[:].rearrange("p (h two) k -> p h two k", two=2)
            tt(CS[:].rearrange("p h (a b) -> p h a b", a=16),
               tv[:, :, 0, :].unsqueeze(3).to_broadcast([128, 8, 16, 16]),
               tv[:, :, 1, :].unsqueeze(2).to_broadcast([128, 8, 16, 16]), ALU.add, [TOPV], [CS])
            yield
            for hh in range(8):
                S.op(V, lambda e, hh=hh: e.max(out=BV[:, hh, 0:8], in_=CS[:, hh, :]), [CS], [BV])
                S.op(V, lambda e, hh=hh: e.max_index(out=BJ[:, hh, 0:8], in_max=BV[:, hh, 0:8], in_values=CS[:, hh, :]),
                     [CS, BV], [BJ])
                S.op(V, lambda e, hh=hh: e.match_replace(out=CS2[:, hh, :], in_to_replace=BV[:, hh, 0:8],
                                                         in_values=CS[:, hh, :], imm_value=-1e30), [CS, BV], [CS2])
                S.op(V, lambda e, hh=hh: e.max(out=BV[:, hh, 8:16], in_=CS2[:, hh, :]), [CS2], [BV])
                S.op(V, lambda e, hh=hh: e.max_index(out=BJ[:, hh, 8:16], in_max=BV[:, hh, 8:16], in_values=CS2[:, hh, :]),
                     [CS2, BV], [BJ])
                yield
            ts(K1[:], BJ[:], 4, ALU.logical_shift_right, [BJ], [K1])
            ts(K2[:], BJ[:], 15, ALU.bitwise_and, [BJ], [K2])
            cp(K1f[:], K1[:], [K1], [K1f])
            cp(K2f[:], K2[:], [K2], [K2f])
            yield
            iob = iota16[:].unsqueeze(1).unsqueeze(1).to_broadcast([128, 8, 16, 16])
            for (kf_, two, of_) in ((K1f, 0, I0f), (K2f, 1, I1f)):
                tt(OH[:], kf_[:].unsqueeze(3).to_broadcast([128, 8, 16, 16]), iob, ALU.is_equal, [kf_, iota16], [OH])
                yield
                tt(OH[:], OH[:], tf[:, :, two, :].unsqueeze(2).to_broadcast([128, 8, 16, 16]), ALU.mult, [OH, TOPF], [OH], eng=G)
                yield
                S.op(V, lambda e, of_=of_: e.tensor_reduce(out=of_[:], in_=OH[:], axis=AX.X, op=ALU.add), [OH], [of_])
                yield
            stt(IDXf[:], I0f[:].rearrange("p h k -> p (h k)"), 128.0, I1f[:].rearrange("p h k -> p (h k)"),
                ALU.mult, ALU.add, [I0f, I1f], [IDXf])
            cp(idx_[:], IDXf[:], [IDXf], [idx_])
            ts(nbv[:], BV[:, :, 0], -1.0, ALU.mult, [BV], [nbv])
            for hh in range(8):
                act(Eg_[:, hh, :], BV[:, hh, :], AF.Exp, [BV, nbv], [Eg_, Zg], bias=nbv[:, hh:hh + 1], accum=Zg[:, hh:hh + 1])
            S.op(V, lambda e: e.reciprocal(out=Zg[:], in_=Zg[:]), [Zg], [Zg])
            tt(Eg_[:], Eg_[:], Zg[:].unsqueeze(2).to_broadcast([128, 8, 16]), ALU.mult, [Eg_, Zg], [Eg_])
            yield

        def gather_block(tc, tb, filler):
            bi = tc * NTB + tb
            r0 = tc * TC + tb * 128
            H1_, Eg_, idx_ = H1b[tc % 2], Egb[bi % 2], IDX[bi % 2]
            egf = Eg_[:].rearrange("p h k -> p (h k)")
            rows = {}

            def second_half(sl):
                u_ = rows.pop(sl)
                act(Wg[:, sl:sl + 1], Gs[:, sl:sl + 1], AF.Copy, [Gs, Eg_], [Wg], scale=egf[:, sl:sl + 1])
                d_ = dg[sl % 4]
                act(d_[:], identb[:], AF.Copy, [identb, Wg], [d_], scale=Wg[:, sl:sl + 1])
                for n4 in range(4):
                    mm(ps[n4][:], d_[:], u_[:, D + n4 * 512:D + (n4 + 1) * 512], sl == 0, sl == 127, [d_, u_], [ps[n4]])

            for sl in range(128):
                u_ = UR[nrow_c[0] % NR]
                nrow_c[0] += 1
                rows[sl] = u_
                S.dma(G, None, None, [idx_, uv_b], [u_], stream=u_.name,
                      fn=lambda e, u_=u_, idx_=idx_, sl=sl: e.indirect_dma_start(
                          out=u_[:], out_offset=None, in_=uv_b[:],
                          in_offset=bass.IndirectOffsetOnAxis(ap=idx_[:, sl:sl + 1], axis=0)))
                stt(junk[:], u_[:, 0:D], 1.0, HG[bi % 2][:], ALU.mult, ALU.mult, [u_, HG[bi % 2]], [junk, Ag], accum=Ag[:, sl:sl + 1])
                act(Gs[:, sl:sl + 1], Ag[:, sl:sl + 1], AF.Gelu, [Ag, r2c], [Gs], scale=r2c[:, tb:tb + 1])
                if sl >= 1:
                    second_half(sl - 1)
                if sl >= 2:
                    next(filler, None)
            second_half(127)
            for _ in filler:
                pass
            for n4 in range(4):
                tt(H1_[:, tb, n4 * 512:(n4 + 1) * 512], ps[n4][:], H1_[:, tb, n4 * 512:(n4 + 1) * 512], ALU.add,
                   [ps[n4], H1_], [H1_])
            S.dma(Q, out_own[r0:r0 + 128, :], H1_[:, tb, :], [H1_], [out_own], stream="outst")

        blocks = [(tc, tb) for tc in range(TO // TC) for tb in range(NTB)]

        def filler_for(nxt):
            if nxt is None:
                return
            if nxt[1] == 0:
                yield from chunk_work(nxt[0])
            yield from prep_block(*nxt)

        for _ in filler_for(blocks[0]):
            pass
        for bi, (tc, tb) in enumerate(blocks):
            nxt = blocks[bi + 1] if bi + 1 < len(blocks) else None
            gather_block(tc, tb, filler_for(nxt))
        S.flush(final_streams=["outst"])

    S.es = es
    es.close()
    return nc


def make_in_maps(inputs, NJ):
    f = lambda a: np.ascontiguousarray(np.asarray(a, dtype=np.float32))
    x = f(inputs["x"])[0]
    S_ = x.shape[0]
    assert S_ == NCORE * NJ * 128
    meta = f(inputs["meta_tokens"])
    xT_all = np.ascontiguousarray(np.concatenate([meta, x], axis=0).T)
    w_in = f(inputs["w_in"])[0]
    w_out = f(inputs["w_out"])[0]
    w_q = f(inputs["peer_w_query"])[0]
    sk = f(inputs["peer_sub_keys"])[0]
    skT = np.ascontiguousarray(sk.reshape(16, 128, 128).transpose(2, 0, 1))
    u = f(inputs["peer_u"])[0]
    v = f(inputs["peer_v"])[0]
    vecs = np.zeros((128, 64), np.float32)
    vecs[:, 0:16] = f(inputs["norm_mix"])[0].reshape(16, 128).T
    vecs[:, 16:32] = f(inputs["norm_ffn"])[0].reshape(16, 128).T
    vecs[:, 32] = f(inputs["fox_q_gain"])[0]
    vecs[:, 33] = f(inputs["fox_k_gain"])[0]
    vecs[:, 34:42] = f(inputs["sb_out_gain"])[0].reshape(8, 128).T
    vecs[:, 42:50] = f(inputs["fox_out_gain"])[0].reshape(8, 128).T
    vecs[0:8, 50] = f(inputs["b_forget"])[0]
    g2row = np.ascontiguousarray(np.broadcast_to(f(inputs["norm_ffn"])[0][None, :], (128, D)))
    consts = np.zeros((128, 4, 128), np.float32)
    kk = np.arange(128)
    consts[:, 0, :] = np.where(kk[:, None] >= kk[None, :], -1.0, 0.0)
    consts[:, 1, :] = 1.0
    consts[:, 2, :] = np.eye(128, dtype=np.float32)
    consts[:, 3, :] = kk[None, :].astype(np.float32)
    maps = []
    for c in range(NCORE):
        blocks = [c + NCORE * j for j in range(NJ)]
        rows = np.concatenate([np.arange(b * 128, (b + 1) * 128) for b in blocks])
        x_own = np.ascontiguousarray(x[rows])
        xT_own = np.ascontiguousarray(x_own.T)
        mk = np.zeros((128, 2, 8, 128), np.float32)
        for i in range(8):
            if i > c:
                mk[:, :, i, :] = NEG
            elif i == c:
                mk[:, 0, i, :] = np.where(kk[:, None] < kk[None, :], 0.0, NEG)
                mk[:, 1, i, :] = np.where(kk[:, None] <= kk[None, :], 0.0, NEG)
        oh = np.zeros((1, 8), np.float32)
        oh[0, c] = 1.0
        maps.append({
            "xT_all": xT_all, "xT_own": xT_own, "x_own": x_own, "w_in": w_in, "w_out": w_out, "w_q": w_q,
            "skT": skT, "peer_u": u, "peer_v": v, "vecs": vecs, "g2row": g2row, "consts": consts,
            "maskadd": mk, "onehot_c": oh,
        })
    return maps


def assemble(results, NJ):
    S_ = NCORE * NJ * 128
    out = np.zeros((1, S_, D), np.float32)
    for c in range(NCORE):
        o = np.asarray(results[c]["out_own"], dtype=np.float32)
        for j in range(NJ):
            b = c + NCORE * j
            out[0, b * 128:(b + 1) * 128] = o[j * 128:(j + 1) * 128]
    return out


_NC_CACHE = {}


def kernel(**inputs):
    NJ = 16
    if NJ not in _NC_CACHE:
        _NC_CACHE[NJ] = build_nc(NJ)
    nc = _NC_CACHE[NJ]
    maps = make_in_maps(inputs, NJ)
    res = run_bass_kernel_spmd(nc, maps, core_ids=list(range(NCORE)))
    return assemble(res.results, NJ)
```
